# Optimizing a Trainium2 kernel written in Bass

```python
import math
import jax
import jax.numpy as jnp
from jax import lax
import numpy as np

D_MODEL = 2048
BATCH = 4
SEQ = 4096
DEPTH = 1

CHUNK = 64

CONV_CH = 1024
CONV_TAPS = 31
S5_WIDTH = 1024
S5_GROUP = 16
S5_GROUPS = S5_WIDTH // S5_GROUP
S5_STATE = 64
N_BRANCHES = 2
COL_CONV_V = CONV_CH
COL_CONV_G = 2 * CONV_CH
COL_S5 = 2 * CONV_CH + S5_WIDTH
IN_COLS = COL_S5 + N_BRANCHES * D_MODEL
N_EXPERTS = 32
TOP_K = 4
D_FF = 2048
SWIGLU_ALPHA = 1.702
SWIGLU_LIMIT = 7.0
MOE_BLOCK = 256
EPS = 1e-6
N_ADA = 6
ADA_SCALE = 0.5

kernel_name = 'hybrid_conformer_s5_moe_adaln_block'


def rms_norm(x, g):
    xf = x.astype(jnp.float32)
    y = xf * lax.rsqrt(jnp.mean(xf * xf, axis=-1, keepdims=True) + EPS)
    return (y * g.astype(jnp.float32)).astype(x.dtype)


def layer_norm(x, g, b):
    xf = x.astype(jnp.float32)
    mu = jnp.mean(xf, axis=-1, keepdims=True)
    xc = xf - mu
    var = jnp.mean(xc * xc, axis=-1, keepdims=True)
    y = xc * lax.rsqrt(var + EPS) * g.astype(jnp.float32) + b.astype(jnp.float32)
    return y.astype(x.dtype)


def causal_depthwise_conv(v, w, b):
    y = lax.conv_general_dilated(
        v, w[:, None, :].astype(v.dtype), window_strides=(1,),
        padding=[(CONV_TAPS - 1, 0)],
        dimension_numbers=('NWC', 'WIO', 'NWC'),
        feature_group_count=v.shape[-1])
    return y + b.astype(v.dtype)


def _complex_linear_combine(left, right):
    ar1, ai1, br1, bi1 = left
    ar2, ai2, br2, bi2 = right
    ar = ar2 * ar1 - ai2 * ai1
    ai = ar2 * ai1 + ai2 * ar1
    br = ar2 * br1 - ai2 * bi1 + br2
    bi = ar2 * bi1 + ai2 * br1 + bi2
    return (ar, ai, br, bi)


def s5_layer(u, a_re, a_im, log_dt, b_re, b_im, c_re, c_im, d_skip):
    f32 = jnp.float32
    bsz, seq, _ = u.shape
    uf = u.astype(f32)
    a_re = a_re.astype(f32)
    a_im = a_im.astype(f32)
    dt = jnp.exp(log_dt.astype(f32))[:, None]
    mag = jnp.exp(a_re * dt)
    abar_re = mag * jnp.cos(a_im * dt)
    abar_im = mag * jnp.sin(a_im * dt)
    den = a_re * a_re + a_im * a_im
    num_re = abar_re - 1.0
    coef_re = (num_re * a_re + abar_im * a_im) / den
    coef_im = (abar_im * a_re - num_re * a_im) / den
    b_re = b_re.astype(f32)
    b_im = b_im.astype(f32)
    bb_re = coef_re[:, :, None] * b_re - coef_im[:, :, None] * b_im
    bb_im = coef_re[:, :, None] * b_im + coef_im[:, :, None] * b_re
    ug = uf.reshape(bsz, seq, S5_GROUPS, S5_GROUP).transpose(1, 0, 2, 3)
    bu_re = jnp.einsum('lbgh,gnh->lbgn', ug, bb_re)
    bu_im = jnp.einsum('lbgh,gnh->lbgn', ug, bb_im)
    ar = jnp.broadcast_to(abar_re[None, None], (seq, 1, S5_GROUPS, S5_STATE))
    ai = jnp.broadcast_to(abar_im[None, None], (seq, 1, S5_GROUPS, S5_STATE))
    _, _, s_re, s_im = lax.associative_scan(_complex_linear_combine, (ar, ai, bu_re, bu_im), axis=0)
    y = (jnp.einsum('ghn,lbgn->blgh', c_re.astype(f32), s_re)
         - jnp.einsum('ghn,lbgn->blgh', c_im.astype(f32), s_im))
    y = y.reshape(bsz, seq, S5_WIDTH) + d_skip.astype(f32) * uf
    return y.astype(u.dtype)


def parallel_mixer(h, w_in, gate_b, dw_w, dw_b, cln_g, cln_b, cp_w, cp_b,
                   s5_a_re, s5_a_im, s5_log_dt, s5_b_re, s5_b_im, s5_c_re, s5_c_im, s5_d,
                   glu_wa, glu_wb, w_out):
    proj = h @ w_in
    conv_v = proj[..., :COL_CONV_V]
    conv_g = proj[..., COL_CONV_V:COL_CONV_G]
    s5_in = proj[..., COL_CONV_G:COL_S5]
    gate_logits = proj[..., COL_S5:] + gate_b
    v = conv_v * jax.nn.sigmoid(conv_g)
    v = causal_depthwise_conv(v, dw_w, dw_b)
    v = jax.nn.silu(layer_norm(v, cln_g, cln_b))
    y_conv = v @ cp_w + cp_b
    y = jax.nn.gelu(s5_layer(s5_in, s5_a_re, s5_a_im, s5_log_dt, s5_b_re, s5_b_im,
                             s5_c_re, s5_c_im, s5_d))
    y_s5 = (y @ glu_wa) * jax.nn.sigmoid(y @ glu_wb)
    gates = jax.nn.sigmoid(gate_logits.astype(jnp.float32)).astype(h.dtype)
    merged = gates[..., :D_MODEL] * y_conv + gates[..., D_MODEL:] * y_s5
    return merged @ w_out


def moe_ffn(h, router_w, router_b, w1, b1, w2, b2):
    bsz, seq, dm = h.shape
    n_tok = bsz * seq
    n_asg = n_tok * TOP_K
    xt = h.reshape(n_tok, dm)
    logits = (xt @ router_w + router_b).astype(jnp.float32)
    top_val, top_idx = lax.top_k(logits, TOP_K)
    gates = jax.nn.softmax(top_val, axis=-1)
    flat_e = top_idx.reshape(n_asg).astype(jnp.int32)
    flat_tok = jnp.broadcast_to(jnp.arange(n_tok, dtype=jnp.int32)[:, None], (n_tok, TOP_K)).reshape(n_asg)
    flat_g = gates.reshape(n_asg)
    order = jnp.argsort(flat_e)
    e_s = flat_e[order]
    tok_s = flat_tok[order]
    g_s = flat_g[order]
    counts = jnp.bincount(flat_e, length=N_EXPERTS).astype(jnp.int32)
    starts = jnp.cumsum(counts) - counts
    padded = (counts + MOE_BLOCK - 1) // MOE_BLOCK * MOE_BLOCK
    pends = jnp.cumsum(padded)
    pstarts = pends - padded
    dest = pstarts[e_s] + jnp.arange(n_asg, dtype=jnp.int32) - starts[e_s]
    n_blocks = -(-n_asg // MOE_BLOCK) + N_EXPERTS
    n_rows = n_blocks * MOE_BLOCK
    tok_buf = jnp.zeros((n_rows,), jnp.int32).at[dest].set(tok_s)
    g_buf = jnp.zeros((n_rows,), jnp.float32).at[dest].set(g_s)
    blk_start = jnp.arange(n_blocks, dtype=jnp.int32) * MOE_BLOCK
    blk_e = jnp.minimum(jnp.searchsorted(pends, blk_start, side='right'), N_EXPERTS - 1).astype(jnp.int32)

    def expert_block(args):
        tok, gate, e = args
        xb = xt[tok]
        hb = xb @ w1[e] + b1[e]
        x_glu = jnp.minimum(hb[:, 0::2], SWIGLU_LIMIT)
        x_lin = jnp.clip(hb[:, 1::2], -SWIGLU_LIMIT, SWIGLU_LIMIT)
        act = x_glu * jax.nn.sigmoid(SWIGLU_ALPHA * x_glu) * (x_lin + 1.0)
        yb = act @ w2[e] + b2[e]
        return yb * gate[:, None].astype(yb.dtype)

    y_buf = lax.map(expert_block, (tok_buf.reshape(n_blocks, MOE_BLOCK),
                                   g_buf.reshape(n_blocks, MOE_BLOCK), blk_e))
    y = jnp.zeros((n_tok, dm), h.dtype).at[tok_buf].add(y_buf.reshape(n_rows, dm).astype(h.dtype))
    return y.reshape(bsz, seq, dm)


def setup_inputs(seed: int = 0) -> dict:
    key = jax.random.key(seed)
    ks = jax.random.split(key, 40)
    f32 = jnp.float32

    def nrm(k, shape, scale):
        return jax.random.normal(k, shape, f32) * scale

    L_ = DEPTH
    D = D_MODEL
    n_idx = jnp.arange(S5_STATE, dtype=f32)
    return {
        'x': nrm(ks[0], (BATCH, SEQ, D), 1.0),
        'c': nrm(ks[1], (BATCH, D), 1.0),
        'ada_w': nrm(ks[2], (L_, D, N_ADA * D), ADA_SCALE * D ** -0.5),
        'ada_b': nrm(ks[3], (L_, N_ADA * D), 0.02),
        'pre_mix_g': 1.0 + nrm(ks[4], (L_, D), 0.05),
        'post_mix_g': 1.0 + nrm(ks[5], (L_, D), 0.05),
        'pre_ffn_g': 1.0 + nrm(ks[6], (L_, D), 0.05),
        'post_ffn_g': 1.0 + nrm(ks[7], (L_, D), 0.05),
        'w_in': nrm(ks[8], (L_, D, IN_COLS), D ** -0.5),
        'gate_b': nrm(ks[9], (L_, N_BRANCHES * D), 0.02),
        'dw_w': nrm(ks[10], (L_, CONV_TAPS, CONV_CH), CONV_TAPS ** -0.5),
        'dw_b': nrm(ks[11], (L_, CONV_CH), 0.02),
        'cln_g': 1.0 + nrm(ks[12], (L_, CONV_CH), 0.05),
        'cln_b': nrm(ks[13], (L_, CONV_CH), 0.02),
        'cp_w': nrm(ks[14], (L_, CONV_CH, D), CONV_CH ** -0.5),
        'cp_b': nrm(ks[15], (L_, D), 0.02),
        's5_a_re': -0.5 + nrm(ks[16], (L_, S5_GROUPS, S5_STATE), 0.01),
        's5_a_im': math.pi * n_idx + nrm(ks[17], (L_, S5_GROUPS, S5_STATE), 0.01),
        's5_log_dt': jax.random.uniform(ks[18], (L_, S5_GROUPS), f32, math.log(1e-3), math.log(1e-1)),
        's5_b_re': nrm(ks[19], (L_, S5_GROUPS, S5_STATE, S5_GROUP), (2 * S5_GROUP) ** -0.5),
        's5_b_im': nrm(ks[20], (L_, S5_GROUPS, S5_STATE, S5_GROUP), (2 * S5_GROUP) ** -0.5),
        's5_c_re': nrm(ks[21], (L_, S5_GROUPS, S5_GROUP, S5_STATE), (2 * S5_STATE) ** -0.5),
        's5_c_im': nrm(ks[22], (L_, S5_GROUPS, S5_GROUP, S5_STATE), (2 * S5_STATE) ** -0.5),
        's5_d': nrm(ks[23], (L_, S5_WIDTH), 1.0),
        'glu_wa': nrm(ks[24], (L_, S5_WIDTH, D), S5_WIDTH ** -0.5),
        'glu_wb': nrm(ks[25], (L_, S5_WIDTH, D), S5_WIDTH ** -0.5),
        'w_out': nrm(ks[26], (L_, D, D), D ** -0.5),
        'router_w': nrm(ks[27], (L_, D, N_EXPERTS), D ** -0.5),
        'router_b': nrm(ks[28], (L_, N_EXPERTS), 0.01),
        'w1': nrm(ks[29], (L_, N_EXPERTS, D, 2 * D_FF), D ** -0.5),
        'b1': nrm(ks[30], (L_, N_EXPERTS, 2 * D_FF), 0.02),
        'w2': nrm(ks[31], (L_, N_EXPERTS, D_FF, D), D_FF ** -0.5),
        'b2': nrm(ks[32], (L_, N_EXPERTS, D), 0.02),
    }


def reference(x, c, ada_w, ada_b, pre_mix_g, post_mix_g, pre_ffn_g, post_ffn_g,
              w_in, gate_b, dw_w, dw_b, cln_g, cln_b, cp_w, cp_b,
              s5_a_re, s5_a_im, s5_log_dt, s5_b_re, s5_b_im, s5_c_re, s5_c_im, s5_d,
              glu_wa, glu_wb, w_out, router_w, router_b, w1, b1, w2, b2):
    for l in range(DEPTH):
        ada = jax.nn.silu(c) @ ada_w[l] + ada_b[l]
        sh1, sc1, gt1, sh2, sc2, gt2 = jnp.split(ada[:, None, :], N_ADA, axis=-1)
        h = rms_norm(x, pre_mix_g[l]) * (1.0 + sc1) + sh1
        m = parallel_mixer(h, w_in[l], gate_b[l], dw_w[l], dw_b[l], cln_g[l], cln_b[l],
                           cp_w[l], cp_b[l], s5_a_re[l], s5_a_im[l], s5_log_dt[l],
                           s5_b_re[l], s5_b_im[l], s5_c_re[l], s5_c_im[l], s5_d[l],
                           glu_wa[l], glu_wb[l], w_out[l])
        x = x + gt1 * rms_norm(m, post_mix_g[l])
        h = rms_norm(x, pre_ffn_g[l]) * (1.0 + sc2) + sh2
        f = moe_ffn(h, router_w[l], router_b[l], w1[l], b1[l], w2[l], b2[l])
        x = x + gt2 * rms_norm(f, post_ffn_g[l])
    return x
```

```python
import math
from contextlib import ExitStack
import numpy as np
import concourse.bass as bass
import concourse.mybir as mybir
from concourse.bass_utils import run_bass_kernel_spmd

F32 = mybir.dt.float32
BF16 = mybir.dt.bfloat16
AF = mybir.ActivationFunctionType
ALU = mybir.AluOpType
AX = mybir.AxisListType

D = 2048
NTOK = 2048
TB = 128
SBK = 16
NBLK = NTOK // TB
E = 32
MTB = 512
EPS = 1e-6

V_PREMIX, V_PREFFN, V_GATEB, V_DWB, V_CLNG, V_CLNB, V_CPB, V_S5D = 0, 16, 32, 64, 72, 80, 88, 104
V_DWW = 112
V_B1G = V_DWW + 8 * 31
V_B1L = V_B1G + 512
V_B2 = V_B1L + 512
V_M0 = V_B2 + 512
NV = V_M0 + 2


class Buf:
    __slots__ = ("t", "w", "r")

    def __init__(self, t):
        self.t = t
        self.w = None
        self.r = {}

    def __getitem__(self, idx):
        return self.t[idx]


class Prog:
    R = 8

    def __init__(self):
        self.ops = {k: [] for k in ("pe", "act", "dve", "pool", "sp")}
        self.cnt = {k: 0 for k in self.ops}
        self.known = {k: {} for k in self.ops}
        self.dq = {"sp": 0, "pool": 0}

    def _emit(self, eng, fn, R, W, key, amt, val, extra=()):
        need = {}

        def add(tok):
            if tok is None:
                return
            k, v = tok
            if need.get(k, 0) < v:
                need[k] = v

        for b in R:
            add(b.w)
        for b in W:
            add(b.w)
            for k, v in b.r.items():
                add((k, v))
        for t in extra:
            add(t)
        waits = []
        for k, v in need.items():
            if k == ("c", "pe") and eng == "pe":
                continue
            if self.known[eng].get(k, 0) >= v:
                continue
            self.known[eng][k] = v
            waits.append((k, v))
        self.ops[eng].append((waits, fn, key, amt))
        tok = (key, val)
        for b in W:
            b.w = tok
            b.r = {}
        for b in R:
            if b not in W and b.r.get(key, 0) < val:
                b.r[key] = val
        return tok

    def op(self, eng, fn, R=(), W=()):
        self.cnt[eng] += 1
        return self._emit(eng, fn, R, W, ("c", eng), 1, self.cnt[eng])

    def dma(self, q, fn, R=(), W=()):
        j = self.dq[q]
        self.dq[q] += 1
        key = ("d", q, j % self.R)
        extra = []
        if j >= self.R:
            extra.append((key, 16 * (j // self.R)))
        return self._emit(q, fn, R, W, key, 16, 16 * (j // self.R + 1), extra)


class _Stop(Exception):
    pass


def build_nc(stop=None, dumps=()):
    nc = bass.Bass("TRN2", target_bir_lowering=False)
    P = Prog()
    dbg = {}

    def dump(name, B, ap, shape, dt=F32):
        if name not in dumps:
            return
        t = nc.dram_tensor("dbg_" + name, list(shape), dt, kind="ExternalOutput").ap()
        P.dma("sp", lambda e: e.dma_start(out=t, in_=ap), [B], ())

    def stop_at(tag):
        if stop == tag:
            raise _Stop()

    def din(name, shape, dt=F32):
        return nc.dram_tensor(name, list(shape), dt, kind="ExternalInput").ap()

    xo = din("xo", [NTOK, D]); xp = din("xp", [NTOK, D]); flagd = din("flag", [128, 1])
    ccold = din("ccol", [128, 16]); adaw = din("ada_w", [D, 6 * D]); adab = din("ada_b", [1, 6 * D])
    vecd = din("vec", [128, NV]); routerbd = din("routerb", [1, E])
    postmixd = din("postmix", [1, D]); postffnd = din("postffn", [1, D])
    aSd = din("aS", [128, 96]); aPd = din("aP", [128, 3072])
    bexpd = din("bexp", [128, 2048]); cexpd = din("cexp", [128, 4096])
    win = din("w_in", [D, 7168]); cpw = din("cp_w", [1024, D]); gwa = din("glu_wa", [1024, D])
    gwb = din("glu_wb", [1024, D]); wout = din("w_out", [D, D]); rwd = din("router_w", [D, E])
    w1g = din("w1g", [E, D, 2048]); w1l = din("w1l", [E, D, 2048]); w2d = din("w2", [E, 2048, D])
    identd = din("ident", [128, 128])
    outd = nc.dram_tensor("out", [NTOK, D], F32, kind="ExternalOutput").ap()
    h2Td = nc.dram_tensor("h2T_scratch", [NBLK, 128, 16 * TB], BF16, kind="Internal").ap()

    es = ExitStack()
    h2dram = Buf(None); outdram = Buf(None)

    def sb(name, shape, dt=F32):
        return Buf(es.enter_context(nc.sbuf_tensor(name, list(shape), dt)))

    ps = [Buf(es.enter_context(nc.psum_tensor(f"ps{i}", [128, 512], F32))) for i in range(8)]
    psi = [0]

    def nps():
        p = ps[psi[0] % 8]
        psi[0] += 1
        return p

    def DMA(q, out, in_, R=(), W=()):
        P.dma(q, lambda e: e.dma_start(out=out, in_=in_), R, W)

    def ACT(out, in_, func, R, W, **kw):
        P.op("act", lambda e: e.activation(out=out, in_=in_, func=func, **kw), R, W)

    def TT(eng, out, a, b, op, R, W):
        P.op(eng, lambda e: e.tensor_tensor(out=out, in0=a, in1=b, op=op), R, W)

    def TS(eng, out, a, s1, s2, op0, op1, R, W):
        if op1 is None:
            P.op(eng, lambda e: e.tensor_scalar(out=out, in0=a, scalar1=s1, scalar2=None, op0=op0), R, W)
        else:
            P.op(eng, lambda e: e.tensor_scalar(out=out, in0=a, scalar1=s1, scalar2=s2, op0=op0, op1=op1), R, W)

    def STT(out, a, s, b, op0, op1, R, W):
        P.op("dve", lambda e: e.scalar_tensor_tensor(out=out, in0=a, scalar=s, in1=b, op0=op0, op1=op1), R, W)

    def MM(out, l, r, st, sp_, R, W):
        P.op("pe", lambda e: e.matmul(out, l, r, start=st, stop=sp_), R, W)

    def TR(out, in_, ident, R, W):
        P.op("pe", lambda e: e.transpose(out, in_, ident), R, W)

    def CP(eng, out, in_, R, W):
        if eng == "act":
            P.op(eng, lambda e: e.activation(out=out, in_=in_, func=AF.Identity), R, W)
        else:
            P.op(eng, lambda e: e.tensor_copy(out=out, in_=in_), R, W)

    def RECIP(out, in_, R, W):
        P.op("dve", lambda e: e.reciprocal(out=out, in_=in_), R, W)

    def MEMSET(eng, ap, c, W):
        P.op(eng, lambda e: e.memset(ap, c), (), W)

    identf = sb("identf", [128, 128]); identb = sb("identb", [128, 128], BF16)
    onesf = sb("onesf", [128, 128]); onesm = sb("onesm", [128, 128])
    vec = sb("vecs", [128, NV]); flag = sb("flagt", [128, 1])
    ccol = sb("ccolt", [128, 16]); silub = sb("silub", [128, 16], BF16)
    adacol = sb("adacol", [128, 96]); scale1 = sb("scale1", [128, 16]); scale2 = sb("scale2", [128, 16])
    pg1 = sb("pg1", [128, D]); pg2 = sb("pg2", [128, D])
    rbb = sb("rbb", [128, E]); rwf = sb("rwf", [128, 16 * E])
    wblk = [sb(f"wblk{i}", [128, 16, 512], BF16) for i in range(2)]
    wbi = [0]

    def nwb():
        w = wblk[wbi[0] % 2]
        wbi[0] += 1
        return w

    xin = [sb("xin0", [128, D])]
    ssq = sb("ssq", [128, 4]); small = sb("small", [128, 16])
    Mt = sb("Mt", [128, 2, 2, 32]); Rt = sb("Rt", [128, 2, 32]); AA = sb("AA", [128, 2, 2, 32])
    lg = sb("lg", [128, E]); mx8 = sb("mx8", [128, 8]); gsm = sb("gsm", [128, E]); gmask = sb("gmask", [128, E])
    GT = sb("GT", [32, NTOK])
    BbTm = [[sb(f"BbTm{i}{j}", [128, 8, 128], BF16) for j in range(2)] for i in range(2)]
    CT = [sb(f"CT{i}", [128, 32, 64], BF16) for i in range(2)]
    aS = sb("aSt", [128, 96]); stmp = sb("stmp", [128, 12 * 32])
    bars = {k: sb(f"bar_{k}", [128, 4]) for k in ("dve", "act", "pool")}
    AW = 19456
    arena = es.enter_context(nc.sbuf_tensor("arena", [128, AW], F32))
    apos = [0]
    abufs = []

    def carve(shape, dt=F32):
        n = 1
        for d_ in shape[1:]:
            n *= d_
        words = n if dt == F32 else (n + 1) // 2
        o = apos[0]
        apos[0] += words
        assert apos[0] <= AW, ("arena overflow", apos[0])
        v = arena[:, o:o + words]
        if dt != F32:
            v = v.bitcast(dt)
        if len(shape) == 3:
            v = v.rearrange("p (a b) -> p a b", a=shape[1])
        elif len(shape) == 4:
            v = v.rearrange("p (a b c) -> p a b c", a=shape[1], b=shape[2])
        b = Buf(v)
        abufs.append(b)
        return b

    def barrier():
        W0 = list(abufs)
        P.op("dve", lambda e: e.memset(bars["dve"][:], 0.0), (), [bars["dve"]] + W0)
        P.op("pool", lambda e: e.memset(bars["pool"][:], 0.0), (), [bars["pool"]])
        P.op("act", lambda e: e.activation(out=bars["act"][:], in_=flag[:, 0:1].broadcast_to([128, 4]), func=AF.Identity), [flag], [bars["act"]])
        pb_ = nps()
        MM(pb_[0:1, 0:2], onesf[0:1, 0:1], onesf[0:1, 0:2], True, True, [onesf], [pb_])
        allb = [bars["dve"], bars["pool"], bars["act"], pb_]
        P.op("dve", lambda e: e.memset(small[:, 8:9], 0.0), allb, [small])
        P.op("pool", lambda e: e.memset(small[:, 9:10], 0.0), allb + [small], [small])
        P.op("act", lambda e: e.activation(out=small[:, 10:11], in_=flag[:, 0:1], func=AF.Identity), allb + [small], [small])
        pb2 = nps()
        MM(pb2[0:1, 0:2], onesf[0:1, 0:1], onesf[0:1, 0:2], True, True, [onesf, small] + allb, [pb2])
        DMA("sp", flag[:], flagd, R=[small, pb2], W=[flag])
        DMA("pool", ssq[:, 3:4], flagd, R=[small, pb2], W=[ssq])
        del abufs[:]
        apos[0] = 0

    rowb = carve([1 + 127, 512]); adabk = carve([128, 512])
    fT0 = carve([128, 8, 1024]); merged0 = carve([128, 3072]); ys0 = carve([128, 2048]); cv0 = carve([128, 4096])
    BbT = [carve([128, 8, 128], BF16) for i in range(2)]

    def V(off, n=1):
        return vec[:, off:off + n]

    def body():
        DMA("sp", identf[:], identd, W=[identf])
        DMA("sp", vec[:], vecd, W=[vec])
        DMA("sp", flag[:], flagd, W=[flag])
        DMA("sp", ccol[:], ccold, W=[ccol])
        DMA("sp", rwf[:].rearrange("p (k n) -> p k n", k=16), rwd.rearrange("(k p) n -> p k n", p=128), W=[rwf])
        DMA("sp", rbb[:], routerbd.broadcast_to([128, E]), W=[rbb])
        DMA("sp", pg1[:], postmixd.broadcast_to([128, D]), W=[pg1])
        DMA("sp", pg2[:], postffnd.broadcast_to([128, D]), W=[pg2])
        DMA("sp", aS[:], aSd, W=[aS])
        CP("dve", identb[:], identf[:], [identf], [identb])
        MEMSET("dve", onesf[:], 1.0, [onesf])
        MEMSET("dve", onesm[:], 1.0 / 1024.0, [onesm])
        ACT(silub[:], ccol[:], AF.Silu, [ccol], [silub])

        for cb in range(24):
            wb = nwb()
            DMA("pool", wb[:], adaw[:, cb * 512:(cb + 1) * 512].rearrange("(k p) n -> p k n", p=128), W=[wb])
            DMA("sp", adabk[0:1, :], adab[0:1, cb * 512:(cb + 1) * 512], W=[adabk])
            p = nps()
            for k in range(16):
                MM(p[0:1, :], silub[:, k:k + 1], wb[:, k, :], k == 0, k == 15, [silub, wb], [p])
            TT("dve", rowb[0:1, :], p[0:1, :], adabk[0:1, :], ALU.add, [p, adabk], [rowb])
            p2 = nps()
            for j in range(4):
                MM(p2[:, j:j + 1], rowb[0:1, j * 128:(j + 1) * 128], onesf[0:1, 0:1], True, True, [rowb, onesf], [p2])
            CP("act", adacol[:, cb * 4:cb * 4 + 4], p2[:, 0:4], [p2], [adacol])
            if 8 <= cb < 12 or 20 <= cb < 24:
                pgx, c0 = (pg1, (cb - 8) * 512) if cb < 12 else (pg2, (cb - 20) * 512)
                p3 = nps()
                MM(p3[:, :], onesf[0:1, :], rowb[0:1, :], True, True, [rowb, onesf], [p3])
                TT("dve", pgx[:, c0:c0 + 512], pgx[:, c0:c0 + 512], p3[:, :], ALU.mult, [pgx, p3], [pgx])
        TS("dve", small[:, 0:16], adacol[:, 16:32], 1.0, None, ALU.add, None, [adacol], [small])
        TT("dve", scale1[:], small[:, 0:16], V(V_PREMIX, 16), ALU.mult, [small, vec], [scale1])
        TS("dve", small[:, 0:16], adacol[:, 64:80], 1.0, None, ALU.add, None, [adacol], [small])
        TT("dve", scale2[:], small[:, 0:16], V(V_PREFFN, 16), ALU.mult, [small, vec], [scale2])
        sh1 = lambda k: adacol[:, k:k + 1]
        sh2 = lambda k: adacol[:, 48 + k:49 + k]

        def abar(aR, aI, lD, t, Bs, B):
            dt_, x_, rho, th, c_, s_, t1, t2 = t
            ACT(dt_, lD, AF.Exp, Bs, [B])
            TT("dve", x_, aR, dt_, ALU.mult, Bs, [B])
            ACT(rho, x_, AF.Exp, [B], [B])
            TT("dve", th, aI, dt_, ALU.mult, Bs, [B])
            ACT(s_, th, AF.Sin, [B], [B], scale=1.0 / 32.0)
            TS("dve", x_, th, 1.0 / 32.0, math.pi / 2, ALU.mult, ALU.add, [B], [B])
            ACT(c_, x_, AF.Sin, [B], [B])
            for _ in range(5):
                TT("dve", t1, c_, c_, ALU.mult, [B], [B])
                TT("dve", t2, s_, s_, ALU.mult, [B], [B])
                TT("dve", x_, c_, s_, ALU.mult, [B], [B])
                TT("dve", c_, t1, t2, ALU.subtract, [B], [B])
                TS("dve", s_, x_, 2.0, None, ALU.mult, None, [B], [B])
            TT("dve", dt_, rho, c_, ALU.mult, [B], [B])
            TT("dve", x_, rho, s_, ALU.mult, [B], [B])
            return dt_, x_

        st = [stmp[:, i * 32:(i + 1) * 32] for i in range(8)]
        abr, abi = abar(aS[:, 0:32], aS[:, 32:64], aS[:, 64:96], st, [stmp, aS], stmp)
        CP("dve", AA[:, 0, 0, :], abr, [stmp], [AA])
        CP("dve", AA[:, 1, 1, :], abr, [stmp], [AA])
        CP("dve", AA[:, 1, 0, :], abi, [stmp], [AA])
        TS("dve", AA[:, 0, 1, :], abi, -1.0, None, ALU.mult, None, [stmp], [AA])
        pt = [fT0[:, i, :] for i in range(8)]
        DMA("sp", merged0[:], aPd, W=[merged0])
        DMA("sp", ys0[:], bexpd, W=[ys0])
        DMA("sp", cv0[:], cexpd, W=[cv0])
        aRp, aIp, lDp = merged0[:, 0:1024], merged0[:, 1024:2048], merged0[:, 2048:3072]
        abrP, abiP = abar(aRp, aIp, lDp, pt, [fT0, merged0], fT0)
        _, _, q2, q3, q4, q5, q6, q7 = pt
        Bs = [fT0, merged0]
        TT("dve", q2, aRp, aRp, ALU.mult, Bs, [fT0])
        TT("dve", q3, aIp, aIp, ALU.mult, Bs, [fT0])
        TT("dve", q2, q2, q3, ALU.add, [fT0], [fT0])
        RECIP(q2, q2, [fT0], [fT0])
        TS("dve", q3, abrP, -1.0, None, ALU.add, None, [fT0], [fT0])
        TT("dve", q4, q3, aRp, ALU.mult, Bs, [fT0])
        TT("dve", q5, abiP, aIp, ALU.mult, Bs, [fT0])
        TT("dve", q4, q4, q5, ALU.add, [fT0], [fT0])
        TT("dve", q4, q4, q2, ALU.mult, [fT0], [fT0])
        TT("dve", q5, abiP, aRp, ALU.mult, Bs, [fT0])
        TT("dve", q6, q3, aIp, ALU.mult, Bs, [fT0])
        TT("dve", q5, q5, q6, ALU.subtract, [fT0], [fT0])
        TT("dve", q5, q5, q2, ALU.mult, [fT0], [fT0])
        bre, bim = ys0[:, 0:1024], ys0[:, 1024:2048]
        Bs2 = [fT0, ys0]
        TT("dve", q6, q4, bre, ALU.mult, Bs2, [fT0])
        TT("dve", q7, q5, bim, ALU.mult, Bs2, [fT0])
        TT("dve", BbT[0][:].rearrange("p k n -> p (k n)"), q6, q7, ALU.subtract, [fT0], [BbT[0]])
        TT("dve", q6, q4, bim, ALU.mult, Bs2, [fT0])
        TT("dve", q7, q5, bre, ALU.mult, Bs2, [fT0])
        TT("dve", BbT[1][:].rearrange("p k n -> p (k n)"), q6, q7, ALU.add, [fT0], [BbT[1]])
        CP("dve", CT[0][:].rearrange("p k n -> p (k n)"), cv0[:, 0:2048], [cv0], [CT[0]])
        TS("dve", CT[1][:].rearrange("p k n -> p (k n)"), cv0[:, 2048:4096], -1.0, None, ALU.mult, None, [cv0], [CT[1]])
        for ri in range(2):
            for j in range(2):
                TS("dve", BbTm[ri][j][:], BbT[ri][:], V(V_M0 + j), None, ALU.mult, None, [BbT[ri], vec], [BbTm[ri][j]])

        dump("adacol", adacol, adacol[:], [128, 96]); dump("AA", AA, AA[:], [128, 2, 2, 32]); dump("pg1", pg1, pg1[:], [128, D])
        dump("BbT0", BbT[0], BbT[0][:], [128, 8, 128], BF16); dump("BbT1", BbT[1], BbT[1][:], [128, 8, 128], BF16)
        dump("CT0", CT[0], CT[0][:], [128, 32, 64], BF16); dump("scale1", scale1, scale1[:], [128, 16])
        stop_at("p0")
        barrier()
        xn = carve([128, D], BF16); hT = carve([128, 16, TB], BF16)
        vT = carve([128, 8, 30 + TB]); cv = carve([128, 8, TB])
        sqt = [carve([128, TB]) for i in range(2)]
        meansb = carve([128, TB]); rstdsb = carve([128, TB]); tmpa = [carve([128, TB]) for i in range(3)]
        vn = carve([128, 8, TB], BF16); uT = carve([128, 8, TB], BF16)
        ys = carve([128, 8, TB]); gy = carve([128, 8, TB], BF16)
        bu = carve([128, 2, 32, SBK]); sS = [carve([128, 2, 32, SBK]) for i in range(2)]
        sbf = carve([128, 2, 32, SBK], BF16)
        merged = carve([128, 16, TB], BF16)
        msb = carve([128, D]); h2Tf = carve([128, 16, 128]); h2Tb = carve([128, 16, TB], BF16)
        xt0 = xin[0]

        def rms_rstd(src_ap, srcB, col):
            ACT(xn[:], src_ap, AF.Square, [srcB], [xn, ssq], accum_out=ssq[:, col:col + 1])
            TS("dve", ssq[:, col:col + 1], ssq[:, col:col + 1], 1.0 / D, EPS, ALU.mult, ALU.add, [ssq], [ssq])
            ACT(ssq[:, col:col + 1], ssq[:, col:col + 1], AF.Sqrt, [ssq], [ssq])
            RECIP(ssq[:, col:col + 1], ssq[:, col:col + 1], [ssq], [ssq])

        def norm_T_bf(src_ap, srcB, scale_t, shf, dst):
            rms_rstd(src_ap, srcB, 0)
            ACT(xn[:], src_ap, AF.Identity, [srcB, ssq], [xn], scale=ssq[:, 0:1])
            for k4 in range(4):
                p = nps()
                pb = p[:].bitcast(BF16)
                for kk in range(4):
                    k = k4 * 4 + kk
                    TR(pb[:, kk * 128:(kk + 1) * 128], xn[:, k * 128:(k + 1) * 128], identb[:], [xn, identb], [p])
                for kk in range(4):
                    k = k4 * 4 + kk
                    ACT(dst[:, k, :], pb[:, kk * 128:(kk + 1) * 128], AF.Identity,
                        [p, scale_t, adacol], [dst], scale=scale_t[:, k:k + 1], bias=shf(k))

        def proj(pB_, pout, wb, c0, rhsB, rhs_fn, nk):
            for k in range(nk):
                MM(pout, wb[:, k, c0:c0 + 128], rhs_fn(k), k == 0, k == nk - 1, [wb, rhsB], [pB_])

        def wload(src_ap, nk, ncol=512):
            wb = nwb()
            DMA("pool", wb[:, 0:nk, 0:ncol], src_ap.rearrange("(k p) n -> p k n", p=128), W=[wb])
            return wb

        MEMSET("dve", sS[1][:], 0.0, [sS[1]])
        MEMSET("dve", vT[:], 0.0, [vT])
        sidx = [0]
        NS = TB // SBK

        def s5_scan_block(with_y):
            for sbk in range(NS):
                t0 = sbk * SBK
                for ri in range(2):
                    pp = [nps(), nps()]
                    pvs = [pp_[:, 0:16 * SBK].rearrange("p (q j n) -> p q j n", q=8, j=2) for pp_ in pp]
                    for kt in range(32):
                        q, ktl = kt // 4, kt % 4
                        h2_, j_ = ktl // 2, ktl % 2
                        MM(pvs[h2_][:, q, j_, :], BbTm[ri][j_][64 * h2_:64 * h2_ + 64, q, :], uT[64 * h2_:64 * h2_ + 64, q, t0:t0 + SBK],
                           True, True, [BbTm[ri][j_], uT], [pp[h2_]])
                    buv = bu[:, ri, :, :].rearrange("p (q k) n -> p q k n", k=4)
                    for h2_ in range(2):
                        CP("act", buv[:, :, 2 * h2_:2 * h2_ + 2, :], pvs[h2_], [pp[h2_]], [bu])
                cur = sS[sidx[0] % 2]
                prv = sS[(sidx[0] + 1) % 2]
                sidx[0] += 1
                for t in range(SBK):
                    if t == 0:
                        pa, pBuf = prv[:, :, :, SBK - 1], prv
                    else:
                        pa, pBuf = cur[:, :, :, t - 1], cur
                    TT("dve", Mt[:], AA[:], pa.unsqueeze(1).broadcast_to([128, 2, 2, 32]), ALU.mult, [AA, pBuf], [Mt])
                    TT("dve", Rt[:], Mt[:, :, 0, :], Mt[:, :, 1, :], ALU.add, [Mt], [Rt])
                    TT("dve", cur[:, :, :, t], Rt[:], bu[:, :, :, t], ALU.add, [Rt, bu], [cur])
                if with_y:
                    CP("act", sbf[:], cur[:], [cur], [sbf])
                    p = nps()
                    pv = p[:, 0:8 * SBK].rearrange("p (a n) -> p a n", a=8)
                    for kt in range(32):
                        q, ktl = kt // 4, kt % 4
                        h2_, j_ = ktl // 2, ktl % 2
                        MM(pv[64 * h2_:64 * h2_ + 64, q, :], CT[0][:, kt, :], sbf[:, 0, kt, :], j_ == 0, False, [CT[0], sbf], [p])
                        MM(pv[64 * h2_:64 * h2_ + 64, q, :], CT[1][:, kt, :], sbf[:, 1, kt, :], False, j_ == 1, [CT[1], sbf], [p])
                    for q in range(8):
                        STT(ys[:, q, t0:t0 + SBK], uT[:, q, t0:t0 + SBK], V(V_S5D + q), pv[:, q, :], ALU.mult, ALU.add,
                            [uT, vec, p], [ys])

        def u_proj():
            for half in range(2):
                wb = wload(win[:, 2048 + half * 512:2048 + (half + 1) * 512], 16)
                for jj in range(4):
                    p = nps()
                    proj(p, p[:, 0:TB], wb, jj * 128, hT, lambda k: hT[:, k, :], 16)
                    CP("act", uT[:, half * 4 + jj, :], p[:, 0:TB], [p], [uT])

        def conv_glu_proj():
            for half in range(2):
                wbv = wload(win[:, half * 512:(half + 1) * 512], 16)
                wbg = wload(win[:, 1024 + half * 512:1024 + (half + 1) * 512], 16)
                for jj in range(4):
                    j = half * 4 + jj
                    pv_ = nps()
                    proj(pv_, pv_[:, 0:TB], wbv, jj * 128, hT, lambda k: hT[:, k, :], 16)
                    pg_ = nps()
                    proj(pg_, pg_[:, 0:TB], wbg, jj * 128, hT, lambda k: hT[:, k, :], 16)
                    sg = sqt[j % 2]
                    ACT(sg[:], pg_[:, 0:TB], AF.Sigmoid, [pg_], [sg])
                    TT("dve", vT[:, j, 30:30 + TB], pv_[:, 0:TB], sg[:], ALU.mult, [pv_, sg], [vT])

        for bi in range(NBLK):
            r0 = bi * TB
            DMA("sp", xt0[:], xp[r0:r0 + 128, :], W=[xt0])
            if bi == 0:
                stop_at("bar")
            norm_T_bf(xt0[:], xt0, scale1, sh1, hT)
            if bi == 0:
                stop_at("n0")
            u_proj()
            if bi == 0:
                stop_at("u0")
            s5_scan_block(False)
            if bi == 0:
                stop_at("s0")
            if bi == NBLK - 1:
                conv_glu_proj()
        last = sS[(sidx[0] + 1) % 2]
        TS("dve", last[:, :, :, SBK - 1], last[:, :, :, SBK - 1], flag[:, 0:1], None, ALU.mult, None, [last, flag], [last])
        TS("dve", vT[:, :, 0:30], vT[:, :, TB:TB + 30], flag[:, 0:1], None, ALU.mult, None, [vT, flag], [vT])

        dump("last", last, last[:], [128, 2, 32, SBK]); dump("halo", vT, vT[:], [128, 8, 30 + TB])
        stop_at("p1a")
        for bi in range(NBLK):
            r0 = bi * TB
            DMA("sp", xt0[:], xo[r0:r0 + 128, :], W=[xt0])
            norm_T_bf(xt0[:], xt0, scale1, sh1, hT)
            if bi == 0:
                dump("hT", hT, hT[:], [128, 16, TB], BF16)
            conv_glu_proj()
            pmean = nps(); pmsq = nps()
            for j in range(8):
                acc = cv[:, j, :]
                TS("dve", acc, vT[:, j, 0:TB], V(V_DWW + j * 31), V(V_DWB + j), ALU.mult, ALU.add, [vT, vec], [cv])
                for k in range(1, 31):
                    STT(acc, vT[:, j, k:k + TB], V(V_DWW + j * 31 + k), acc, ALU.mult, ALU.add, [vT, vec, cv], [cv])
                sq = sqt[j % 2]
                ACT(sq[:], acc, AF.Square, [cv], [sq])
                MM(pmean[:, 0:TB], onesm[:], acc, j == 0, j == 7, [onesm, cv], [pmean])
                MM(pmsq[:, 0:TB], onesm[:], sq[:], j == 0, j == 7, [onesm, sq], [pmsq])
            CP("act", meansb[:], pmean[:, 0:TB], [pmean], [meansb])
            TT("dve", rstdsb[:], meansb[:], meansb[:], ALU.mult, [meansb], [rstdsb])
            TT("dve", rstdsb[:], pmsq[:, 0:TB], rstdsb[:], ALU.subtract, [pmsq, rstdsb], [rstdsb])
            TS("dve", rstdsb[:], rstdsb[:], EPS, None, ALU.add, None, [rstdsb], [rstdsb])
            ACT(rstdsb[:], rstdsb[:], AF.Sqrt, [rstdsb], [rstdsb])
            RECIP(rstdsb[:], rstdsb[:], [rstdsb], [rstdsb])
            for j in range(8):
                t_ = tmpa[j % 2]
                TT("dve", t_[:], cv[:, j, :], meansb[:], ALU.subtract, [cv, meansb], [t_])
                TT("dve", t_[:], t_[:], rstdsb[:], ALU.mult, [t_, rstdsb], [t_])
                ACT(vn[:, j, :], t_[:], AF.Silu, [t_, vec], [vn], scale=V(V_CLNG + j), bias=V(V_CLNB + j))
            if bi == 0:
                dump("vT", vT, vT[:], [128, 8, 30 + TB]); dump("cv", cv, cv[:], [128, 8, TB]); dump("vn", vn, vn[:], [128, 8, TB], BF16)
            CP("dve", vT[:, :, 0:30], vT[:, :, TB:TB + 30], [vT], [vT])
            for db in range(4):
                wb = wload(cpw[:, db * 512:(db + 1) * 512], 8)
                wg = wload(win[:, 3072 + db * 512:3072 + (db + 1) * 512], 16)
                for dd in range(4):
                    d = db * 4 + dd
                    py = nps()
                    proj(py, py[:, 0:TB], wb, dd * 128, vn, lambda k: vn[:, k, :], 8)
                    pgt = nps()
                    proj(pgt, pgt[:, 0:TB], wg, dd * 128, hT, lambda k: hT[:, k, :], 16)
                    sg = sqt[d % 2]
                    ACT(sg[:], pgt[:, 0:TB], AF.Sigmoid, [pgt, vec], [sg], bias=V(V_GATEB + d))
                    STT(merged[:, d, :], py[:, 0:TB], V(V_CPB + d), sg[:], ALU.add, ALU.mult, [py, vec, sg], [merged])
            if bi == 0:
                dump("mergedc", merged, merged[:], [128, 16, TB], BF16)
            u_proj()
            s5_scan_block(True)
            for q in range(8):
                t_ = tmpa[q % 2]
                ACT(t_[:], ys[:, q, :], AF.Square, [ys], [t_])
                TS("dve", t_[:], t_[:], 0.044715, 1.0, ALU.mult, ALU.add, [t_], [t_])
                TT("dve", t_[:], t_[:], ys[:, q, :], ALU.mult, [t_, ys], [t_])
                ACT(t_[:], t_[:], AF.Sigmoid, [t_], [t_], scale=1.5957691216057308)
                TT("dve", gy[:, q, :], t_[:], ys[:, q, :], ALU.mult, [t_, ys], [gy])
            for db in range(4):
                wab = nwb()
                DMA("pool", wab[:, 0:8, :], gwa[:, db * 512:(db + 1) * 512].rearrange("(k p) n -> p k n", p=128), W=[wab])
                DMA("pool", wab[:, 8:16, :], gwb[:, db * 512:(db + 1) * 512].rearrange("(k p) n -> p k n", p=128), W=[wab])
                wg = wload(win[:, 5120 + db * 512:5120 + (db + 1) * 512], 16)
                for dd in range(4):
                    d = db * 4 + dd
                    pa_ = nps()
                    for k in range(8):
                        MM(pa_[:, 0:TB], wab[:, k, dd * 128:(dd + 1) * 128], gy[:, k, :], k == 0, k == 7, [wab, gy], [pa_])
                    pb_ = nps()
                    for k in range(8):
                        MM(pb_[:, 0:TB], wab[:, 8 + k, dd * 128:(dd + 1) * 128], gy[:, k, :], k == 0, k == 7, [wab, gy], [pb_])
                    pg2_ = nps()
                    proj(pg2_, pg2_[:, 0:TB], wg, dd * 128, hT, lambda k: hT[:, k, :], 16)
                    t1 = tmpa[0]; t2 = tmpa[1]; t3 = tmpa[2]
                    ACT(t1[:], pb_[:, 0:TB], AF.Sigmoid, [pb_], [t1])
                    TT("dve", t2[:], pa_[:, 0:TB], t1[:], ALU.mult, [pa_, t1], [t2])
                    ACT(t3[:], pg2_[:, 0:TB], AF.Sigmoid, [pg2_, vec], [t3], bias=V(V_GATEB + 16 + d))
                    TT("dve", t2[:], t2[:], t3[:], ALU.mult, [t2, t3], [t2])
                    TT("dve", merged[:, d, :], merged[:, d, :], t2[:], ALU.add, [merged, t2], [merged])
            if bi == 0:
                dump("uT", uT, uT[:], [128, 8, TB], BF16); dump("ys", ys, ys[:], [128, 8, TB]); dump("gy", gy, gy[:], [128, 8, TB], BF16)
                dump("merged", merged, merged[:], [128, 16, TB], BF16)
            for nb in range(4):
                wo = wload(wout[:, nb * 512:(nb + 1) * 512], 16)
                pm = nps()
                for k in range(16):
                    MM(pm[:, :], merged[:, k, :], wo[:, k, :], k == 0, k == 15, [merged, wo], [pm])
                CP("act", msb[:, nb * 512:(nb + 1) * 512], pm[:, :], [pm], [msb])
            if bi == 0:
                dump("m", msb, msb[:], [128, D])
            rms_rstd(msb[:], msb, 1)
            STT(msb[:], msb[:], ssq[:, 1:2], pg1[:], ALU.mult, ALU.mult, [msb, ssq, pg1], [msb])
            DMA("sp", xt0[:], xo[r0:r0 + 128, :], W=[xt0])
            TT("dve", xt0[:], xt0[:], msb[:], ALU.add, [xt0, msb], [xt0])
            DMA("sp", outd[r0:r0 + 128, :], xt0[:], R=[xt0], W=[outdram])
            rms_rstd(xt0[:], xt0, 2)
            ACT(msb[:], xt0[:], AF.Identity, [xt0, ssq], [msb], scale=ssq[:, 2:3])
            for k4 in range(4):
                p = nps()
                for kk in range(4):
                    k = k4 * 4 + kk
                    TR(p[:, kk * 128:(kk + 1) * 128], msb[:, k * 128:(k + 1) * 128], identf[:], [msb, identf], [p])
                for kk in range(4):
                    k = k4 * 4 + kk
                    ACT(h2Tf[:, k, :], p[:, kk * 128:(kk + 1) * 128], AF.Identity, [p, scale2, adacol], [h2Tf],
                        scale=scale2[:, k:k + 1], bias=sh2(k))
            CP("pool", h2Tb[:], h2Tf[:], [h2Tf], [h2Tb])
            pl = nps()
            for k in range(16):
                MM(pl[:, 0:E], h2Tf[:, k, :], rwf[:, k * E:(k + 1) * E], k == 0, k == 15, [h2Tf, rwf], [pl])
            TT("dve", lg[:], pl[:, 0:E], rbb[:], ALU.add, [pl, rbb], [lg])
            P.op("dve", lambda e: e.max(out=mx8[:], in_=lg[:]), [lg], [mx8])
            TS("dve", gmask[:], lg[:], mx8[:, 3:4], None, ALU.is_ge, None, [lg, mx8], [gmask])
            TS("dve", small[:, 0:1], mx8[:, 0:1], -1.0, None, ALU.mult, None, [mx8], [small])
            ACT(gsm[:], lg[:], AF.Exp, [lg, small], [gsm], bias=small[:, 0:1])
            TT("dve", gsm[:], gsm[:], gmask[:], ALU.mult, [gsm, gmask], [gsm])
            P.op("dve", lambda e: e.tensor_reduce(out=small[:, 1:2], in_=gsm[:], axis=AX.X, op=ALU.add), [gsm], [small])
            RECIP(small[:, 1:2], small[:, 1:2], [small], [small])
            TS("dve", gsm[:], gsm[:], small[:, 1:2], None, ALU.mult, None, [gsm, small], [gsm])
            pt_ = nps()
            TR(pt_[0:E, 0:128], gsm[:], identf[:], [gsm, identf], [pt_])
            CP("act", GT[:, r0:r0 + 128], pt_[0:E, 0:128], [pt_], [GT])
            if bi == 0:
                dump("h2Tb", h2Tb, h2Tb[:], [128, 16, TB], BF16); dump("lg", lg, lg[:], [128, E]); dump("gsm", gsm, gsm[:], [128, E])
            DMA("sp", h2Td[bi], h2Tb[:].rearrange("p k n -> p (k n)"), R=[h2Tb], W=[h2dram])
            if bi == 0:
                stop_at("b0")

        def actb_alias(v):
            b = Buf(v)
            abufs.append(b)
            return b

        stop_at("p1b")
        barrier()
        fT = carve([128, 16, MTB]); actb = carve([128, 16, MTB], BF16); h2T2 = carve([128, 16, MTB], BF16)
        gbc = carve([128, MTB]); gte = carve([128, MTB])
        gtt = [carve([128, MTB]) for i in range(2)]; ltt = [carve([128, MTB])]; sgt = [carve([128, MTB])]
        actf = actb[:].rearrange("p a b -> p (a b)").bitcast(F32)
        ftk = actb_alias(actf[:, 0:D]); ftk_junk = actf[:, D:2 * D]
        for tb in range(NTOK // MTB):
            for hb in range(MTB // TB):
                DMA("sp", h2T2[:, :, hb * TB:(hb + 1) * TB], h2Td[tb * (MTB // TB) + hb].rearrange("p (k n) -> p k n", k=16),
                    R=[h2dram], W=[h2T2])
            MEMSET("pool", fT[:], 0.0, [fT])
            for e in range(E):
                TS("pool", gte[0:32, :], GT[:, tb * MTB:(tb + 1) * MTB], identf[0:32, e:e + 1], None, ALU.mult, None, [GT, identf], [gte])
                pgb = nps()
                MM(pgb[:, :], onesf[0:32, :], gte[0:32, :], True, True, [onesf, gte], [pgb])
                CP("act", gbc[:], pgb[:, :], [pgb], [gbc])
                for fb in range(8):
                    wb = nwb()
                    DMA("pool", wb[:, :, 0:256], w1g[e][:, fb * 256:(fb + 1) * 256].rearrange("(k p) n -> p k n", p=128), W=[wb])
                    DMA("pool", wb[:, :, 256:512], w1l[e][:, fb * 256:(fb + 1) * 256].rearrange("(k p) n -> p k n", p=128), W=[wb])
                    for fc in range(2):
                        f = fb * 2 + fc
                        pg_ = nps()
                        proj(pg_, pg_[:, :], wb, fc * 128, h2T2, lambda k: h2T2[:, k, :], 16)
                        pl_ = nps()
                        proj(pl_, pl_[:, :], wb, 256 + fc * 128, h2T2, lambda k: h2T2[:, k, :], 16)
                        g_ = gtt[f % 2]; l_ = ltt[0]; s_ = sgt[0]
                        TS("dve", g_[:], pg_[:, :], V(V_B1G + e * 16 + f), 7.0, ALU.add, ALU.min, [pg_, vec], [g_])
                        ACT(s_[:], g_[:], AF.Sigmoid, [g_], [s_], scale=1.702)
                        TS("dve", l_[:], pl_[:, :], V(V_B1L + e * 16 + f), 7.0, ALU.add, ALU.min, [pl_, vec], [l_])
                        TS("dve", l_[:], l_[:], -7.0, 1.0, ALU.max, ALU.add, [l_], [l_])
                        TT("dve", g_[:], g_[:], s_[:], ALU.mult, [g_, s_], [g_])
                        TT("dve", actb[:, f, :], g_[:], l_[:], ALU.mult, [g_, l_], [actb])
                for db in range(4):
                    wb = wload(w2d[e][:, db * 512:(db + 1) * 512], 16)
                    for dd in range(4):
                        d = db * 4 + dd
                        py = nps()
                        proj(py, py[:, :], wb, dd * 128, actb, lambda k: actb[:, k, :], 16)
                        t_ = gtt[d % 2]
                        STT(t_[:], py[:, :], V(V_B2 + e * 16 + d), gbc[:], ALU.add, ALU.mult, [py, vec, gbc], [t_])
                        TT("dve", fT[:, d, :], fT[:, d, :], t_[:], ALU.add, [fT, t_], [fT])
            for tt in range(MTB // 128):
                r0 = tb * MTB + tt * 128
                for k4 in range(4):
                    p = nps()
                    for kk in range(4):
                        d = k4 * 4 + kk
                        TR(p[:, kk * 128:(kk + 1) * 128], fT[:, d, tt * 128:(tt + 1) * 128], identf[:], [fT, identf], [p])
                    CP("act", ftk[:, k4 * 512:(k4 + 1) * 512], p[:, :], [p], [ftk, actb])
                ACT(ftk_junk, ftk[:], AF.Square, [ftk], [actb, ssq], accum_out=ssq[:, 3:4])
                TS("dve", ssq[:, 3:4], ssq[:, 3:4], 1.0 / D, EPS, ALU.mult, ALU.add, [ssq], [ssq])
                ACT(ssq[:, 3:4], ssq[:, 3:4], AF.Sqrt, [ssq], [ssq])
                RECIP(ssq[:, 3:4], ssq[:, 3:4], [ssq], [ssq])
                STT(ftk[:], ftk[:], ssq[:, 3:4], pg2[:], ALU.mult, ALU.mult, [ftk, ssq, pg2, actb], [ftk, actb])
                DMA("sp", xt0[:], outd[r0:r0 + 128, :], R=[outdram], W=[xt0])
                TT("dve", xt0[:], xt0[:], ftk[:], ALU.add, [xt0, ftk, actb], [xt0])
                DMA("sp", outd[r0:r0 + 128, :], xt0[:], R=[xt0], W=[outdram])


    try:
        body()
    except _Stop:
        pass

    sems = {}
    for k in ("pe", "act", "dve", "pool", "sp"):
        sems[("c", k)] = es.enter_context(nc.semaphore(f"c_{k}"))
    for q in ("sp", "pool"):
        for i in range(Prog.R):
            sems[("d", q, i)] = es.enter_context(nc.semaphore(f"d_{q}{i}"))

    def run(name, e):
        for waits, fn, key, amt in P.ops[name]:
            for wk, wv in waits:
                e.wait_ge(sems[wk], wv)
            fn(e).then_inc(sems[key], amt)
        if name == "sp":
            n = P.dq["sp"]
            for i in range(Prog.R):
                cnt = (n - i + Prog.R - 1) // Prog.R
                if cnt > 0:
                    e.wait_ge(sems[("d", "sp", i)], 16 * cnt)

    with es:
        with nc.Block() as block:
            @block.tensor
            def _(e):
                run("pe", e)

            @block.scalar
            def _(e):
                run("act", e)

            @block.vector
            def _(e):
                run("dve", e)

            @block.gpsimd
            def _(e):
                run("pool", e)

            @block.sync
            def _(e):
                run("sp", e)
    return nc


def _prep(inputs):
    f32 = np.float32
    g = {k: np.asarray(v) for k, v in inputs.items()}
    x = g["x"]; c = g["c"]
    sh = {}
    sh["ada_w"] = np.ascontiguousarray(g["ada_w"][0]); sh["ada_b"] = np.ascontiguousarray(g["ada_b"][0][None, :])
    def fm(v, n):
        return np.ascontiguousarray(v.reshape(n, 128).T)
    vec = np.zeros((128, NV), f32)
    vec[:, V_PREMIX:V_PREMIX + 16] = fm(g["pre_mix_g"][0], 16)
    vec[:, V_PREFFN:V_PREFFN + 16] = fm(g["pre_ffn_g"][0], 16)
    vec[:, V_GATEB:V_GATEB + 32] = fm(g["gate_b"][0], 32)
    vec[:, V_DWB:V_DWB + 8] = fm(g["dw_b"][0], 8)
    vec[:, V_CLNG:V_CLNG + 8] = fm(g["cln_g"][0], 8)
    vec[:, V_CLNB:V_CLNB + 8] = fm(g["cln_b"][0], 8)
    vec[:, V_CPB:V_CPB + 16] = fm(g["cp_b"][0], 16)
    vec[:, V_S5D:V_S5D + 8] = fm(g["s5_d"][0], 8)
    dww = g["dw_w"][0]
    vec[:, V_DWW:V_DWW + 248] = dww.T.reshape(8, 128, 31).transpose(1, 0, 2).reshape(128, 248)
    b1 = g["b1"][0]
    vec[:, V_B1G:V_B1G + 512] = b1[:, 0::2].reshape(32, 16, 128).transpose(2, 0, 1).reshape(128, 512)
    vec[:, V_B1L:V_B1L + 512] = b1[:, 1::2].reshape(32, 16, 128).transpose(2, 0, 1).reshape(128, 512)
    vec[:, V_B2:V_B2 + 512] = g["b2"][0].reshape(32, 16, 128).transpose(2, 0, 1).reshape(128, 512)
    pidx = (np.arange(128) // 32) % 2
    vec[:, V_M0] = (pidx == 0).astype(f32); vec[:, V_M0 + 1] = (pidx == 1).astype(f32)
    sh["vec"] = vec
    sh["routerb"] = np.ascontiguousarray(g["router_b"][0][None, :])
    sh["postmix"] = np.ascontiguousarray(g["post_mix_g"][0][None, :])
    sh["postffn"] = np.ascontiguousarray(g["post_ffn_g"][0][None, :])
    are, aim, ldt = g["s5_a_re"][0], g["s5_a_im"][0], g["s5_log_dt"][0]
    ldt2 = np.broadcast_to(ldt[:, None], (64, 64))
    def Sl(a):
        return a.reshape(32, 2, 64).transpose(1, 2, 0).reshape(128, 32)
    sh["aS"] = np.ascontiguousarray(np.concatenate([Sl(are), Sl(aim), Sl(ldt2)], axis=1).astype(f32))
    def Pl(a):
        t = a.reshape(8, 4, 2, 64)
        t = t.transpose(1, 0, 2, 3)
        t = np.broadcast_to(t[:, None, None], (4, 2, 16, 8, 2, 64))
        return t.reshape(128, 1024)
    sh["aP"] = np.ascontiguousarray(np.concatenate([Pl(are), Pl(aim), Pl(ldt2)], axis=1).astype(f32))
    def Bexp(b):
        t = b.reshape(8, 4, 2, 64, 16)
        o = np.zeros((4, 2, 16, 8, 2, 64), f32)
        for gl in range(2):
            o[:, gl, :, :, gl, :] = t[:, :, gl].transpose(1, 3, 0, 2)
        return o.reshape(128, 1024)
    sh["bexp"] = np.concatenate([Bexp(g["s5_b_re"][0]), Bexp(g["s5_b_im"][0])], axis=1)
    def Cexp(cc):
        t = cc.reshape(32, 2, 16, 64)
        o = np.zeros((2, 64, 32, 2, 2, 16), f32)
        for kt in range(32):
            for gl in range(2):
                o[gl, :, kt, kt % 2, gl, :] = t[kt, gl].T
        return o.reshape(128, 2048)
    sh["cexp"] = np.concatenate([Cexp(g["s5_c_re"][0]), Cexp(g["s5_c_im"][0])], axis=1)
    sh["w_in"] = np.ascontiguousarray(g["w_in"][0]); sh["cp_w"] = np.ascontiguousarray(g["cp_w"][0])
    sh["glu_wa"] = np.ascontiguousarray(g["glu_wa"][0]); sh["glu_wb"] = np.ascontiguousarray(g["glu_wb"][0])
    sh["w_out"] = np.ascontiguousarray(g["w_out"][0]); sh["router_w"] = np.ascontiguousarray(g["router_w"][0])
    w1 = g["w1"][0]
    sh["w1g"] = np.ascontiguousarray(w1[:, :, 0::2]); sh["w1l"] = np.ascontiguousarray(w1[:, :, 1::2])
    sh["w2"] = np.ascontiguousarray(g["w2"][0])
    sh["ident"] = np.eye(128, dtype=f32)
    maps = []
    for core in range(8):
        b, half = core // 2, core % 2
        m = dict(sh)
        m["xo"] = np.ascontiguousarray(x[b, half * NTOK:(half + 1) * NTOK])
        m["xp"] = np.ascontiguousarray(x[b, 0:NTOK]) if half == 1 else np.zeros((NTOK, D), f32)
        m["flag"] = np.full((128, 1), float(half), f32)
        m["ccol"] = fm(c[b], 16)
        maps.append(m)
    return maps


def kernel(**inputs):
    maps = _prep(inputs)
    nc = build_nc()
    res = run_bass_kernel_spmd(nc, maps, core_ids=list(range(8)))
    out = np.zeros((4, 4096, D), np.float32)
    for core in range(8):
        b, half = core // 2, core % 2
        out[b, half * NTOK:(half + 1) * NTOK] = res.results[core]["out"]
    return out
```

```python
import math
from contextlib import ExitStack
import numpy as np
import concourse.bass as bass
import concourse.mybir as mybir
from concourse.bass_utils import run_bass_kernel_spmd

F32 = mybir.dt.float32
BF16 = mybir.dt.bfloat16
AF = mybir.ActivationFunctionType
ALU = mybir.AluOpType
AX = mybir.AxisListType

D = 2048
NTOK = 2048
TB = 128
SBK = 16
NBLK = NTOK // TB
E = 32
MTB = 512
CAP = 768
SBW = 384
BIGI = 4000000.0
EPS = 1e-6

V_PREMIX, V_PREFFN, V_GATEB, V_DWB, V_CLNG, V_CLNB, V_CPB, V_S5D = 0, 16, 32, 64, 72, 80, 88, 104
V_DWW = 112
V_B1G = V_DWW + 8 * 31
V_B1L = V_B1G + 512
V_B2 = V_B1L + 512
V_M0 = V_B2 + 512
NV = V_M0 + 2


class Buf:
    __slots__ = ("t", "w", "r")

    def __init__(self, t):
        self.t = t
        self.w = None
        self.r = {}

    def __getitem__(self, idx):
        return self.t[idx]


class Prog:
    R = 8

    def __init__(self):
        self.ops = {k: [] for k in ("pe", "act", "dve", "pool", "sp")}
        self.cnt = {k: 0 for k in self.ops}
        self.known = {k: {} for k in self.ops}
        self.dq = {"sp": 0, "pool": 0}

    def _emit(self, eng, fn, R, W, key, amt, val, extra=()):
        need = {}

        def add(tok):
            if tok is None:
                return
            k, v = tok
            if need.get(k, 0) < v:
                need[k] = v

        for b in R:
            add(b.w)
        for b in W:
            add(b.w)
            for k, v in b.r.items():
                add((k, v))
        for t in extra:
            add(t)
        waits = []
        for k, v in need.items():
            if k == ("c", "pe") and eng == "pe":
                continue
            if self.known[eng].get(k, 0) >= v:
                continue
            self.known[eng][k] = v
            waits.append((k, v))
        self.ops[eng].append((waits, fn, key, amt))
        tok = (key, val)
        for b in W:
            b.w = tok
            b.r = {}
        for b in R:
            if b not in W and b.r.get(key, 0) < val:
                b.r[key] = val
        return tok

    def op(self, eng, fn, R=(), W=()):
        self.cnt[eng] += 1
        return self._emit(eng, fn, R, W, ("c", eng), 1, self.cnt[eng])

    def dma(self, q, fn, R=(), W=()):
        j = self.dq[q]
        self.dq[q] += 1
        key = ("d", q, j % self.R)
        extra = []
        if j >= self.R:
            extra.append((key, 16 * (j // self.R)))
        return self._emit(q, fn, R, W, key, 16, 16 * (j // self.R + 1), extra)


class _Stop(Exception):
    pass


def build_nc(stop=None, dumps=()):
    nc = bass.Bass("TRN2", target_bir_lowering=False)
    P = Prog()
    dbg = {}

    def dump(name, B, ap, shape, dt=F32):
        if name not in dumps:
            return
        t = nc.dram_tensor("dbg_" + name, list(shape), dt, kind="ExternalOutput").ap()
        P.dma("sp", lambda e: e.dma_start(out=t, in_=ap), [B], ())

    def stop_at(tag):
        if stop == tag:
            raise _Stop()

    def din(name, shape, dt=F32):
        return nc.dram_tensor(name, list(shape), dt, kind="ExternalInput").ap()

    xo = din("xo", [NTOK, D]); xp = din("xp", [NTOK, D]); flagd = din("flag", [128, 1])
    ccold = din("ccol", [128, 16]); adaw = din("ada_w", [D, 6 * D]); adab = din("ada_b", [1, 6 * D])
    vecd = din("vec", [128, NV]); routerbd = din("routerb", [1, E])
    postmixd = din("postmix", [1, D]); postffnd = din("postffn", [1, D])
    aSd = din("aS", [128, 96]); aPd = din("aP", [128, 3072])
    bexpd = din("bexp", [128, 2048]); cexpd = din("cexp", [128, 4096])
    win = din("w_in", [D, 7168]); cpw = din("cp_w", [1024, D]); gwa = din("glu_wa", [1024, D])
    gwb = din("glu_wb", [1024, D]); wout = din("w_out", [D, D]); rwd = din("router_w", [D, E])
    w1g = din("w1g", [E, D, 2048]); w1l = din("w1l", [E, D, 2048]); w2d = din("w2", [E, 2048, D])
    identd = din("ident", [128, 128]); trid = din("tri", [128, 128]); ecapd = din("ecap", [128, E]); b2d = din("b2s", [E, D])
    outd = nc.dram_tensor("out", [NTOK, D], F32, kind="ExternalOutput").ap()
    Xgd = nc.dram_tensor("xg_scratch", [E * CAP, D], BF16, kind="Internal").ap()
    Ygd = nc.dram_tensor("yg_scratch", [E * CAP, D], F32, kind="Internal").ap()

    es = ExitStack()
    h2dram = Buf(None); outdram = Buf(None); xgdram = Buf(None); ygdram = Buf(None)

    def sb(name, shape, dt=F32):
        return Buf(es.enter_context(nc.sbuf_tensor(name, list(shape), dt)))

    ps = [Buf(es.enter_context(nc.psum_tensor(f"ps{i}", [128, 512], F32))) for i in range(8)]
    psi = [0]

    def nps():
        p = ps[psi[0] % 8]
        psi[0] += 1
        return p

    def DMA(q, out, in_, R=(), W=()):
        P.dma(q, lambda e: e.dma_start(out=out, in_=in_), R, W)

    def ACT(out, in_, func, R, W, **kw):
        P.op("act", lambda e: e.activation(out=out, in_=in_, func=func, **kw), R, W)

    def TT(eng, out, a, b, op, R, W):
        P.op(eng, lambda e: e.tensor_tensor(out=out, in0=a, in1=b, op=op), R, W)

    def TS(eng, out, a, s1, s2, op0, op1, R, W):
        if op1 is None:
            P.op(eng, lambda e: e.tensor_scalar(out=out, in0=a, scalar1=s1, scalar2=None, op0=op0), R, W)
        else:
            P.op(eng, lambda e: e.tensor_scalar(out=out, in0=a, scalar1=s1, scalar2=s2, op0=op0, op1=op1), R, W)

    def STT(out, a, s, b, op0, op1, R, W):
        P.op("dve", lambda e: e.scalar_tensor_tensor(out=out, in0=a, scalar=s, in1=b, op0=op0, op1=op1), R, W)

    def MM(out, l, r, st, sp_, R, W):
        P.op("pe", lambda e: e.matmul(out, l, r, start=st, stop=sp_), R, W)

    def TR(out, in_, ident, R, W):
        P.op("pe", lambda e: e.transpose(out, in_, ident), R, W)

    def CP(eng, out, in_, R, W):
        if eng == "act":
            P.op(eng, lambda e: e.activation(out=out, in_=in_, func=AF.Identity), R, W)
        else:
            P.op(eng, lambda e: e.tensor_copy(out=out, in_=in_), R, W)

    def RECIP(out, in_, R, W):
        P.op("dve", lambda e: e.reciprocal(out=out, in_=in_), R, W)

    def MEMSET(eng, ap, c, W):
        P.op(eng, lambda e: e.memset(ap, c), (), W)

    identf = sb("identf", [128, 128]); identb = sb("identb", [128, 128], BF16)
    onesf = sb("onesf", [128, 128]); onesm = sb("onesm", [128, 128])
    vec = sb("vecs", [128, NV]); flag = sb("flagt", [128, 1])
    ccol = sb("ccolt", [128, 16]); silub = sb("silub", [128, 16], BF16)
    adacol = sb("adacol", [128, 96]); scale1 = sb("scale1", [128, 16]); scale2 = sb("scale2", [128, 16])
    pg1 = sb("pg1", [128, D]); pg2 = sb("pg2", [128, D])
    rbb = sb("rbb", [128, E]); rwf = sb("rwf", [128, 16 * E])
    wblk = [sb(f"wblk{i}", [128, 16, 512], BF16) for i in range(2)]
    wbi = [0]

    def nwb():
        w = wblk[wbi[0] % 2]
        wbi[0] += 1
        return w

    xin = [sb("xin0", [128, D])]
    ssq = sb("ssq", [128, 4]); small = sb("small", [128, 16])
    Mt = sb("Mt", [128, 2, 2, 32]); Rt = sb("Rt", [128, 2, 32]); AA = sb("AA", [128, 2, 2, 32])
    lg = sb("lg", [128, E]); mx8 = sb("mx8", [128, 8]); gsm = sb("gsm", [128, E]); gmask = sb("gmask", [128, E])
    GT = sb("GT", [32, NTOK])
    tri = sb("tri_t", [128, 128]); ecap = sb("ecap_t", [128, E]); basebc = sb("basebc", [128, E]); b2s = sb("b2s_t", [E, D])
    IDXF = sb("IDXF", [128, NBLK, 4]); GK = sb("GK", [128, NBLK, 4]); IDXI = sb("IDXI", [128, NBLK, 4], mybir.dt.int32)
    rt = [sb(f"rt{i}", [128, E]) for i in range(6)]
    BbTm = [[sb(f"BbTm{i}{j}", [128, 8, 128], BF16) for j in range(2)] for i in range(2)]
    CT = [sb(f"CT{i}", [128, 32, 64], BF16) for i in range(2)]
    aS = sb("aSt", [128, 96]); stmp = sb("stmp", [128, 12 * 32])
    bars = {k: sb(f"bar_{k}", [128, 4]) for k in ("dve", "act", "pool")}
    AW = 19456
    arena = es.enter_context(nc.sbuf_tensor("arena", [128, AW], F32))
    apos = [0]
    abufs = []

    def carve(shape, dt=F32):
        n = 1
        for d_ in shape[1:]:
            n *= d_
        words = n if dt == F32 else (n + 1) // 2
        o = apos[0]
        apos[0] += words
        assert apos[0] <= AW, ("arena overflow", apos[0])
        v = arena[:, o:o + words]
        if dt != F32:
            v = v.bitcast(dt)
        if len(shape) == 3:
            v = v.rearrange("p (a b) -> p a b", a=shape[1])
        elif len(shape) == 4:
            v = v.rearrange("p (a b c) -> p a b c", a=shape[1], b=shape[2])
        b = Buf(v)
        abufs.append(b)
        return b

    def barrier():
        W0 = list(abufs)
        P.op("dve", lambda e: e.memset(bars["dve"][:], 0.0), (), [bars["dve"]] + W0)
        P.op("pool", lambda e: e.memset(bars["pool"][:], 0.0), (), [bars["pool"]])
        P.op("act", lambda e: e.activation(out=bars["act"][:], in_=flag[:, 0:1].broadcast_to([128, 4]), func=AF.Identity), [flag], [bars["act"]])
        pb_ = nps()
        MM(pb_[0:1, 0:2], onesf[0:1, 0:1], onesf[0:1, 0:2], True, True, [onesf], [pb_])
        allb = [bars["dve"], bars["pool"], bars["act"], pb_]
        P.op("dve", lambda e: e.memset(small[:, 8:9], 0.0), allb, [small])
        P.op("pool", lambda e: e.memset(small[:, 9:10], 0.0), allb + [small], [small])
        P.op("act", lambda e: e.activation(out=small[:, 10:11], in_=flag[:, 0:1], func=AF.Identity), allb + [small], [small])
        pb2 = nps()
        MM(pb2[0:1, 0:2], onesf[0:1, 0:1], onesf[0:1, 0:2], True, True, [onesf, small] + allb, [pb2])
        DMA("sp", flag[:], flagd, R=[small, pb2], W=[flag])
        DMA("pool", ssq[:, 3:4], flagd, R=[small, pb2], W=[ssq])
        del abufs[:]
        apos[0] = 0

    rowb = carve([1 + 127, 512]); adabk = carve([128, 512])
    fT0 = carve([128, 8, 1024]); merged0 = carve([128, 3072]); ys0 = carve([128, 2048]); cv0 = carve([128, 4096])
    BbT = [carve([128, 8, 128], BF16) for i in range(2)]

    def V(off, n=1):
        return vec[:, off:off + n]

    def body():
        DMA("sp", identf[:], identd, W=[identf])
        DMA("sp", vec[:], vecd, W=[vec])
        DMA("sp", flag[:], flagd, W=[flag])
        DMA("sp", ccol[:], ccold, W=[ccol])
        DMA("sp", rwf[:].rearrange("p (k n) -> p k n", k=16), rwd.rearrange("(k p) n -> p k n", p=128), W=[rwf])
        DMA("sp", rbb[:], routerbd.broadcast_to([128, E]), W=[rbb])
        DMA("sp", pg1[:], postmixd.broadcast_to([128, D]), W=[pg1])
        DMA("sp", pg2[:], postffnd.broadcast_to([128, D]), W=[pg2])
        DMA("sp", aS[:], aSd, W=[aS])
        DMA("sp", tri[:], trid, W=[tri])
        DMA("sp", ecap[:], ecapd, W=[ecap])
        DMA("sp", b2s[:], b2d, W=[b2s])
        MEMSET("dve", basebc[:], 0.0, [basebc])
        CP("dve", identb[:], identf[:], [identf], [identb])
        MEMSET("dve", onesf[:], 1.0, [onesf])
        MEMSET("dve", onesm[:], 1.0 / 1024.0, [onesm])
        ACT(silub[:], ccol[:], AF.Silu, [ccol], [silub])

        for cb in range(24):
            wb = nwb()
            DMA("pool", wb[:], adaw[:, cb * 512:(cb + 1) * 512].rearrange("(k p) n -> p k n", p=128), W=[wb])
            DMA("sp", adabk[0:1, :], adab[0:1, cb * 512:(cb + 1) * 512], W=[adabk])
            p = nps()
            for k in range(16):
                MM(p[0:1, :], silub[:, k:k + 1], wb[:, k, :], k == 0, k == 15, [silub, wb], [p])
            TT("dve", rowb[0:1, :], p[0:1, :], adabk[0:1, :], ALU.add, [p, adabk], [rowb])
            p2 = nps()
            for j in range(4):
                MM(p2[:, j:j + 1], rowb[0:1, j * 128:(j + 1) * 128], onesf[0:1, 0:1], True, True, [rowb, onesf], [p2])
            CP("act", adacol[:, cb * 4:cb * 4 + 4], p2[:, 0:4], [p2], [adacol])
            if 8 <= cb < 12 or 20 <= cb < 24:
                pgx, c0 = (pg1, (cb - 8) * 512) if cb < 12 else (pg2, (cb - 20) * 512)
                p3 = nps()
                MM(p3[:, :], onesf[0:1, :], rowb[0:1, :], True, True, [rowb, onesf], [p3])
                TT("dve", pgx[:, c0:c0 + 512], pgx[:, c0:c0 + 512], p3[:, :], ALU.mult, [pgx, p3], [pgx])
        TS("dve", small[:, 0:16], adacol[:, 16:32], 1.0, None, ALU.add, None, [adacol], [small])
        TT("dve", scale1[:], small[:, 0:16], V(V_PREMIX, 16), ALU.mult, [small, vec], [scale1])
        TS("dve", small[:, 0:16], adacol[:, 64:80], 1.0, None, ALU.add, None, [adacol], [small])
        TT("dve", scale2[:], small[:, 0:16], V(V_PREFFN, 16), ALU.mult, [small, vec], [scale2])
        sh1 = lambda k: adacol[:, k:k + 1]
        sh2 = lambda k: adacol[:, 48 + k:49 + k]

        def abar(aR, aI, lD, t, Bs, B):
            dt_, x_, rho, th, c_, s_, t1, t2 = t
            ACT(dt_, lD, AF.Exp, Bs, [B])
            TT("dve", x_, aR, dt_, ALU.mult, Bs, [B])
            ACT(rho, x_, AF.Exp, [B], [B])
            TT("dve", th, aI, dt_, ALU.mult, Bs, [B])
            ACT(s_, th, AF.Sin, [B], [B], scale=1.0 / 32.0)
            TS("dve", x_, th, 1.0 / 32.0, math.pi / 2, ALU.mult, ALU.add, [B], [B])
            ACT(c_, x_, AF.Sin, [B], [B])
            for _ in range(5):
                TT("dve", t1, c_, c_, ALU.mult, [B], [B])
                TT("dve", t2, s_, s_, ALU.mult, [B], [B])
                TT("dve", x_, c_, s_, ALU.mult, [B], [B])
                TT("dve", c_, t1, t2, ALU.subtract, [B], [B])
                TS("dve", s_, x_, 2.0, None, ALU.mult, None, [B], [B])
            TT("dve", dt_, rho, c_, ALU.mult, [B], [B])
            TT("dve", x_, rho, s_, ALU.mult, [B], [B])
            return dt_, x_

        st = [stmp[:, i * 32:(i + 1) * 32] for i in range(8)]
        abr, abi = abar(aS[:, 0:32], aS[:, 32:64], aS[:, 64:96], st, [stmp, aS], stmp)
        CP("dve", AA[:, 0, 0, :], abr, [stmp], [AA])
        CP("dve", AA[:, 1, 1, :], abr, [stmp], [AA])
        CP("dve", AA[:, 1, 0, :], abi, [stmp], [AA])
        TS("dve", AA[:, 0, 1, :], abi, -1.0, None, ALU.mult, None, [stmp], [AA])
        pt = [fT0[:, i, :] for i in range(8)]
        DMA("sp", merged0[:], aPd, W=[merged0])
        DMA("sp", ys0[:], bexpd, W=[ys0])
        DMA("sp", cv0[:], cexpd, W=[cv0])
        aRp, aIp, lDp = merged0[:, 0:1024], merged0[:, 1024:2048], merged0[:, 2048:3072]
        abrP, abiP = abar(aRp, aIp, lDp, pt, [fT0, merged0], fT0)
        _, _, q2, q3, q4, q5, q6, q7 = pt
        Bs = [fT0, merged0]
        TT("dve", q2, aRp, aRp, ALU.mult, Bs, [fT0])
        TT("dve", q3, aIp, aIp, ALU.mult, Bs, [fT0])
        TT("dve", q2, q2, q3, ALU.add, [fT0], [fT0])
        RECIP(q2, q2, [fT0], [fT0])
        TS("dve", q3, abrP, -1.0, None, ALU.add, None, [fT0], [fT0])
        TT("dve", q4, q3, aRp, ALU.mult, Bs, [fT0])
        TT("dve", q5, abiP, aIp, ALU.mult, Bs, [fT0])
        TT("dve", q4, q4, q5, ALU.add, [fT0], [fT0])
        TT("dve", q4, q4, q2, ALU.mult, [fT0], [fT0])
        TT("dve", q5, abiP, aRp, ALU.mult, Bs, [fT0])
        TT("dve", q6, q3, aIp, ALU.mult, Bs, [fT0])
        TT("dve", q5, q5, q6, ALU.subtract, [fT0], [fT0])
        TT("dve", q5, q5, q2, ALU.mult, [fT0], [fT0])
        bre, bim = ys0[:, 0:1024], ys0[:, 1024:2048]
        Bs2 = [fT0, ys0]
        TT("dve", q6, q4, bre, ALU.mult, Bs2, [fT0])
        TT("dve", q7, q5, bim, ALU.mult, Bs2, [fT0])
        TT("dve", BbT[0][:].rearrange("p k n -> p (k n)"), q6, q7, ALU.subtract, [fT0], [BbT[0]])
        TT("dve", q6, q4, bim, ALU.mult, Bs2, [fT0])
        TT("dve", q7, q5, bre, ALU.mult, Bs2, [fT0])
        TT("dve", BbT[1][:].rearrange("p k n -> p (k n)"), q6, q7, ALU.add, [fT0], [BbT[1]])
        CP("dve", CT[0][:].rearrange("p k n -> p (k n)"), cv0[:, 0:2048], [cv0], [CT[0]])
        TS("dve", CT[1][:].rearrange("p k n -> p (k n)"), cv0[:, 2048:4096], -1.0, None, ALU.mult, None, [cv0], [CT[1]])
        for ri in range(2):
            for j in range(2):
                TS("dve", BbTm[ri][j][:], BbT[ri][:], V(V_M0 + j), None, ALU.mult, None, [BbT[ri], vec], [BbTm[ri][j]])

        dump("adacol", adacol, adacol[:], [128, 96]); dump("AA", AA, AA[:], [128, 2, 2, 32]); dump("pg1", pg1, pg1[:], [128, D])
        dump("BbT0", BbT[0], BbT[0][:], [128, 8, 128], BF16); dump("BbT1", BbT[1], BbT[1][:], [128, 8, 128], BF16)
        dump("CT0", CT[0], CT[0][:], [128, 32, 64], BF16); dump("scale1", scale1, scale1[:], [128, 16])
        stop_at("p0")
        barrier()
        xn = carve([128, D], BF16); hT = carve([128, 16, TB], BF16)
        vT = carve([128, 8, 30 + TB]); cv = carve([128, 8, TB])
        sqt = [carve([128, TB]) for i in range(2)]
        meansb = carve([128, TB]); rstdsb = carve([128, TB]); tmpa = [carve([128, TB]) for i in range(3)]
        vn = carve([128, 8, TB], BF16); uT = carve([128, 8, TB], BF16)
        ys = carve([128, 8, TB]); gy = carve([128, 8, TB], BF16)
        bu = carve([128, 2, 32, SBK]); sS = [carve([128, 2, 32, SBK]) for i in range(2)]
        sbf = carve([128, 2, 32, SBK], BF16)
        merged = carve([128, 16, TB], BF16)
        msb = carve([128, D]); h2Tf = carve([128, 16, 128]); h2Tb = carve([128, 16, TB], BF16)
        xt0 = xin[0]

        def rms_rstd(src_ap, srcB, col):
            ACT(xn[:], src_ap, AF.Square, [srcB], [xn, ssq], accum_out=ssq[:, col:col + 1])
            TS("dve", ssq[:, col:col + 1], ssq[:, col:col + 1], 1.0 / D, EPS, ALU.mult, ALU.add, [ssq], [ssq])
            ACT(ssq[:, col:col + 1], ssq[:, col:col + 1], AF.Sqrt, [ssq], [ssq])
            RECIP(ssq[:, col:col + 1], ssq[:, col:col + 1], [ssq], [ssq])

        def norm_T_bf(src_ap, srcB, scale_t, shf, dst):
            rms_rstd(src_ap, srcB, 0)
            ACT(xn[:], src_ap, AF.Identity, [srcB, ssq], [xn], scale=ssq[:, 0:1])
            for k4 in range(4):
                p = nps()
                pb = p[:].bitcast(BF16)
                for kk in range(4):
                    k = k4 * 4 + kk
                    TR(pb[:, kk * 128:(kk + 1) * 128], xn[:, k * 128:(k + 1) * 128], identb[:], [xn, identb], [p])
                for kk in range(4):
                    k = k4 * 4 + kk
                    ACT(dst[:, k, :], pb[:, kk * 128:(kk + 1) * 128], AF.Identity,
                        [p, scale_t, adacol], [dst], scale=scale_t[:, k:k + 1], bias=shf(k))

        def proj(pB_, pout, wb, c0, rhsB, rhs_fn, nk):
            for k in range(nk):
                MM(pout, wb[:, k, c0:c0 + 128], rhs_fn(k), k == 0, k == nk - 1, [wb, rhsB], [pB_])

        def wload(src_ap, nk, ncol=512):
            wb = nwb()
            DMA("pool", wb[:, 0:nk, 0:ncol], src_ap.rearrange("(k p) n -> p k n", p=128), W=[wb])
            return wb

        MEMSET("dve", sS[1][:], 0.0, [sS[1]])
        MEMSET("dve", vT[:], 0.0, [vT])
        sidx = [0]
        NS = TB // SBK

        def s5_scan_block(with_y):
            for sbk in range(NS):
                t0 = sbk * SBK
                for ri in range(2):
                    pp = [nps(), nps()]
                    pvs = [pp_[:, 0:16 * SBK].rearrange("p (q j n) -> p q j n", q=8, j=2) for pp_ in pp]
                    for kt in range(32):
                        q, ktl = kt // 4, kt % 4
                        h2_, j_ = ktl // 2, ktl % 2
                        MM(pvs[h2_][:, q, j_, :], BbTm[ri][j_][64 * h2_:64 * h2_ + 64, q, :], uT[64 * h2_:64 * h2_ + 64, q, t0:t0 + SBK],
                           True, True, [BbTm[ri][j_], uT], [pp[h2_]])
                    buv = bu[:, ri, :, :].rearrange("p (q k) n -> p q k n", k=4)
                    for h2_ in range(2):
                        CP("act", buv[:, :, 2 * h2_:2 * h2_ + 2, :], pvs[h2_], [pp[h2_]], [bu])
                cur = sS[sidx[0] % 2]
                prv = sS[(sidx[0] + 1) % 2]
                sidx[0] += 1
                for t in range(SBK):
                    if t == 0:
                        pa, pBuf = prv[:, :, :, SBK - 1], prv
                    else:
                        pa, pBuf = cur[:, :, :, t - 1], cur
                    TT("dve", Mt[:], AA[:], pa.unsqueeze(1).broadcast_to([128, 2, 2, 32]), ALU.mult, [AA, pBuf], [Mt])
                    TT("dve", Rt[:], Mt[:, :, 0, :], Mt[:, :, 1, :], ALU.add, [Mt], [Rt])
                    TT("dve", cur[:, :, :, t], Rt[:], bu[:, :, :, t], ALU.add, [Rt, bu], [cur])
                if with_y:
                    CP("act", sbf[:], cur[:], [cur], [sbf])
                    p = nps()
                    pv = p[:, 0:8 * SBK].rearrange("p (a n) -> p a n", a=8)
                    for kt in range(32):
                        q, ktl = kt // 4, kt % 4
                        h2_, j_ = ktl // 2, ktl % 2
                        MM(pv[64 * h2_:64 * h2_ + 64, q, :], CT[0][:, kt, :], sbf[:, 0, kt, :], j_ == 0, False, [CT[0], sbf], [p])
                        MM(pv[64 * h2_:64 * h2_ + 64, q, :], CT[1][:, kt, :], sbf[:, 1, kt, :], False, j_ == 1, [CT[1], sbf], [p])
                    for q in range(8):
                        STT(ys[:, q, t0:t0 + SBK], uT[:, q, t0:t0 + SBK], V(V_S5D + q), pv[:, q, :], ALU.mult, ALU.add,
                            [uT, vec, p], [ys])

        def u_proj():
            for half in range(2):
                wb = wload(win[:, 2048 + half * 512:2048 + (half + 1) * 512], 16)
                for jj in range(4):
                    p = nps()
                    proj(p, p[:, 0:TB], wb, jj * 128, hT, lambda k: hT[:, k, :], 16)
                    CP("act", uT[:, half * 4 + jj, :], p[:, 0:TB], [p], [uT])

        def conv_glu_proj():
            for half in range(2):
                wbv = wload(win[:, half * 512:(half + 1) * 512], 16)
                wbg = wload(win[:, 1024 + half * 512:1024 + (half + 1) * 512], 16)
                for jj in range(4):
                    j = half * 4 + jj
                    pv_ = nps()
                    proj(pv_, pv_[:, 0:TB], wbv, jj * 128, hT, lambda k: hT[:, k, :], 16)
                    pg_ = nps()
                    proj(pg_, pg_[:, 0:TB], wbg, jj * 128, hT, lambda k: hT[:, k, :], 16)
                    sg = sqt[j % 2]
                    ACT(sg[:], pg_[:, 0:TB], AF.Sigmoid, [pg_], [sg])
                    TT("dve", vT[:, j, 30:30 + TB], pv_[:, 0:TB], sg[:], ALU.mult, [pv_, sg], [vT])

        for bi in range(NBLK):
            r0 = bi * TB
            DMA("sp", xt0[:], xp[r0:r0 + 128, :], W=[xt0])
            if bi == 0:
                stop_at("bar")
            norm_T_bf(xt0[:], xt0, scale1, sh1, hT)
            if bi == 0:
                stop_at("n0")
            u_proj()
            if bi == 0:
                stop_at("u0")
            s5_scan_block(False)
            if bi == 0:
                stop_at("s0")
            if bi == NBLK - 1:
                conv_glu_proj()
        last = sS[(sidx[0] + 1) % 2]
        TS("dve", last[:, :, :, SBK - 1], last[:, :, :, SBK - 1], flag[:, 0:1], None, ALU.mult, None, [last, flag], [last])
        TS("dve", vT[:, :, 0:30], vT[:, :, TB:TB + 30], flag[:, 0:1], None, ALU.mult, None, [vT, flag], [vT])

        dump("last", last, last[:], [128, 2, 32, SBK]); dump("halo", vT, vT[:], [128, 8, 30 + TB])
        stop_at("p1a")
        for bi in range(NBLK):
            r0 = bi * TB
            DMA("sp", xt0[:], xo[r0:r0 + 128, :], W=[xt0])
            norm_T_bf(xt0[:], xt0, scale1, sh1, hT)
            if bi == 0:
                dump("hT", hT, hT[:], [128, 16, TB], BF16)
            conv_glu_proj()
            pmean = nps(); pmsq = nps()
            for j in range(8):
                acc = cv[:, j, :]
                TS("dve", acc, vT[:, j, 0:TB], V(V_DWW + j * 31), V(V_DWB + j), ALU.mult, ALU.add, [vT, vec], [cv])
                for k in range(1, 31):
                    STT(acc, vT[:, j, k:k + TB], V(V_DWW + j * 31 + k), acc, ALU.mult, ALU.add, [vT, vec, cv], [cv])
                sq = sqt[j % 2]
                ACT(sq[:], acc, AF.Square, [cv], [sq])
                MM(pmean[:, 0:TB], onesm[:], acc, j == 0, j == 7, [onesm, cv], [pmean])
                MM(pmsq[:, 0:TB], onesm[:], sq[:], j == 0, j == 7, [onesm, sq], [pmsq])
            CP("act", meansb[:], pmean[:, 0:TB], [pmean], [meansb])
            TT("dve", rstdsb[:], meansb[:], meansb[:], ALU.mult, [meansb], [rstdsb])
            TT("dve", rstdsb[:], pmsq[:, 0:TB], rstdsb[:], ALU.subtract, [pmsq, rstdsb], [rstdsb])
            TS("dve", rstdsb[:], rstdsb[:], EPS, None, ALU.add, None, [rstdsb], [rstdsb])
            ACT(rstdsb[:], rstdsb[:], AF.Sqrt, [rstdsb], [rstdsb])
            RECIP(rstdsb[:], rstdsb[:], [rstdsb], [rstdsb])
            for j in range(8):
                t_ = tmpa[j % 2]
                TT("dve", t_[:], cv[:, j, :], meansb[:], ALU.subtract, [cv, meansb], [t_])
                TT("dve", t_[:], t_[:], rstdsb[:], ALU.mult, [t_, rstdsb], [t_])
                ACT(vn[:, j, :], t_[:], AF.Silu, [t_, vec], [vn], scale=V(V_CLNG + j), bias=V(V_CLNB + j))
            if bi == 0:
                dump("vT", vT, vT[:], [128, 8, 30 + TB]); dump("cv", cv, cv[:], [128, 8, TB]); dump("vn", vn, vn[:], [128, 8, TB], BF16)
            CP("dve", vT[:, :, 0:30], vT[:, :, TB:TB + 30], [vT], [vT])
            for db in range(4):
                wb = wload(cpw[:, db * 512:(db + 1) * 512], 8)
                wg = wload(win[:, 3072 + db * 512:3072 + (db + 1) * 512], 16)
                for dd in range(4):
                    d = db * 4 + dd
                    py = nps()
                    proj(py, py[:, 0:TB], wb, dd * 128, vn, lambda k: vn[:, k, :], 8)
                    pgt = nps()
                    proj(pgt, pgt[:, 0:TB], wg, dd * 128, hT, lambda k: hT[:, k, :], 16)
                    sg = sqt[d % 2]
                    ACT(sg[:], pgt[:, 0:TB], AF.Sigmoid, [pgt, vec], [sg], bias=V(V_GATEB + d))
                    STT(merged[:, d, :], py[:, 0:TB], V(V_CPB + d), sg[:], ALU.add, ALU.mult, [py, vec, sg], [merged])
            if bi == 0:
                dump("mergedc", merged, merged[:], [128, 16, TB], BF16)
            u_proj()
            s5_scan_block(True)
            for q in range(8):
                t_ = tmpa[q % 2]
                ACT(t_[:], ys[:, q, :], AF.Square, [ys], [t_])
                TS("dve", t_[:], t_[:], 0.044715, 1.0, ALU.mult, ALU.add, [t_], [t_])
                TT("dve", t_[:], t_[:], ys[:, q, :], ALU.mult, [t_, ys], [t_])
                ACT(t_[:], t_[:], AF.Sigmoid, [t_], [t_], scale=1.5957691216057308)
                TT("dve", gy[:, q, :], t_[:], ys[:, q, :], ALU.mult, [t_, ys], [gy])
            for db in range(4):
                wab = nwb()
                DMA("pool", wab[:, 0:8, :], gwa[:, db * 512:(db + 1) * 512].rearrange("(k p) n -> p k n", p=128), W=[wab])
                DMA("pool", wab[:, 8:16, :], gwb[:, db * 512:(db + 1) * 512].rearrange("(k p) n -> p k n", p=128), W=[wab])
                wg = wload(win[:, 5120 + db * 512:5120 + (db + 1) * 512], 16)
                for dd in range(4):
                    d = db * 4 + dd
                    pa_ = nps()
                    for k in range(8):
                        MM(pa_[:, 0:TB], wab[:, k, dd * 128:(dd + 1) * 128], gy[:, k, :], k == 0, k == 7, [wab, gy], [pa_])
                    pb_ = nps()
                    for k in range(8):
                        MM(pb_[:, 0:TB], wab[:, 8 + k, dd * 128:(dd + 1) * 128], gy[:, k, :], k == 0, k == 7, [wab, gy], [pb_])
                    pg2_ = nps()
                    proj(pg2_, pg2_[:, 0:TB], wg, dd * 128, hT, lambda k: hT[:, k, :], 16)
                    t1 = tmpa[0]; t2 = tmpa[1]; t3 = tmpa[2]
                    ACT(t1[:], pb_[:, 0:TB], AF.Sigmoid, [pb_], [t1])
                    TT("dve", t2[:], pa_[:, 0:TB], t1[:], ALU.mult, [pa_, t1], [t2])
                    ACT(t3[:], pg2_[:, 0:TB], AF.Sigmoid, [pg2_, vec], [t3], bias=V(V_GATEB + 16 + d))
                    TT("dve", t2[:], t2[:], t3[:], ALU.mult, [t2, t3], [t2])
                    TT("dve", merged[:, d, :], merged[:, d, :], t2[:], ALU.add, [merged, t2], [merged])
            if bi == 0:
                dump("uT", uT, uT[:], [128, 8, TB], BF16); dump("ys", ys, ys[:], [128, 8, TB]); dump("gy", gy, gy[:], [128, 8, TB], BF16)
                dump("merged", merged, merged[:], [128, 16, TB], BF16)
            for nb in range(4):
                wo = wload(wout[:, nb * 512:(nb + 1) * 512], 16)
                pm = nps()
                for k in range(16):
                    MM(pm[:, :], merged[:, k, :], wo[:, k, :], k == 0, k == 15, [merged, wo], [pm])
                CP("act", msb[:, nb * 512:(nb + 1) * 512], pm[:, :], [pm], [msb])
            if bi == 0:
                dump("m", msb, msb[:], [128, D])
            rms_rstd(msb[:], msb, 1)
            STT(msb[:], msb[:], ssq[:, 1:2], pg1[:], ALU.mult, ALU.mult, [msb, ssq, pg1], [msb])
            DMA("sp", xt0[:], xo[r0:r0 + 128, :], W=[xt0])
            TT("dve", xt0[:], xt0[:], msb[:], ALU.add, [xt0, msb], [xt0])
            DMA("sp", outd[r0:r0 + 128, :], xt0[:], R=[xt0], W=[outdram])
            rms_rstd(xt0[:], xt0, 2)
            ACT(msb[:], xt0[:], AF.Identity, [xt0, ssq], [msb], scale=ssq[:, 2:3])
            for k4 in range(4):
                p = nps()
                for kk in range(4):
                    k = k4 * 4 + kk
                    TR(p[:, kk * 128:(kk + 1) * 128], msb[:, k * 128:(k + 1) * 128], identf[:], [msb, identf], [p])
                for kk in range(4):
                    k = k4 * 4 + kk
                    ACT(h2Tf[:, k, :], p[:, kk * 128:(kk + 1) * 128], AF.Identity, [p, scale2, adacol], [h2Tf],
                        scale=scale2[:, k:k + 1], bias=sh2(k))
            pl = nps()
            for k in range(16):
                MM(pl[:, 0:E], h2Tf[:, k, :], rwf[:, k * E:(k + 1) * E], k == 0, k == 15, [h2Tf, rwf], [pl])
            TT("dve", lg[:], pl[:, 0:E], rbb[:], ALU.add, [pl, rbb], [lg])
            P.op("dve", lambda e: e.max(out=mx8[:], in_=lg[:]), [lg], [mx8])
            TS("dve", gmask[:], lg[:], mx8[:, 3:4], None, ALU.is_ge, None, [lg, mx8], [gmask])
            TS("dve", small[:, 0:1], mx8[:, 0:1], -1.0, None, ALU.mult, None, [mx8], [small])
            ACT(gsm[:], lg[:], AF.Exp, [lg, small], [gsm], bias=small[:, 0:1])
            TT("dve", gsm[:], gsm[:], gmask[:], ALU.mult, [gsm, gmask], [gsm])
            P.op("dve", lambda e: e.tensor_reduce(out=small[:, 1:2], in_=gsm[:], axis=AX.X, op=ALU.add), [gsm], [small])
            RECIP(small[:, 1:2], small[:, 1:2], [small], [small])
            TS("dve", gsm[:], gsm[:], small[:, 1:2], None, ALU.mult, None, [gsm, small], [gsm])
            pt_ = nps()
            TR(pt_[0:E, 0:128], gsm[:], identf[:], [gsm, identf], [pt_])
            CP("act", GT[:, r0:r0 + 128], pt_[0:E, 0:128], [pt_], [GT])
            ACT(xn[:], xt0[:], AF.Identity, [xt0, ssq], [xn], scale=ssq[:, 2:3])
            pc = nps()
            MM(pc[:, 0:E], tri[:], gmask[:], True, True, [tri, gmask], [pc])
            MM(pc[:, E:2 * E], onesf[:], gmask[:], True, True, [onesf, gmask], [pc])
            slot, valid, idxm, rk, selk, tmpk = rt
            TT("dve", slot[:], pc[:, 0:E], basebc[:], ALU.add, [pc, basebc], [slot])
            TT("dve", basebc[:], basebc[:], pc[:, E:2 * E], ALU.add, [pc, basebc], [basebc])
            TS("dve", valid[:], slot[:], float(CAP), None, ALU.is_lt, None, [slot], [valid])
            TT("dve", valid[:], valid[:], gmask[:], ALU.mult, [valid, gmask], [valid])
            TT("dve", idxm[:], slot[:], ecap[:], ALU.add, [slot, ecap], [idxm])
            TS("dve", idxm[:], idxm[:], -BIGI, None, ALU.add, None, [idxm], [idxm])
            P.op("dve", lambda e: e.tensor_tensor_scan(out=rk[:], data0=onesf[:, 0:E], data1=gmask[:], initial=0.0,
                                                       op0=ALU.mult, op1=ALU.add), [onesf, gmask], [rk])
            TT("dve", rk[:], rk[:], gmask[:], ALU.subtract, [rk, gmask], [rk])
            for k in range(4):
                TS("dve", selk[:], rk[:], float(k), None, ALU.is_equal, None, [rk], [selk])
                TT("dve", selk[:], selk[:], valid[:], ALU.mult, [selk, valid], [selk])
                TT("dve", tmpk[:], selk[:], idxm[:], ALU.mult, [selk, idxm], [tmpk])
                P.op("dve", lambda e, k=k, bi=bi: e.tensor_reduce(out=IDXF[:, bi, k:k + 1], in_=tmpk[:], axis=AX.X, op=ALU.add), [tmpk], [IDXF])
                TT("dve", tmpk[:], selk[:], gsm[:], ALU.mult, [selk, gsm], [tmpk])
                P.op("dve", lambda e, k=k, bi=bi: e.tensor_reduce(out=GK[:, bi, k:k + 1], in_=tmpk[:], axis=AX.X, op=ALU.add), [tmpk], [GK])
            TS("dve", IDXF[:, bi, :], IDXF[:, bi, :], BIGI, None, ALU.add, None, [IDXF], [IDXF])
            CP("dve", IDXI[:, bi, :], IDXF[:, bi, :], [IDXF], [IDXI])
            for k in range(4):
                P.dma("pool", lambda e, k=k, bi=bi: e.indirect_dma_start(
                    out=Xgd[:, :], out_offset=bass.IndirectOffsetOnAxis(ap=IDXI[:, bi, k:k + 1], axis=0),
                    in_=xn[:], in_offset=None, bounds_check=breg[0], oob_is_err=False), [xn, IDXI], [xgdram])
            if bi == 0:
                dump("lg", lg, lg[:], [128, E]); dump("gsm", gsm, gsm[:], [128, E])
                dump("idx", IDXI, IDXI[:, 0, :], [128, 4], mybir.dt.int32); dump("gk", GK, GK[:, 0, :], [128, 4])
                stop_at("b0")

        def actb_alias(v):
            b = Buf(v)
            abufs.append(b)
            return b

        stop_at("p1b")
        barrier()
        XgT = carve([128, 16, CAP], BF16); actb = carve([128, 16, CAP], BF16)
        xgs = [carve([128, D], BF16) for i in range(2)]
        ybuf = [carve([128, 512]) for i in range(4)]
        gtt = [carve([128, SBW]) for i in range(2)]; ltt = [carve([128, SBW])]; sgt = [carve([128, SBW])]
        NST = CAP // 128
        yi = [0]
        for e in range(E):
            for st_ in range(NST):
                xg = xgs[st_ % 2]
                DMA("sp", xg[:], Xgd[e * CAP + st_ * 128:e * CAP + (st_ + 1) * 128, :], R=[xgdram], W=[xg])
                for k4 in range(4):
                    p = nps()
                    pb = p[:].bitcast(BF16)
                    for kk in range(4):
                        k = k4 * 4 + kk
                        TR(pb[:, kk * 128:(kk + 1) * 128], xg[:, k * 128:(k + 1) * 128], identb[:], [xg, identb], [p])
                    for kk in range(4):
                        k = k4 * 4 + kk
                        ACT(XgT[:, k, st_ * 128:(st_ + 1) * 128], pb[:, kk * 128:(kk + 1) * 128], AF.Identity,
                            [p, scale2, adacol], [XgT], scale=scale2[:, k:k + 1], bias=sh2(k))
            for fb in range(8):
                wb = nwb()
                DMA("pool", wb[:, :, 0:256], w1g[e][:, fb * 256:(fb + 1) * 256].rearrange("(k p) n -> p k n", p=128), W=[wb])
                DMA("pool", wb[:, :, 256:512], w1l[e][:, fb * 256:(fb + 1) * 256].rearrange("(k p) n -> p k n", p=128), W=[wb])
                for fc in range(2):
                    f = fb * 2 + fc
                    for sb_ in range(CAP // SBW):
                        s0 = sb_ * SBW
                        pg_ = nps()
                        proj(pg_, pg_[:, 0:SBW], wb, fc * 128, XgT, lambda k: XgT[:, k, s0:s0 + SBW], 16)
                        pl_ = nps()
                        proj(pl_, pl_[:, 0:SBW], wb, 256 + fc * 128, XgT, lambda k: XgT[:, k, s0:s0 + SBW], 16)
                        g_ = gtt[(f + sb_) % 2]; l_ = ltt[0]; s_ = sgt[0]
                        TS("dve", g_[:], pg_[:, 0:SBW], V(V_B1G + e * 16 + f), 7.0, ALU.add, ALU.min, [pg_, vec], [g_])
                        ACT(s_[:], g_[:], AF.Sigmoid, [g_], [s_], scale=1.702)
                        TS("dve", l_[:], pl_[:, 0:SBW], V(V_B1L + e * 16 + f), 7.0, ALU.add, ALU.min, [pl_, vec], [l_])
                        TS("pool", l_[:], l_[:], -7.0, 1.0, ALU.max, ALU.add, [l_], [l_])
                        TT("dve", g_[:], g_[:], s_[:], ALU.mult, [g_, s_], [g_])
                        TT("pool", actb[:, f, s0:s0 + SBW], g_[:], l_[:], ALU.mult, [g_, l_], [actb])
            for db in range(4):
                wb = wload(w2d[e][:, db * 512:(db + 1) * 512], 16)
                for st_ in range(NST):
                    py = nps()
                    for k in range(16):
                        MM(py[:, :], actb[:, k, st_ * 128:(st_ + 1) * 128], wb[:, k, :], k == 0, k == 15, [actb, wb], [py])
                    yb = ybuf[yi[0] % 4]
                    yi[0] += 1
                    CP("act", yb[:], py[:, :], [py], [yb])
                    DMA("sp", Ygd[e * CAP + st_ * 128:e * CAP + (st_ + 1) * 128, db * 512:(db + 1) * 512], yb[:], R=[yb], W=[ygdram])
        barrier()
        facc = carve([128, D]); yk = [carve([128, D]) for i in range(2)]; junk = carve([128, D], BF16)
        gi = [0]
        for bi in range(NBLK):
            r0 = bi * TB
            for nb in range(4):
                pbias = nps()
                MM(pbias[:, :], GT[:, r0:r0 + 128], b2s[:, nb * 512:(nb + 1) * 512], True, True, [GT, b2s], [pbias])
                CP("act", facc[:, nb * 512:(nb + 1) * 512], pbias[:, :], [pbias], [facc])
            for k in range(4):
                y_ = yk[gi[0] % 2]
                gi[0] += 1
                MEMSET("pool", y_[:], 0.0, [y_])
                P.dma("pool", lambda e, k=k, bi=bi, y_=y_: e.indirect_dma_start(
                    out=y_[:], out_offset=None, in_=Ygd[:, :],
                    in_offset=bass.IndirectOffsetOnAxis(ap=IDXI[:, bi, k:k + 1], axis=0),
                    bounds_check=breg[0], oob_is_err=False), [IDXI, ygdram], [y_])
                STT(facc[:], y_[:], GK[:, bi, k:k + 1], facc[:], ALU.mult, ALU.add, [y_, GK, facc], [facc])
            ACT(junk[:], facc[:], AF.Square, [facc], [junk, ssq], accum_out=ssq[:, 3:4])
            TS("dve", ssq[:, 3:4], ssq[:, 3:4], 1.0 / D, EPS, ALU.mult, ALU.add, [ssq], [ssq])
            ACT(ssq[:, 3:4], ssq[:, 3:4], AF.Sqrt, [ssq], [ssq])
            RECIP(ssq[:, 3:4], ssq[:, 3:4], [ssq], [ssq])
            STT(facc[:], facc[:], ssq[:, 3:4], pg2[:], ALU.mult, ALU.mult, [facc, ssq, pg2], [facc])
            DMA("sp", xt0[:], outd[r0:r0 + 128, :], R=[outdram], W=[xt0])
            TT("dve", xt0[:], xt0[:], facc[:], ALU.add, [xt0, facc], [xt0])
            DMA("sp", outd[r0:r0 + 128, :], xt0[:], R=[xt0], W=[outdram])

    try:
        body()
    except _Stop:
        pass

    sems = {}
    for k in ("pe", "act", "dve", "pool", "sp"):
        sems[("c", k)] = es.enter_context(nc.semaphore(f"c_{k}"))
    for q in ("sp", "pool"):
        for i in range(Prog.R):
            sems[("d", q, i)] = es.enter_context(nc.semaphore(f"d_{q}{i}"))

    breg = [None]

    def run(name, e):
        if name == "pool":
            breg[0] = e.to_reg(E * CAP - 1)
        for waits, fn, key, amt in P.ops[name]:
            for wk, wv in waits:
                e.wait_ge(sems[wk], wv)
            fn(e).then_inc(sems[key], amt)
        if name == "sp":
            n = P.dq["sp"]
            for i in range(Prog.R):
                cnt = (n - i + Prog.R - 1) // Prog.R
                if cnt > 0:
                    e.wait_ge(sems[("d", "sp", i)], 16 * cnt)

    with es:
        with nc.Block() as block:
            @block.tensor
            def _(e):
                run("pe", e)

            @block.scalar
            def _(e):
                run("act", e)

            @block.vector
            def _(e):
                run("dve", e)

            @block.gpsimd
            def _(e):
                run("pool", e)

            @block.sync
            def _(e):
                run("sp", e)
    return nc


def _prep(inputs):
    f32 = np.float32
    g = {k: np.asarray(v) for k, v in inputs.items()}
    x = g["x"]; c = g["c"]
    sh = {}
    sh["ada_w"] = np.ascontiguousarray(g["ada_w"][0]); sh["ada_b"] = np.ascontiguousarray(g["ada_b"][0][None, :])
    def fm(v, n):
        return np.ascontiguousarray(v.reshape(n, 128).T)
    vec = np.zeros((128, NV), f32)
    vec[:, V_PREMIX:V_PREMIX + 16] = fm(g["pre_mix_g"][0], 16)
    vec[:, V_PREFFN:V_PREFFN + 16] = fm(g["pre_ffn_g"][0], 16)
    vec[:, V_GATEB:V_GATEB + 32] = fm(g["gate_b"][0], 32)
    vec[:, V_DWB:V_DWB + 8] = fm(g["dw_b"][0], 8)
    vec[:, V_CLNG:V_CLNG + 8] = fm(g["cln_g"][0], 8)
    vec[:, V_CLNB:V_CLNB + 8] = fm(g["cln_b"][0], 8)
    vec[:, V_CPB:V_CPB + 16] = fm(g["cp_b"][0], 16)
    vec[:, V_S5D:V_S5D + 8] = fm(g["s5_d"][0], 8)
    dww = g["dw_w"][0]
    vec[:, V_DWW:V_DWW + 248] = dww.T.reshape(8, 128, 31).transpose(1, 0, 2).reshape(128, 248)
    b1 = g["b1"][0]
    vec[:, V_B1G:V_B1G + 512] = b1[:, 0::2].reshape(32, 16, 128).transpose(2, 0, 1).reshape(128, 512)
    vec[:, V_B1L:V_B1L + 512] = b1[:, 1::2].reshape(32, 16, 128).transpose(2, 0, 1).reshape(128, 512)
    vec[:, V_B2:V_B2 + 512] = g["b2"][0].reshape(32, 16, 128).transpose(2, 0, 1).reshape(128, 512)
    pidx = (np.arange(128) // 32) % 2
    vec[:, V_M0] = (pidx == 0).astype(f32); vec[:, V_M0 + 1] = (pidx == 1).astype(f32)
    sh["vec"] = vec
    sh["routerb"] = np.ascontiguousarray(g["router_b"][0][None, :])
    sh["postmix"] = np.ascontiguousarray(g["post_mix_g"][0][None, :])
    sh["postffn"] = np.ascontiguousarray(g["post_ffn_g"][0][None, :])
    are, aim, ldt = g["s5_a_re"][0], g["s5_a_im"][0], g["s5_log_dt"][0]
    ldt2 = np.broadcast_to(ldt[:, None], (64, 64))
    def Sl(a):
        return a.reshape(32, 2, 64).transpose(1, 2, 0).reshape(128, 32)
    sh["aS"] = np.ascontiguousarray(np.concatenate([Sl(are), Sl(aim), Sl(ldt2)], axis=1).astype(f32))
    def Pl(a):
        t = a.reshape(8, 4, 2, 64)
        t = t.transpose(1, 0, 2, 3)
        t = np.broadcast_to(t[:, None, None], (4, 2, 16, 8, 2, 64))
        return t.reshape(128, 1024)
    sh["aP"] = np.ascontiguousarray(np.concatenate([Pl(are), Pl(aim), Pl(ldt2)], axis=1).astype(f32))
    def Bexp(b):
        t = b.reshape(8, 4, 2, 64, 16)
        o = np.zeros((4, 2, 16, 8, 2, 64), f32)
        for gl in range(2):
            o[:, gl, :, :, gl, :] = t[:, :, gl].transpose(1, 3, 0, 2)
        return o.reshape(128, 1024)
    sh["bexp"] = np.concatenate([Bexp(g["s5_b_re"][0]), Bexp(g["s5_b_im"][0])], axis=1)
    def Cexp(cc):
        t = cc.reshape(32, 2, 16, 64)
        o = np.zeros((2, 64, 32, 2, 2, 16), f32)
        for kt in range(32):
            for gl in range(2):
                o[gl, :, kt, kt % 2, gl, :] = t[kt, gl].T
        return o.reshape(128, 2048)
    sh["cexp"] = np.concatenate([Cexp(g["s5_c_re"][0]), Cexp(g["s5_c_im"][0])], axis=1)
    sh["w_in"] = np.ascontiguousarray(g["w_in"][0]); sh["cp_w"] = np.ascontiguousarray(g["cp_w"][0])
    sh["glu_wa"] = np.ascontiguousarray(g["glu_wa"][0]); sh["glu_wb"] = np.ascontiguousarray(g["glu_wb"][0])
    sh["w_out"] = np.ascontiguousarray(g["w_out"][0]); sh["router_w"] = np.ascontiguousarray(g["router_w"][0])
    w1 = g["w1"][0]
    sh["w1g"] = np.ascontiguousarray(w1[:, :, 0::2]); sh["w1l"] = np.ascontiguousarray(w1[:, :, 1::2])
    sh["w2"] = np.ascontiguousarray(g["w2"][0])
    sh["ident"] = np.eye(128, dtype=f32)
    sh["tri"] = np.triu(np.ones((128, 128), f32), 1)
    sh["ecap"] = np.broadcast_to((np.arange(E, dtype=f32) * CAP)[None, :], (128, E)).copy()
    sh["b2s"] = np.ascontiguousarray(g["b2"][0])
    maps = []
    for core in range(8):
        b, half = core // 2, core % 2
        m = dict(sh)
        m["xo"] = np.ascontiguousarray(x[b, half * NTOK:(half + 1) * NTOK])
        m["xp"] = np.ascontiguousarray(x[b, 0:NTOK]) if half == 1 else np.zeros((NTOK, D), f32)
        m["flag"] = np.full((128, 1), float(half), f32)
        m["ccol"] = fm(c[b], 16)
        maps.append(m)
    return maps


def kernel(**inputs):
    maps = _prep(inputs)
    nc = build_nc()
    res = run_bass_kernel_spmd(nc, maps, core_ids=list(range(8)))
    out = np.zeros((4, 4096, D), np.float32)
    for core in range(8):
        b, half = core // 2, core % 2
        out[b, half * NTOK:(half + 1) * NTOK] = res.results[core]["out"]
    return out
```

```python
import math
from contextlib import ExitStack
import numpy as np
import concourse.bass as bass
import concourse.mybir as mybir
from concourse.bass_utils import run_bass_kernel_spmd

F32 = mybir.dt.float32
BF16 = mybir.dt.bfloat16
AF = mybir.ActivationFunctionType
ALU = mybir.AluOpType
AX = mybir.AxisListType

D = 2048
NTOK = 2048
TB = 128
SBK = 16
NBLK = NTOK // TB
E = 32
MTB = 512
CAP = 768
SBW = 384
BIGI = 4000000.0
EPS = 1e-6

V_PREMIX, V_PREFFN, V_GATEB, V_DWB, V_CLNG, V_CLNB, V_CPB, V_S5D = 0, 16, 32, 64, 72, 80, 88, 104
V_DWW = 112
V_B1G = V_DWW + 8 * 31
V_B1L = V_B1G + 512
V_B2 = V_B1L + 512
V_M0 = V_B2 + 512
NV = V_M0 + 2


class Buf:
    __slots__ = ("t", "w", "r")

    def __init__(self, t):
        self.t = t
        self.w = None
        self.r = {}

    def __getitem__(self, idx):
        return self.t[idx]


class Prog:
    R = 8

    def __init__(self):
        self.ops = {k: [] for k in ("pe", "act", "dve", "pool", "sp")}
        self.cnt = {k: 0 for k in self.ops}
        self.known = {k: {} for k in self.ops}
        self.dq = {"sp": 0, "pool": 0}

    def _emit(self, eng, fn, R, W, key, amt, val, extra=()):
        need = {}

        def add(tok):
            if tok is None:
                return
            k, v = tok
            if need.get(k, 0) < v:
                need[k] = v

        for b in R:
            add(b.w)
        for b in W:
            add(b.w)
            for k, v in b.r.items():
                add((k, v))
        for t in extra:
            add(t)
        waits = []
        for k, v in need.items():
            if k == ("c", "pe") and eng == "pe":
                continue
            if self.known[eng].get(k, 0) >= v:
                continue
            self.known[eng][k] = v
            waits.append((k, v))
        self.ops[eng].append((waits, fn, key, amt))
        tok = (key, val)
        for b in W:
            b.w = tok
            b.r = {}
        for b in R:
            if b not in W and b.r.get(key, 0) < val:
                b.r[key] = val
        return tok

    def op(self, eng, fn, R=(), W=()):
        self.cnt[eng] += 1
        return self._emit(eng, fn, R, W, ("c", eng), 1, self.cnt[eng])

    def dma(self, q, fn, R=(), W=()):
        j = self.dq[q]
        self.dq[q] += 1
        key = ("d", q, j % self.R)
        extra = []
        if j >= self.R:
            extra.append((key, 16 * (j // self.R)))
        return self._emit(q, fn, R, W, key, 16, 16 * (j // self.R + 1), extra)


class _Stop(Exception):
    pass


def build_nc(stop=None, dumps=()):
    nc = bass.Bass("TRN2", target_bir_lowering=False)
    P = Prog()
    dbg = {}

    def dump(name, B, ap, shape, dt=F32):
        if name not in dumps:
            return
        t = nc.dram_tensor("dbg_" + name, list(shape), dt, kind="ExternalOutput").ap()
        P.dma("sp", lambda e: e.dma_start(out=t, in_=ap), [B], ())

    def stop_at(tag):
        if stop == tag:
            raise _Stop()

    def din(name, shape, dt=F32):
        return nc.dram_tensor(name, list(shape), dt, kind="ExternalInput").ap()

    xo = din("xo", [NTOK, D]); xp = din("xp", [NTOK, D]); flagd = din("flag", [128, 1])
    ccold = din("ccol", [128, 16]); adaw = din("ada_w", [D, 6 * D]); adab = din("ada_b", [1, 6 * D])
    vecd = din("vec", [128, NV]); routerbd = din("routerb", [1, E])
    postmixd = din("postmix", [1, D]); postffnd = din("postffn", [1, D])
    aSd = din("aS", [128, 96]); aPd = din("aP", [128, 3072])
    bexpd = din("bexp", [128, 2048]); cexpd = din("cexp", [128, 4096])
    win = din("w_in", [D, 7168]); cpw = din("cp_w", [1024, D]); gwa = din("glu_wa", [1024, D])
    gwb = din("glu_wb", [1024, D]); wout = din("w_out", [D, D]); rwd = din("router_w", [D, E])
    w1g = din("w1g", [E, D, 2048]); w1l = din("w1l", [E, D, 2048]); w2d = din("w2", [E, 2048, D])
    identd = din("ident", [128, 128]); trid = din("tri", [128, 128]); ecapd = din("ecap", [128, E]); b2d = din("b2s", [E, D])
    outd = nc.dram_tensor("out", [NTOK, D], F32, kind="ExternalOutput").ap()
    Xgd = nc.dram_tensor("xg_scratch", [E * CAP, D], BF16, kind="Internal").ap()
    Ygd = nc.dram_tensor("yg_scratch", [E * CAP, D], F32, kind="Internal").ap()

    es = ExitStack()
    h2dram = Buf(None); outdram = Buf(None); xgdram = Buf(None); ygdram = Buf(None)

    def sb(name, shape, dt=F32):
        return Buf(es.enter_context(nc.sbuf_tensor(name, list(shape), dt)))

    ps = [Buf(es.enter_context(nc.psum_tensor(f"ps{i}", [128, 512], F32))) for i in range(8)]
    psi = [0]

    def nps():
        p = ps[psi[0] % 8]
        psi[0] += 1
        return p

    def DMA(q, out, in_, R=(), W=()):
        P.dma(q, lambda e: e.dma_start(out=out, in_=in_), R, W)

    def ACT(out, in_, func, R, W, **kw):
        P.op("act", lambda e: e.activation(out=out, in_=in_, func=func, **kw), R, W)

    def TT(eng, out, a, b, op, R, W):
        P.op(eng, lambda e: e.tensor_tensor(out=out, in0=a, in1=b, op=op), R, W)

    def TS(eng, out, a, s1, s2, op0, op1, R, W):
        if op1 is None:
            P.op(eng, lambda e: e.tensor_scalar(out=out, in0=a, scalar1=s1, scalar2=None, op0=op0), R, W)
        else:
            P.op(eng, lambda e: e.tensor_scalar(out=out, in0=a, scalar1=s1, scalar2=s2, op0=op0, op1=op1), R, W)

    def STT(out, a, s, b, op0, op1, R, W):
        P.op("dve", lambda e: e.scalar_tensor_tensor(out=out, in0=a, scalar=s, in1=b, op0=op0, op1=op1), R, W)

    def MM(out, l, r, st, sp_, R, W):
        P.op("pe", lambda e: e.matmul(out, l, r, start=st, stop=sp_), R, W)

    def TR(out, in_, ident, R, W):
        P.op("pe", lambda e: e.transpose(out, in_, ident), R, W)

    def CP(eng, out, in_, R, W):
        if eng == "act":
            P.op(eng, lambda e: e.activation(out=out, in_=in_, func=AF.Identity), R, W)
        else:
            P.op(eng, lambda e: e.tensor_copy(out=out, in_=in_), R, W)

    def RECIP(out, in_, R, W):
        P.op("dve", lambda e: e.reciprocal(out=out, in_=in_), R, W)

    def MEMSET(eng, ap, c, W):
        P.op(eng, lambda e: e.memset(ap, c), (), W)

    identf = sb("identf", [128, 128]); identb = sb("identb", [128, 128], BF16)
    onesf = sb("onesf", [128, 128]); onesm = sb("onesm", [128, 128])
    vec = sb("vecs", [128, NV]); flag = sb("flagt", [128, 1])
    ccol = sb("ccolt", [128, 16]); silub = sb("silub", [128, 16], BF16)
    adacol = sb("adacol", [128, 96]); scale1 = sb("scale1", [128, 16]); scale2 = sb("scale2", [128, 16])
    pg1 = sb("pg1", [128, D]); pg2 = sb("pg2", [128, D])
    rbb = sb("rbb", [128, E]); rwf = sb("rwf", [128, 16 * E])
    wblk = [sb(f"wblk{i}", [128, 16, 512], BF16) for i in range(2)]
    wbi = [0]

    def nwb():
        w = wblk[wbi[0] % 2]
        wbi[0] += 1
        return w

    xin = [sb("xin0", [128, D])]
    ssq = sb("ssq", [128, 4]); small = sb("small", [128, 16])
    Mt = sb("Mt", [128, 2, 2, 32]); Rt = sb("Rt", [128, 2, 32]); AA = sb("AA", [128, 2, 2, 32])
    lg = sb("lg", [128, E]); mx8 = sb("mx8", [128, 8]); gsm = sb("gsm", [128, E]); gmask = sb("gmask", [128, E])
    GT = sb("GT", [32, NTOK])
    tri = sb("tri_t", [128, 128]); ecap = sb("ecap_t", [128, E]); basebc = sb("basebc", [128, E]); b2s = sb("b2s_t", [E, D])
    IDXF = sb("IDXF", [128, NBLK, 4]); GK = sb("GK", [128, NBLK, 4]); IDXI = sb("IDXI", [128, NBLK, 4], mybir.dt.int32)
    rt = [sb(f"rt{i}", [128, E]) for i in range(6)]
    BbTm = [[sb(f"BbTm{i}{j}", [128, 8, 128], BF16) for j in range(2)] for i in range(2)]
    CT = [sb(f"CT{i}", [128, 32, 64], BF16) for i in range(2)]
    aS = sb("aSt", [128, 96]); stmp = sb("stmp", [128, 12 * 32])
    bars = {k: sb(f"bar_{k}", [128, 4]) for k in ("dve", "act", "pool")}
    AW = 19456
    arena = es.enter_context(nc.sbuf_tensor("arena", [128, AW], F32))
    apos = [0]
    abufs = []

    def carve(shape, dt=F32):
        n = 1
        for d_ in shape[1:]:
            n *= d_
        words = n if dt == F32 else (n + 1) // 2
        o = apos[0]
        apos[0] += words
        assert apos[0] <= AW, ("arena overflow", apos[0])
        v = arena[:, o:o + words]
        if dt != F32:
            v = v.bitcast(dt)
        if len(shape) == 3:
            v = v.rearrange("p (a b) -> p a b", a=shape[1])
        elif len(shape) == 4:
            v = v.rearrange("p (a b c) -> p a b c", a=shape[1], b=shape[2])
        b = Buf(v)
        abufs.append(b)
        return b

    def barrier():
        W0 = list(abufs)
        P.op("dve", lambda e: e.memset(bars["dve"][:], 0.0), (), [bars["dve"]] + W0)
        P.op("pool", lambda e: e.memset(bars["pool"][:], 0.0), (), [bars["pool"]])
        P.op("act", lambda e: e.activation(out=bars["act"][:], in_=flag[:, 0:1].broadcast_to([128, 4]), func=AF.Identity), [flag], [bars["act"]])
        pb_ = nps()
        MM(pb_[0:1, 0:2], onesf[0:1, 0:1], onesf[0:1, 0:2], True, True, [onesf], [pb_])
        allb = [bars["dve"], bars["pool"], bars["act"], pb_]
        P.op("dve", lambda e: e.memset(small[:, 8:9], 0.0), allb, [small])
        P.op("pool", lambda e: e.memset(small[:, 9:10], 0.0), allb + [small], [small])
        P.op("act", lambda e: e.activation(out=small[:, 10:11], in_=flag[:, 0:1], func=AF.Identity), allb + [small], [small])
        pb2 = nps()
        MM(pb2[0:1, 0:2], onesf[0:1, 0:1], onesf[0:1, 0:2], True, True, [onesf, small] + allb, [pb2])
        DMA("sp", flag[:], flagd, R=[small, pb2], W=[flag])
        DMA("pool", ssq[:, 3:4], flagd, R=[small, pb2], W=[ssq])
        del abufs[:]
        apos[0] = 0

    rowb = carve([1 + 127, 512]); adabk = carve([128, 512])
    fT0 = carve([128, 8, 1024]); merged0 = carve([128, 3072]); ys0 = carve([128, 2048]); cv0 = carve([128, 4096])
    BbT = [carve([128, 8, 128], BF16) for i in range(2)]

    def V(off, n=1):
        return vec[:, off:off + n]

    def body():
        DMA("sp", identf[:], identd, W=[identf])
        DMA("sp", vec[:], vecd, W=[vec])
        DMA("sp", flag[:], flagd, W=[flag])
        DMA("sp", ccol[:], ccold, W=[ccol])
        DMA("sp", rwf[:].rearrange("p (k n) -> p k n", k=16), rwd.rearrange("(k p) n -> p k n", p=128), W=[rwf])
        DMA("sp", rbb[:], routerbd.broadcast_to([128, E]), W=[rbb])
        DMA("sp", pg1[:], postmixd.broadcast_to([128, D]), W=[pg1])
        DMA("sp", pg2[:], postffnd.broadcast_to([128, D]), W=[pg2])
        DMA("sp", aS[:], aSd, W=[aS])
        DMA("sp", tri[:], trid, W=[tri])
        DMA("sp", ecap[:], ecapd, W=[ecap])
        DMA("sp", b2s[:], b2d, W=[b2s])
        MEMSET("dve", basebc[:], 0.0, [basebc])
        CP("dve", identb[:], identf[:], [identf], [identb])
        MEMSET("dve", onesf[:], 1.0, [onesf])
        MEMSET("dve", onesm[:], 1.0 / 1024.0, [onesm])
        ACT(silub[:], ccol[:], AF.Silu, [ccol], [silub])

        for cb in range(24):
            wb = nwb()
            DMA("pool", wb[:], adaw[:, cb * 512:(cb + 1) * 512].rearrange("(k p) n -> p k n", p=128), W=[wb])
            DMA("sp", adabk[0:1, :], adab[0:1, cb * 512:(cb + 1) * 512], W=[adabk])
            p = nps()
            for k in range(16):
                MM(p[0:1, :], silub[:, k:k + 1], wb[:, k, :], k == 0, k == 15, [silub, wb], [p])
            TT("dve", rowb[0:1, :], p[0:1, :], adabk[0:1, :], ALU.add, [p, adabk], [rowb])
            p2 = nps()
            for j in range(4):
                MM(p2[:, j:j + 1], rowb[0:1, j * 128:(j + 1) * 128], onesf[0:1, 0:1], True, True, [rowb, onesf], [p2])
            CP("act", adacol[:, cb * 4:cb * 4 + 4], p2[:, 0:4], [p2], [adacol])
            if 8 <= cb < 12 or 20 <= cb < 24:
                pgx, c0 = (pg1, (cb - 8) * 512) if cb < 12 else (pg2, (cb - 20) * 512)
                p3 = nps()
                MM(p3[:, :], onesf[0:1, :], rowb[0:1, :], True, True, [rowb, onesf], [p3])
                TT("dve", pgx[:, c0:c0 + 512], pgx[:, c0:c0 + 512], p3[:, :], ALU.mult, [pgx, p3], [pgx])
        TS("dve", small[:, 0:16], adacol[:, 16:32], 1.0, None, ALU.add, None, [adacol], [small])
        TT("dve", scale1[:], small[:, 0:16], V(V_PREMIX, 16), ALU.mult, [small, vec], [scale1])
        TS("dve", small[:, 0:16], adacol[:, 64:80], 1.0, None, ALU.add, None, [adacol], [small])
        TT("dve", scale2[:], small[:, 0:16], V(V_PREFFN, 16), ALU.mult, [small, vec], [scale2])
        sh1 = lambda k: adacol[:, k:k + 1]
        sh2 = lambda k: adacol[:, 48 + k:49 + k]

        def abar(aR, aI, lD, t, Bs, B):
            dt_, x_, rho, th, c_, s_, t1, t2 = t
            ACT(dt_, lD, AF.Exp, Bs, [B])
            TT("dve", x_, aR, dt_, ALU.mult, Bs, [B])
            ACT(rho, x_, AF.Exp, [B], [B])
            TT("dve", th, aI, dt_, ALU.mult, Bs, [B])
            ACT(s_, th, AF.Sin, [B], [B], scale=1.0 / 32.0)
            TS("dve", x_, th, 1.0 / 32.0, math.pi / 2, ALU.mult, ALU.add, [B], [B])
            ACT(c_, x_, AF.Sin, [B], [B])
            for _ in range(5):
                TT("dve", t1, c_, c_, ALU.mult, [B], [B])
                TT("dve", t2, s_, s_, ALU.mult, [B], [B])
                TT("dve", x_, c_, s_, ALU.mult, [B], [B])
                TT("dve", c_, t1, t2, ALU.subtract, [B], [B])
                TS("dve", s_, x_, 2.0, None, ALU.mult, None, [B], [B])
            TT("dve", dt_, rho, c_, ALU.mult, [B], [B])
            TT("dve", x_, rho, s_, ALU.mult, [B], [B])
            return dt_, x_

        st = [stmp[:, i * 32:(i + 1) * 32] for i in range(8)]
        abr, abi = abar(aS[:, 0:32], aS[:, 32:64], aS[:, 64:96], st, [stmp, aS], stmp)
        CP("dve", AA[:, 0, 0, :], abr, [stmp], [AA])
        CP("dve", AA[:, 1, 1, :], abr, [stmp], [AA])
        CP("dve", AA[:, 1, 0, :], abi, [stmp], [AA])
        TS("dve", AA[:, 0, 1, :], abi, -1.0, None, ALU.mult, None, [stmp], [AA])
        pt = [fT0[:, i, :] for i in range(8)]
        DMA("sp", merged0[:], aPd, W=[merged0])
        DMA("sp", ys0[:], bexpd, W=[ys0])
        DMA("sp", cv0[:], cexpd, W=[cv0])
        aRp, aIp, lDp = merged0[:, 0:1024], merged0[:, 1024:2048], merged0[:, 2048:3072]
        abrP, abiP = abar(aRp, aIp, lDp, pt, [fT0, merged0], fT0)
        _, _, q2, q3, q4, q5, q6, q7 = pt
        Bs = [fT0, merged0]
        TT("dve", q2, aRp, aRp, ALU.mult, Bs, [fT0])
        TT("dve", q3, aIp, aIp, ALU.mult, Bs, [fT0])
        TT("dve", q2, q2, q3, ALU.add, [fT0], [fT0])
        RECIP(q2, q2, [fT0], [fT0])
        TS("dve", q3, abrP, -1.0, None, ALU.add, None, [fT0], [fT0])
        TT("dve", q4, q3, aRp, ALU.mult, Bs, [fT0])
        TT("dve", q5, abiP, aIp, ALU.mult, Bs, [fT0])
        TT("dve", q4, q4, q5, ALU.add, [fT0], [fT0])
        TT("dve", q4, q4, q2, ALU.mult, [fT0], [fT0])
        TT("dve", q5, abiP, aRp, ALU.mult, Bs, [fT0])
        TT("dve", q6, q3, aIp, ALU.mult, Bs, [fT0])
        TT("dve", q5, q5, q6, ALU.subtract, [fT0], [fT0])
        TT("dve", q5, q5, q2, ALU.mult, [fT0], [fT0])
        bre, bim = ys0[:, 0:1024], ys0[:, 1024:2048]
        Bs2 = [fT0, ys0]
        TT("dve", q6, q4, bre, ALU.mult, Bs2, [fT0])
        TT("dve", q7, q5, bim, ALU.mult, Bs2, [fT0])
        TT("dve", BbT[0][:].rearrange("p k n -> p (k n)"), q6, q7, ALU.subtract, [fT0], [BbT[0]])
        TT("dve", q6, q4, bim, ALU.mult, Bs2, [fT0])
        TT("dve", q7, q5, bre, ALU.mult, Bs2, [fT0])
        TT("dve", BbT[1][:].rearrange("p k n -> p (k n)"), q6, q7, ALU.add, [fT0], [BbT[1]])
        CP("dve", CT[0][:].rearrange("p k n -> p (k n)"), cv0[:, 0:2048], [cv0], [CT[0]])
        TS("dve", CT[1][:].rearrange("p k n -> p (k n)"), cv0[:, 2048:4096], -1.0, None, ALU.mult, None, [cv0], [CT[1]])
        for ri in range(2):
            for j in range(2):
                TS("dve", BbTm[ri][j][:], BbT[ri][:], V(V_M0 + j), None, ALU.mult, None, [BbT[ri], vec], [BbTm[ri][j]])

        dump("adacol", adacol, adacol[:], [128, 96]); dump("AA", AA, AA[:], [128, 2, 2, 32]); dump("pg1", pg1, pg1[:], [128, D])
        dump("BbT0", BbT[0], BbT[0][:], [128, 8, 128], BF16); dump("BbT1", BbT[1], BbT[1][:], [128, 8, 128], BF16)
        dump("CT0", CT[0], CT[0][:], [128, 32, 64], BF16); dump("scale1", scale1, scale1[:], [128, 16])
        stop_at("p0")
        barrier()
        xn = carve([128, D], BF16); hT = carve([128, 16, TB], BF16)
        vT = carve([128, 8, 30 + TB]); cv = carve([128, 8, TB])
        sqt = [carve([128, TB]) for i in range(2)]
        meansb = carve([128, TB]); rstdsb = carve([128, TB]); tmpa = [carve([128, TB]) for i in range(3)]
        vn = carve([128, 8, TB], BF16); uT = carve([128, 8, TB], BF16)
        ys = carve([128, 8, TB]); gy = carve([128, 8, TB], BF16)
        bu = carve([128, 2, 32, SBK]); sS = [carve([128, 2, 32, SBK]) for i in range(2)]
        sbf = carve([128, 2, 32, SBK], BF16)
        merged = carve([128, 16, TB], BF16)
        msb = carve([128, D]); h2Tf = carve([128, 16, 128]); h2Tb = carve([128, 16, TB], BF16)
        xt0 = xin[0]

        def rms_rstd(src_ap, srcB, col):
            ACT(xn[:], src_ap, AF.Square, [srcB], [xn, ssq], accum_out=ssq[:, col:col + 1])
            TS("dve", ssq[:, col:col + 1], ssq[:, col:col + 1], 1.0 / D, EPS, ALU.mult, ALU.add, [ssq], [ssq])
            ACT(ssq[:, col:col + 1], ssq[:, col:col + 1], AF.Sqrt, [ssq], [ssq])
            RECIP(ssq[:, col:col + 1], ssq[:, col:col + 1], [ssq], [ssq])

        def norm_T_bf(src_ap, srcB, scale_t, shf, dst):
            rms_rstd(src_ap, srcB, 0)
            ACT(xn[:], src_ap, AF.Identity, [srcB, ssq], [xn], scale=ssq[:, 0:1])
            for k4 in range(4):
                p = nps()
                pb = p[:].bitcast(BF16)
                for kk in range(4):
                    k = k4 * 4 + kk
                    TR(pb[:, kk * 128:(kk + 1) * 128], xn[:, k * 128:(k + 1) * 128], identb[:], [xn, identb], [p])
                for kk in range(4):
                    k = k4 * 4 + kk
                    ACT(dst[:, k, :], pb[:, kk * 128:(kk + 1) * 128], AF.Identity,
                        [p, scale_t, adacol], [dst], scale=scale_t[:, k:k + 1], bias=shf(k))

        def proj(pB_, pout, wb, c0, rhsB, rhs_fn, nk):
            for k in range(nk):
                MM(pout, wb[:, k, c0:c0 + 128], rhs_fn(k), k == 0, k == nk - 1, [wb, rhsB], [pB_])

        def wload(src_ap, nk, ncol=512):
            wb = nwb()
            DMA("pool", wb[:, 0:nk, 0:ncol], src_ap.rearrange("(k p) n -> p k n", p=128), W=[wb])
            return wb

        MEMSET("dve", sS[1][:], 0.0, [sS[1]])
        MEMSET("dve", vT[:], 0.0, [vT])
        sidx = [0]
        NS = TB // SBK

        def s5_scan_block(with_y):
            for sbk in range(NS):
                t0 = sbk * SBK
                for ri in range(2):
                    pp = [nps(), nps()]
                    pvs = [pp_[:, 0:16 * SBK].rearrange("p (q j n) -> p q j n", q=8, j=2) for pp_ in pp]
                    for kt in range(32):
                        q, ktl = kt // 4, kt % 4
                        h2_, j_ = ktl // 2, ktl % 2
                        MM(pvs[h2_][:, q, j_, :], BbTm[ri][j_][64 * h2_:64 * h2_ + 64, q, :], uT[64 * h2_:64 * h2_ + 64, q, t0:t0 + SBK],
                           True, True, [BbTm[ri][j_], uT], [pp[h2_]])
                    buv = bu[:, ri, :, :].rearrange("p (q k) n -> p q k n", k=4)
                    for h2_ in range(2):
                        CP("act", buv[:, :, 2 * h2_:2 * h2_ + 2, :], pvs[h2_], [pp[h2_]], [bu])
                cur = sS[sidx[0] % 2]
                prv = sS[(sidx[0] + 1) % 2]
                sidx[0] += 1
                for t in range(SBK):
                    if t == 0:
                        pa, pBuf = prv[:, :, :, SBK - 1], prv
                    else:
                        pa, pBuf = cur[:, :, :, t - 1], cur
                    TT("dve", Mt[:], AA[:], pa.unsqueeze(1).broadcast_to([128, 2, 2, 32]), ALU.mult, [AA, pBuf], [Mt])
                    TT("dve", Rt[:], Mt[:, :, 0, :], Mt[:, :, 1, :], ALU.add, [Mt], [Rt])
                    TT("dve", cur[:, :, :, t], Rt[:], bu[:, :, :, t], ALU.add, [Rt, bu], [cur])
                if with_y:
                    CP("act", sbf[:], cur[:], [cur], [sbf])
                    p = nps()
                    pv = p[:, 0:8 * SBK].rearrange("p (a n) -> p a n", a=8)
                    for kt in range(32):
                        q, ktl = kt // 4, kt % 4
                        h2_, j_ = ktl // 2, ktl % 2
                        MM(pv[64 * h2_:64 * h2_ + 64, q, :], CT[0][:, kt, :], sbf[:, 0, kt, :], j_ == 0, False, [CT[0], sbf], [p])
                        MM(pv[64 * h2_:64 * h2_ + 64, q, :], CT[1][:, kt, :], sbf[:, 1, kt, :], False, j_ == 1, [CT[1], sbf], [p])
                    for q in range(8):
                        STT(ys[:, q, t0:t0 + SBK], uT[:, q, t0:t0 + SBK], V(V_S5D + q), pv[:, q, :], ALU.mult, ALU.add,
                            [uT, vec, p], [ys])

        def u_proj():
            for half in range(2):
                wb = wload(win[:, 2048 + half * 512:2048 + (half + 1) * 512], 16)
                for jj in range(4):
                    p = nps()
                    proj(p, p[:, 0:TB], wb, jj * 128, hT, lambda k: hT[:, k, :], 16)
                    CP("act", uT[:, half * 4 + jj, :], p[:, 0:TB], [p], [uT])

        def conv_glu_proj():
            for half in range(2):
                wbv = wload(win[:, half * 512:(half + 1) * 512], 16)
                wbg = wload(win[:, 1024 + half * 512:1024 + (half + 1) * 512], 16)
                for jj in range(4):
                    j = half * 4 + jj
                    pv_ = nps()
                    proj(pv_, pv_[:, 0:TB], wbv, jj * 128, hT, lambda k: hT[:, k, :], 16)
                    pg_ = nps()
                    proj(pg_, pg_[:, 0:TB], wbg, jj * 128, hT, lambda k: hT[:, k, :], 16)
                    sg = sqt[j % 2]
                    ACT(sg[:], pg_[:, 0:TB], AF.Sigmoid, [pg_], [sg])
                    TT("dve", vT[:, j, 30:30 + TB], pv_[:, 0:TB], sg[:], ALU.mult, [pv_, sg], [vT])

        for bi in range(NBLK):
            r0 = bi * TB
            DMA("sp", xt0[:], xp[r0:r0 + 128, :], W=[xt0])
            if bi == 0:
                stop_at("bar")
            norm_T_bf(xt0[:], xt0, scale1, sh1, hT)
            if bi == 0:
                stop_at("n0")
            u_proj()
            if bi == 0:
                stop_at("u0")
            s5_scan_block(False)
            if bi == 0:
                stop_at("s0")
            if bi == NBLK - 1:
                conv_glu_proj()
        last = sS[(sidx[0] + 1) % 2]
        TS("dve", last[:, :, :, SBK - 1], last[:, :, :, SBK - 1], flag[:, 0:1], None, ALU.mult, None, [last, flag], [last])
        TS("dve", vT[:, :, 0:30], vT[:, :, TB:TB + 30], flag[:, 0:1], None, ALU.mult, None, [vT, flag], [vT])

        dump("last", last, last[:], [128, 2, 32, SBK]); dump("halo", vT, vT[:], [128, 8, 30 + TB])
        stop_at("p1a")
        for bi in range(NBLK):
            r0 = bi * TB
            DMA("sp", xt0[:], xo[r0:r0 + 128, :], W=[xt0])
            norm_T_bf(xt0[:], xt0, scale1, sh1, hT)
            if bi == 0:
                dump("hT", hT, hT[:], [128, 16, TB], BF16)
            conv_glu_proj()
            pmean = nps(); pmsq = nps()
            for j in range(8):
                acc = cv[:, j, :]
                TS("dve", acc, vT[:, j, 0:TB], V(V_DWW + j * 31), V(V_DWB + j), ALU.mult, ALU.add, [vT, vec], [cv])
                for k in range(1, 31):
                    STT(acc, vT[:, j, k:k + TB], V(V_DWW + j * 31 + k), acc, ALU.mult, ALU.add, [vT, vec, cv], [cv])
                sq = sqt[j % 2]
                ACT(sq[:], acc, AF.Square, [cv], [sq])
                MM(pmean[:, 0:TB], onesm[:], acc, j == 0, j == 7, [onesm, cv], [pmean])
                MM(pmsq[:, 0:TB], onesm[:], sq[:], j == 0, j == 7, [onesm, sq], [pmsq])
            CP("act", meansb[:], pmean[:, 0:TB], [pmean], [meansb])
            TT("dve", rstdsb[:], meansb[:], meansb[:], ALU.mult, [meansb], [rstdsb])
            TT("dve", rstdsb[:], pmsq[:, 0:TB], rstdsb[:], ALU.subtract, [pmsq, rstdsb], [rstdsb])
            TS("dve", rstdsb[:], rstdsb[:], EPS, None, ALU.add, None, [rstdsb], [rstdsb])
            ACT(rstdsb[:], rstdsb[:], AF.Sqrt, [rstdsb], [rstdsb])
            RECIP(rstdsb[:], rstdsb[:], [rstdsb], [rstdsb])
            for j in range(8):
                t_ = tmpa[j % 2]
                TT("dve", t_[:], cv[:, j, :], meansb[:], ALU.subtract, [cv, meansb], [t_])
                TT("dve", t_[:], t_[:], rstdsb[:], ALU.mult, [t_, rstdsb], [t_])
                ACT(vn[:, j, :], t_[:], AF.Silu, [t_, vec], [vn], scale=V(V_CLNG + j), bias=V(V_CLNB + j))
            if bi == 0:
                dump("vT", vT, vT[:], [128, 8, 30 + TB]); dump("cv", cv, cv[:], [128, 8, TB]); dump("vn", vn, vn[:], [128, 8, TB], BF16)
            CP("dve", vT[:, :, 0:30], vT[:, :, TB:TB + 30], [vT], [vT])
            for db in range(4):
                wb = wload(cpw[:, db * 512:(db + 1) * 512], 8)
                wg = wload(win[:, 3072 + db * 512:3072 + (db + 1) * 512], 16)
                for dd in range(4):
                    d = db * 4 + dd
                    py = nps()
                    proj(py, py[:, 0:TB], wb, dd * 128, vn, lambda k: vn[:, k, :], 8)
                    pgt = nps()
                    proj(pgt, pgt[:, 0:TB], wg, dd * 128, hT, lambda k: hT[:, k, :], 16)
                    sg = sqt[d % 2]
                    ACT(sg[:], pgt[:, 0:TB], AF.Sigmoid, [pgt, vec], [sg], bias=V(V_GATEB + d))
                    STT(merged[:, d, :], py[:, 0:TB], V(V_CPB + d), sg[:], ALU.add, ALU.mult, [py, vec, sg], [merged])
            if bi == 0:
                dump("mergedc", merged, merged[:], [128, 16, TB], BF16)
            u_proj()
            s5_scan_block(True)
            for q in range(8):
                t_ = tmpa[q % 2]
                ACT(t_[:], ys[:, q, :], AF.Square, [ys], [t_])
                TS("dve", t_[:], t_[:], 0.044715, 1.0, ALU.mult, ALU.add, [t_], [t_])
                TT("dve", t_[:], t_[:], ys[:, q, :], ALU.mult, [t_, ys], [t_])
                ACT(t_[:], t_[:], AF.Sigmoid, [t_], [t_], scale=1.5957691216057308)
                TT("dve", gy[:, q, :], t_[:], ys[:, q, :], ALU.mult, [t_, ys], [gy])
            for db in range(4):
                wab = nwb()
                DMA("pool", wab[:, 0:8, :], gwa[:, db * 512:(db + 1) * 512].rearrange("(k p) n -> p k n", p=128), W=[wab])
                DMA("pool", wab[:, 8:16, :], gwb[:, db * 512:(db + 1) * 512].rearrange("(k p) n -> p k n", p=128), W=[wab])
                wg = wload(win[:, 5120 + db * 512:5120 + (db + 1) * 512], 16)
                for dd in range(4):
                    d = db * 4 + dd
                    pa_ = nps()
                    for k in range(8):
                        MM(pa_[:, 0:TB], wab[:, k, dd * 128:(dd + 1) * 128], gy[:, k, :], k == 0, k == 7, [wab, gy], [pa_])
                    pb_ = nps()
                    for k in range(8):
                        MM(pb_[:, 0:TB], wab[:, 8 + k, dd * 128:(dd + 1) * 128], gy[:, k, :], k == 0, k == 7, [wab, gy], [pb_])
                    pg2_ = nps()
                    proj(pg2_, pg2_[:, 0:TB], wg, dd * 128, hT, lambda k: hT[:, k, :], 16)
                    t1 = tmpa[0]; t2 = tmpa[1]; t3 = tmpa[2]
                    ACT(t1[:], pb_[:, 0:TB], AF.Sigmoid, [pb_], [t1])
                    TT("dve", t2[:], pa_[:, 0:TB], t1[:], ALU.mult, [pa_, t1], [t2])
                    ACT(t3[:], pg2_[:, 0:TB], AF.Sigmoid, [pg2_, vec], [t3], bias=V(V_GATEB + 16 + d))
                    TT("dve", t2[:], t2[:], t3[:], ALU.mult, [t2, t3], [t2])
                    TT("dve", merged[:, d, :], merged[:, d, :], t2[:], ALU.add, [merged, t2], [merged])
            if bi == 0:
                dump("uT", uT, uT[:], [128, 8, TB], BF16); dump("ys", ys, ys[:], [128, 8, TB]); dump("gy", gy, gy[:], [128, 8, TB], BF16)
                dump("merged", merged, merged[:], [128, 16, TB], BF16)
            for nb in range(4):
                wo = wload(wout[:, nb * 512:(nb + 1) * 512], 16)
                pm = nps()
                for k in range(16):
                    MM(pm[:, :], merged[:, k, :], wo[:, k, :], k == 0, k == 15, [merged, wo], [pm])
                CP("act", msb[:, nb * 512:(nb + 1) * 512], pm[:, :], [pm], [msb])
            if bi == 0:
                dump("m", msb, msb[:], [128, D])
            rms_rstd(msb[:], msb, 1)
            STT(msb[:], msb[:], ssq[:, 1:2], pg1[:], ALU.mult, ALU.mult, [msb, ssq, pg1], [msb])
            DMA("sp", xt0[:], xo[r0:r0 + 128, :], W=[xt0])
            TT("dve", xt0[:], xt0[:], msb[:], ALU.add, [xt0, msb], [xt0])
            DMA("sp", outd[r0:r0 + 128, :], xt0[:], R=[xt0], W=[outdram])
            rms_rstd(xt0[:], xt0, 2)
            ACT(msb[:], xt0[:], AF.Identity, [xt0, ssq], [msb], scale=ssq[:, 2:3])
            for k4 in range(4):
                p = nps()
                for kk in range(4):
                    k = k4 * 4 + kk
                    TR(p[:, kk * 128:(kk + 1) * 128], msb[:, k * 128:(k + 1) * 128], identf[:], [msb, identf], [p])
                for kk in range(4):
                    k = k4 * 4 + kk
                    ACT(h2Tf[:, k, :], p[:, kk * 128:(kk + 1) * 128], AF.Identity, [p, scale2, adacol], [h2Tf],
                        scale=scale2[:, k:k + 1], bias=sh2(k))
            pl = nps()
            for k in range(16):
                MM(pl[:, 0:E], h2Tf[:, k, :], rwf[:, k * E:(k + 1) * E], k == 0, k == 15, [h2Tf, rwf], [pl])
            TT("dve", lg[:], pl[:, 0:E], rbb[:], ALU.add, [pl, rbb], [lg])
            P.op("dve", lambda e: e.max(out=mx8[:], in_=lg[:]), [lg], [mx8])
            TS("dve", gmask[:], lg[:], mx8[:, 3:4], None, ALU.is_ge, None, [lg, mx8], [gmask])
            TS("dve", small[:, 0:1], mx8[:, 0:1], -1.0, None, ALU.mult, None, [mx8], [small])
            ACT(gsm[:], lg[:], AF.Exp, [lg, small], [gsm], bias=small[:, 0:1])
            TT("dve", gsm[:], gsm[:], gmask[:], ALU.mult, [gsm, gmask], [gsm])
            P.op("dve", lambda e: e.tensor_reduce(out=small[:, 1:2], in_=gsm[:], axis=AX.X, op=ALU.add), [gsm], [small])
            RECIP(small[:, 1:2], small[:, 1:2], [small], [small])
            TS("dve", gsm[:], gsm[:], small[:, 1:2], None, ALU.mult, None, [gsm, small], [gsm])
            pt_ = nps()
            TR(pt_[0:E, 0:128], gsm[:], identf[:], [gsm, identf], [pt_])
            CP("act", GT[:, r0:r0 + 128], pt_[0:E, 0:128], [pt_], [GT])
            ACT(xn[:], xt0[:], AF.Identity, [xt0, ssq], [xn], scale=ssq[:, 2:3])
            pc = nps()
            MM(pc[:, 0:E], tri[:], gmask[:], True, True, [tri, gmask], [pc])
            MM(pc[:, E:2 * E], onesf[:], gmask[:], True, True, [onesf, gmask], [pc])
            slot, valid, idxm, rk, selk, tmpk = rt
            TT("dve", slot[:], pc[:, 0:E], basebc[:], ALU.add, [pc, basebc], [slot])
            TT("dve", basebc[:], basebc[:], pc[:, E:2 * E], ALU.add, [pc, basebc], [basebc])
            TS("dve", valid[:], slot[:], float(CAP), None, ALU.is_lt, None, [slot], [valid])
            TT("dve", valid[:], valid[:], gmask[:], ALU.mult, [valid, gmask], [valid])
            TT("dve", idxm[:], slot[:], ecap[:], ALU.add, [slot, ecap], [idxm])
            TS("dve", idxm[:], idxm[:], -BIGI, None, ALU.add, None, [idxm], [idxm])
            P.op("dve", lambda e: e.tensor_tensor_scan(out=rk[:], data0=onesf[:, 0:E], data1=gmask[:], initial=0.0,
                                                       op0=ALU.mult, op1=ALU.add), [onesf, gmask], [rk])
            TT("dve", rk[:], rk[:], gmask[:], ALU.subtract, [rk, gmask], [rk])
            for k in range(4):
                TS("dve", selk[:], rk[:], float(k), None, ALU.is_equal, None, [rk], [selk])
                TT("dve", selk[:], selk[:], valid[:], ALU.mult, [selk, valid], [selk])
                TT("dve", tmpk[:], selk[:], idxm[:], ALU.mult, [selk, idxm], [tmpk])
                P.op("dve", lambda e, k=k, bi=bi: e.tensor_reduce(out=IDXF[:, bi, k:k + 1], in_=tmpk[:], axis=AX.X, op=ALU.add), [tmpk], [IDXF])
                TT("dve", tmpk[:], selk[:], gsm[:], ALU.mult, [selk, gsm], [tmpk])
                P.op("dve", lambda e, k=k, bi=bi: e.tensor_reduce(out=GK[:, bi, k:k + 1], in_=tmpk[:], axis=AX.X, op=ALU.add), [tmpk], [GK])
            TS("dve", IDXF[:, bi, :], IDXF[:, bi, :], BIGI, None, ALU.add, None, [IDXF], [IDXF])
            CP("dve", IDXI[:, bi, :], IDXF[:, bi, :], [IDXF], [IDXI])
            for k in range(4):
                P.dma("pool", lambda e, k=k, bi=bi: e.indirect_dma_start(
                    out=Xgd[:, :], out_offset=bass.IndirectOffsetOnAxis(ap=IDXI[:, bi, k:k + 1], axis=0),
                    in_=xn[:], in_offset=None, bounds_check=breg[0], oob_is_err=False), [xn, IDXI], [xgdram])
            if bi == 0:
                dump("lg", lg, lg[:], [128, E]); dump("gsm", gsm, gsm[:], [128, E])
                dump("idx", IDXI, IDXI[:, 0, :], [128, 4], mybir.dt.int32); dump("gk", GK, GK[:, 0, :], [128, 4])
                stop_at("b0")

        def actb_alias(v):
            b = Buf(v)
            abufs.append(b)
            return b

        stop_at("p1b")
        barrier()
        XgT = carve([128, 16, CAP], BF16); actb = carve([128, 16, CAP], BF16)
        xgs = [carve([128, D], BF16) for i in range(2)]
        ybuf = [carve([128, 512]) for i in range(4)]
        gtt = [carve([128, SBW]) for i in range(2)]; ltt = [carve([128, SBW])]; sgt = [carve([128, SBW])]
        NST = CAP // 128
        yi = [0]
        for e in range(E):
            for st_ in range(NST):
                xg = xgs[st_ % 2]
                DMA("sp", xg[:], Xgd[e * CAP + st_ * 128:e * CAP + (st_ + 1) * 128, :], R=[xgdram], W=[xg])
                for k4 in range(4):
                    p = nps()
                    pb = p[:].bitcast(BF16)
                    for kk in range(4):
                        k = k4 * 4 + kk
                        TR(pb[:, kk * 128:(kk + 1) * 128], xg[:, k * 128:(k + 1) * 128], identb[:], [xg, identb], [p])
                    for kk in range(4):
                        k = k4 * 4 + kk
                        ACT(XgT[:, k, st_ * 128:(st_ + 1) * 128], pb[:, kk * 128:(kk + 1) * 128], AF.Identity,
                            [p, scale2, adacol], [XgT], scale=scale2[:, k:k + 1], bias=sh2(k))
            for fb in range(8):
                wb = nwb()
                DMA("pool", wb[:, :, 0:256], w1g[e][:, fb * 256:(fb + 1) * 256].rearrange("(k p) n -> p k n", p=128), W=[wb])
                DMA("pool", wb[:, :, 256:512], w1l[e][:, fb * 256:(fb + 1) * 256].rearrange("(k p) n -> p k n", p=128), W=[wb])
                for fc in range(2):
                    f = fb * 2 + fc
                    for sb_ in range(CAP // SBW):
                        s0 = sb_ * SBW
                        pg_ = nps()
                        proj(pg_, pg_[:, 0:SBW], wb, fc * 128, XgT, lambda k: XgT[:, k, s0:s0 + SBW], 16)
                        pl_ = nps()
                        proj(pl_, pl_[:, 0:SBW], wb, 256 + fc * 128, XgT, lambda k: XgT[:, k, s0:s0 + SBW], 16)
                        g_ = gtt[(f + sb_) % 2]; l_ = ltt[0]; s_ = sgt[0]
                        TS("dve", g_[:], pg_[:, 0:SBW], V(V_B1G + e * 16 + f), 7.0, ALU.add, ALU.min, [pg_, vec], [g_])
                        ACT(s_[:], g_[:], AF.Sigmoid, [g_], [s_], scale=1.702)
                        TS("dve", l_[:], pl_[:, 0:SBW], V(V_B1L + e * 16 + f), 7.0, ALU.add, ALU.min, [pl_, vec], [l_])
                        TS("dve", l_[:], l_[:], -7.0, 1.0, ALU.max, ALU.add, [l_], [l_])
                        TT("dve", g_[:], g_[:], s_[:], ALU.mult, [g_, s_], [g_])
                        TT("dve", actb[:, f, s0:s0 + SBW], g_[:], l_[:], ALU.mult, [g_, l_], [actb])
            for db in range(4):
                wb = wload(w2d[e][:, db * 512:(db + 1) * 512], 16)
                for st_ in range(NST):
                    py = nps()
                    for k in range(16):
                        MM(py[:, :], actb[:, k, st_ * 128:(st_ + 1) * 128], wb[:, k, :], k == 0, k == 15, [actb, wb], [py])
                    yb = ybuf[yi[0] % 4]
                    yi[0] += 1
                    CP("act", yb[:], py[:, :], [py], [yb])
                    DMA("sp", Ygd[e * CAP + st_ * 128:e * CAP + (st_ + 1) * 128, db * 512:(db + 1) * 512], yb[:], R=[yb], W=[ygdram])
        barrier()
        facc = carve([128, D]); yk = [carve([128, D]) for i in range(2)]; junk = carve([128, D], BF16)
        gi = [0]
        for bi in range(NBLK):
            r0 = bi * TB
            for nb in range(4):
                pbias = nps()
                MM(pbias[:, :], GT[:, r0:r0 + 128], b2s[:, nb * 512:(nb + 1) * 512], True, True, [GT, b2s], [pbias])
                CP("act", facc[:, nb * 512:(nb + 1) * 512], pbias[:, :], [pbias], [facc])
            for k in range(4):
                y_ = yk[gi[0] % 2]
                gi[0] += 1
                MEMSET("pool", y_[:], 0.0, [y_])
                P.dma("pool", lambda e, k=k, bi=bi, y_=y_: e.indirect_dma_start(
                    out=y_[:], out_offset=None, in_=Ygd[:, :],
                    in_offset=bass.IndirectOffsetOnAxis(ap=IDXI[:, bi, k:k + 1], axis=0),
                    bounds_check=breg[0], oob_is_err=False), [IDXI, ygdram], [y_])
                STT(facc[:], y_[:], GK[:, bi, k:k + 1], facc[:], ALU.mult, ALU.add, [y_, GK, facc], [facc])
            ACT(junk[:], facc[:], AF.Square, [facc], [junk, ssq], accum_out=ssq[:, 3:4])
            TS("dve", ssq[:, 3:4], ssq[:, 3:4], 1.0 / D, EPS, ALU.mult, ALU.add, [ssq], [ssq])
            ACT(ssq[:, 3:4], ssq[:, 3:4], AF.Sqrt, [ssq], [ssq])
            RECIP(ssq[:, 3:4], ssq[:, 3:4], [ssq], [ssq])
            STT(facc[:], facc[:], ssq[:, 3:4], pg2[:], ALU.mult, ALU.mult, [facc, ssq, pg2], [facc])
            DMA("sp", xt0[:], outd[r0:r0 + 128, :], R=[outdram], W=[xt0])
            TT("dve", xt0[:], xt0[:], facc[:], ALU.add, [xt0, facc], [xt0])
            DMA("sp", outd[r0:r0 + 128, :], xt0[:], R=[xt0], W=[outdram])

    try:
        body()
    except _Stop:
        pass

    sems = {}
    for k in ("pe", "act", "dve", "pool", "sp"):
        sems[("c", k)] = es.enter_context(nc.semaphore(f"c_{k}"))
    for q in ("sp", "pool"):
        for i in range(Prog.R):
            sems[("d", q, i)] = es.enter_context(nc.semaphore(f"d_{q}{i}"))

    breg = [None]

    def run(name, e):
        if name == "pool":
            breg[0] = e.to_reg(E * CAP - 1)
        for waits, fn, key, amt in P.ops[name]:
            for wk, wv in waits:
                e.wait_ge(sems[wk], wv)
            fn(e).then_inc(sems[key], amt)
        if name == "sp":
            n = P.dq["sp"]
            for i in range(Prog.R):
                cnt = (n - i + Prog.R - 1) // Prog.R
                if cnt > 0:
                    e.wait_ge(sems[("d", "sp", i)], 16 * cnt)

    with es:
        with nc.Block() as block:
            @block.tensor
            def _(e):
                run("pe", e)

            @block.scalar
            def _(e):
                run("act", e)

            @block.vector
            def _(e):
                run("dve", e)

            @block.gpsimd
            def _(e):
                run("pool", e)

            @block.sync
            def _(e):
                run("sp", e)
    return nc


def _prep(inputs):
    f32 = np.float32
    g = {k: np.asarray(v) for k, v in inputs.items()}
    x = g["x"]; c = g["c"]
    sh = {}
    sh["ada_w"] = np.ascontiguousarray(g["ada_w"][0]); sh["ada_b"] = np.ascontiguousarray(g["ada_b"][0][None, :])
    def fm(v, n):
        return np.ascontiguousarray(v.reshape(n, 128).T)
    vec = np.zeros((128, NV), f32)
    vec[:, V_PREMIX:V_PREMIX + 16] = fm(g["pre_mix_g"][0], 16)
    vec[:, V_PREFFN:V_PREFFN + 16] = fm(g["pre_ffn_g"][0], 16)
    vec[:, V_GATEB:V_GATEB + 32] = fm(g["gate_b"][0], 32)
    vec[:, V_DWB:V_DWB + 8] = fm(g["dw_b"][0], 8)
    vec[:, V_CLNG:V_CLNG + 8] = fm(g["cln_g"][0], 8)
    vec[:, V_CLNB:V_CLNB + 8] = fm(g["cln_b"][0], 8)
    vec[:, V_CPB:V_CPB + 16] = fm(g["cp_b"][0], 16)
    vec[:, V_S5D:V_S5D + 8] = fm(g["s5_d"][0], 8)
    dww = g["dw_w"][0]
    vec[:, V_DWW:V_DWW + 248] = dww.T.reshape(8, 128, 31).transpose(1, 0, 2).reshape(128, 248)
    b1 = g["b1"][0]
    vec[:, V_B1G:V_B1G + 512] = b1[:, 0::2].reshape(32, 16, 128).transpose(2, 0, 1).reshape(128, 512)
    vec[:, V_B1L:V_B1L + 512] = b1[:, 1::2].reshape(32, 16, 128).transpose(2, 0, 1).reshape(128, 512)
    vec[:, V_B2:V_B2 + 512] = g["b2"][0].reshape(32, 16, 128).transpose(2, 0, 1).reshape(128, 512)
    pidx = (np.arange(128) // 32) % 2
    vec[:, V_M0] = (pidx == 0).astype(f32); vec[:, V_M0 + 1] = (pidx == 1).astype(f32)
    sh["vec"] = vec
    sh["routerb"] = np.ascontiguousarray(g["router_b"][0][None, :])
    sh["postmix"] = np.ascontiguousarray(g["post_mix_g"][0][None, :])
    sh["postffn"] = np.ascontiguousarray(g["post_ffn_g"][0][None, :])
    are, aim, ldt = g["s5_a_re"][0], g["s5_a_im"][0], g["s5_log_dt"][0]
    ldt2 = np.broadcast_to(ldt[:, None], (64, 64))
    def Sl(a):
        return a.reshape(32, 2, 64).transpose(1, 2, 0).reshape(128, 32)
    sh["aS"] = np.ascontiguousarray(np.concatenate([Sl(are), Sl(aim), Sl(ldt2)], axis=1).astype(f32))
    def Pl(a):
        t = a.reshape(8, 4, 2, 64)
        t = t.transpose(1, 0, 2, 3)
        t = np.broadcast_to(t[:, None, None], (4, 2, 16, 8, 2, 64))
        return t.reshape(128, 1024)
    sh["aP"] = np.ascontiguousarray(np.concatenate([Pl(are), Pl(aim), Pl(ldt2)], axis=1).astype(f32))
    def Bexp(b):
        t = b.reshape(8, 4, 2, 64, 16)
        o = np.zeros((4, 2, 16, 8, 2, 64), f32)
        for gl in range(2):
            o[:, gl, :, :, gl, :] = t[:, :, gl].transpose(1, 3, 0, 2)
        return o.reshape(128, 1024)
    sh["bexp"] = np.concatenate([Bexp(g["s5_b_re"][0]), Bexp(g["s5_b_im"][0])], axis=1)
    def Cexp(cc):
        t = cc.reshape(32, 2, 16, 64)
        o = np.zeros((2, 64, 32, 2, 2, 16), f32)
        for kt in range(32):
            for gl in range(2):
                o[gl, :, kt, kt % 2, gl, :] = t[kt, gl].T
        return o.reshape(128, 2048)
    sh["cexp"] = np.concatenate([Cexp(g["s5_c_re"][0]), Cexp(g["s5_c_im"][0])], axis=1)
    sh["w_in"] = np.ascontiguousarray(g["w_in"][0]); sh["cp_w"] = np.ascontiguousarray(g["cp_w"][0])
    sh["glu_wa"] = np.ascontiguousarray(g["glu_wa"][0]); sh["glu_wb"] = np.ascontiguousarray(g["glu_wb"][0])
    sh["w_out"] = np.ascontiguousarray(g["w_out"][0]); sh["router_w"] = np.ascontiguousarray(g["router_w"][0])
    w1 = g["w1"][0]
    sh["w1g"] = np.ascontiguousarray(w1[:, :, 0::2]); sh["w1l"] = np.ascontiguousarray(w1[:, :, 1::2])
    sh["w2"] = np.ascontiguousarray(g["w2"][0])
    sh["ident"] = np.eye(128, dtype=f32)
    sh["tri"] = np.triu(np.ones((128, 128), f32), 1)
    sh["ecap"] = np.broadcast_to((np.arange(E, dtype=f32) * CAP)[None, :], (128, E)).copy()
    sh["b2s"] = np.ascontiguousarray(g["b2"][0])
    maps = []
    for core in range(8):
        b, half = core // 2, core % 2
        m = dict(sh)
        m["xo"] = np.ascontiguousarray(x[b, half * NTOK:(half + 1) * NTOK])
        m["xp"] = np.ascontiguousarray(x[b, 0:NTOK]) if half == 1 else np.zeros((NTOK, D), f32)
        m["flag"] = np.full((128, 1), float(half), f32)
        m["ccol"] = fm(c[b], 16)
        maps.append(m)
    return maps


def kernel(**inputs):
    maps = _prep(inputs)
    nc = build_nc()
    res = run_bass_kernel_spmd(nc, maps, core_ids=list(range(8)))
    out = np.zeros((4, 4096, D), np.float32)
    for core in range(8):
        b, half = core // 2, core % 2
        out[b, half * NTOK:(half + 1) * NTOK] = res.results[core]["out"]
    return out
```

```python
import math
from contextlib import ExitStack
import numpy as np
import concourse.bass as bass
import concourse.mybir as mybir
from concourse.bass_utils import run_bass_kernel_spmd

F32 = mybir.dt.float32
BF16 = mybir.dt.bfloat16
AF = mybir.ActivationFunctionType
ALU = mybir.AluOpType
AX = mybir.AxisListType

D = 2048
NTOK = 2048
TB = 256
NT = NTOK // 128
SBK = 16
NBLK = NTOK // TB
E = 32
MTB = 512
CAP = 768
SBW = 384
BIGI = 4000000.0
EPS = 1e-6

V_PREMIX, V_PREFFN, V_GATEB, V_DWB, V_CLNG, V_CLNB, V_CPB, V_S5D = 0, 16, 32, 64, 72, 80, 88, 104
V_DWW = 112
V_B1G = V_DWW + 8 * 31
V_B1L = V_B1G + 512
V_B2 = V_B1L + 512
V_M0 = V_B2 + 512
NV = V_M0 + 2


class Buf:

    def __init__(self, t):
        self.t = t
        self.w = None
        self.r = {}

    def __getitem__(self, idx):
        return self.t[idx]


class Alias(Buf):
    def __init__(self, parent, t):
        self.p = parent
        self.t = t

    @property
    def w(self):
        return self.p.w

    @w.setter
    def w(self, v):
        self.p.w = v

    @property
    def r(self):
        return self.p.r

    @r.setter
    def r(self, v):
        self.p.r = v


class Prog:
    R = 8

    def __init__(self):
        self.ops = {k: [] for k in ("pe", "act", "dve", "pool", "sp")}
        self.cnt = {k: 0 for k in self.ops}
        self.known = {k: {} for k in self.ops}
        self.dq = {"sp": 0, "pool": 0}

    def _emit(self, eng, fn, R, W, key, amt, val, extra=()):
        need = {}

        def add(tok):
            if tok is None:
                return
            k, v = tok
            if need.get(k, 0) < v:
                need[k] = v

        for b in R:
            add(b.w)
        for b in W:
            add(b.w)
            for k, v in b.r.items():
                add((k, v))
        for t in extra:
            add(t)
        waits = []
        for k, v in need.items():
            if k == ("c", "pe") and eng == "pe":
                continue
            if self.known[eng].get(k, 0) >= v:
                continue
            self.known[eng][k] = v
            waits.append((k, v))
        self.ops[eng].append((waits, fn, key, amt))
        tok = (key, val)
        for b in W:
            b.w = tok
            b.r = {}
        for b in R:
            if b not in W and b.r.get(key, 0) < val:
                b.r[key] = val
        return tok

    def op(self, eng, fn, R=(), W=()):
        self.cnt[eng] += 1
        return self._emit(eng, fn, R, W, ("c", eng), 1, self.cnt[eng])

    def dma(self, q, fn, R=(), W=()):
        j = self.dq[q]
        self.dq[q] += 1
        key = ("d", q, j % self.R)
        extra = []
        if j >= self.R:
            extra.append((key, 16 * (j // self.R)))
        return self._emit(q, fn, R, W, key, 16, 16 * (j // self.R + 1), extra)


class _Stop(Exception):
    pass


def build_nc(stop=None, dumps=()):
    nc = bass.Bass("TRN2", target_bir_lowering=False)
    P = Prog()
    dbg = {}

    def dump(name, B, ap, shape, dt=F32):
        if name not in dumps:
            return
        t = nc.dram_tensor("dbg_" + name, list(shape), dt, kind="ExternalOutput").ap()
        P.dma("sp", lambda e: e.dma_start(out=t, in_=ap), [B], ())

    def stop_at(tag):
        if stop == tag:
            raise _Stop()

    def din(name, shape, dt=F32):
        return nc.dram_tensor(name, list(shape), dt, kind="ExternalInput").ap()

    xo = din("xo", [NTOK, D]); xp = din("xp", [NTOK, D]); flagd = din("flag", [128, 1])
    ccold = din("ccol", [128, 16]); adaw = din("ada_w", [D, 6 * D]); adab = din("ada_b", [1, 6 * D])
    vecd = din("vec", [128, NV]); routerbd = din("routerb", [1, E])
    postmixd = din("postmix", [1, D]); postffnd = din("postffn", [1, D])
    aSd = din("aS", [128, 96]); aPd = din("aP", [128, 3072])
    bexpd = din("bexp", [128, 2048]); cexpd = din("cexp", [128, 4096])
    win = din("w_in", [D, 7168]); cpw = din("cp_w", [1024, D]); gwa = din("glu_wa", [1024, D])
    gwb = din("glu_wb", [1024, D]); wout = din("w_out", [D, D]); rwd = din("router_w", [D, E])
    w1g = din("w1g", [E, D, 2048]); w1l = din("w1l", [E, D, 2048]); w2d = din("w2", [E, 2048, D])
    identd = din("ident", [128, 128]); trid = din("tri", [128, 128]); ecapd = din("ecap", [128, E]); b2d = din("b2s", [E, D])
    outd = nc.dram_tensor("out", [NTOK, D], F32, kind="ExternalOutput").ap()
    Xgd = nc.dram_tensor("xg_scratch", [E * CAP, D], BF16, kind="Internal").ap()
    Ygd = nc.dram_tensor("yg_scratch", [E * CAP, D], F32, kind="Internal").ap()

    es = ExitStack()
    h2dram = Buf(None); outdram = Buf(None); xgdram = Buf(None); ygdram = Buf(None)

    def sb(name, shape, dt=F32):
        return Buf(es.enter_context(nc.sbuf_tensor(name, list(shape), dt)))

    ps = [Buf(es.enter_context(nc.psum_tensor(f"ps{i}", [128, 512], F32))) for i in range(8)]
    psi = [0]

    def nps():
        p = ps[psi[0] % 8]
        psi[0] += 1
        return p

    def DMA(q, out, in_, R=(), W=()):
        P.dma(q, lambda e: e.dma_start(out=out, in_=in_), R, W)

    def ACT(out, in_, func, R, W, **kw):
        P.op("act", lambda e: e.activation(out=out, in_=in_, func=func, **kw), R, W)

    def TT(eng, out, a, b, op, R, W):
        P.op(eng, lambda e: e.tensor_tensor(out=out, in0=a, in1=b, op=op), R, W)

    def TS(eng, out, a, s1, s2, op0, op1, R, W):
        if op1 is None:
            P.op(eng, lambda e: e.tensor_scalar(out=out, in0=a, scalar1=s1, scalar2=None, op0=op0), R, W)
        else:
            P.op(eng, lambda e: e.tensor_scalar(out=out, in0=a, scalar1=s1, scalar2=s2, op0=op0, op1=op1), R, W)

    def STT(out, a, s, b, op0, op1, R, W):
        P.op("dve", lambda e: e.scalar_tensor_tensor(out=out, in0=a, scalar=s, in1=b, op0=op0, op1=op1), R, W)

    def MM(out, l, r, st, sp_, R, W):
        P.op("pe", lambda e: e.matmul(out, l, r, start=st, stop=sp_), R, W)

    def TR(out, in_, ident, R, W):
        P.op("pe", lambda e: e.transpose(out, in_, ident), R, W)

    def CP(eng, out, in_, R, W):
        if eng == "act":
            P.op(eng, lambda e: e.activation(out=out, in_=in_, func=AF.Identity), R, W)
        else:
            P.op(eng, lambda e: e.tensor_copy(out=out, in_=in_), R, W)

    def RECIP(out, in_, R, W):
        P.op("dve", lambda e: e.reciprocal(out=out, in_=in_), R, W)

    def MEMSET(eng, ap, c, W):
        P.op(eng, lambda e: e.memset(ap, c), (), W)

    identf = sb("identf", [128, 128]); identb = sb("identb", [128, 128], BF16)
    onesf = sb("onesf", [128, 128]); onesm = sb("onesm", [128, 128])
    vec = sb("vecs", [128, NV]); flag = sb("flagt", [128, 1])
    ccol = sb("ccolt", [128, 16]); silub = sb("silub", [128, 16], BF16)
    adacol = sb("adacol", [128, 96]); scale1 = sb("scale1", [128, 16]); scale2 = sb("scale2", [128, 16])
    pg1 = sb("pg1", [128, D]); pg2 = sb("pg2", [128, D])
    rbb = sb("rbb", [128, E]); rwf = sb("rwf", [128, 16 * E])
    wblk = [sb(f"wblk{i}", [128, 16, 512], BF16) for i in range(2)]
    wbi = [0]

    def nwb():
        w = wblk[wbi[0] % 2]
        wbi[0] += 1
        return w

    xin = [sb("xin0", [128, D])]
    ssq = sb("ssq", [128, 4]); small = sb("small", [128, 16])
    Mt = sb("Mt", [128, 2, 2, 32]); Rt = sb("Rt", [128, 2, 32]); AA = sb("AA", [128, 2, 2, 32])
    lg = sb("lg", [128, E]); mx8 = sb("mx8", [128, 8]); gsm = sb("gsm", [128, E]); gmask = sb("gmask", [128, E])
    GT = sb("GT", [32, NTOK])
    tri = sb("tri_t", [128, 128]); ecap = sb("ecap_t", [128, E]); basebc = sb("basebc", [128, E]); b2s = sb("b2s_t", [E, D])
    IDXF = sb("IDXF", [128, NT, 4]); GK = sb("GK", [128, NT, 4]); IDXI = sb("IDXI", [128, NT, 4], mybir.dt.int32)
    rt = [sb(f"rt{i}", [128, E]) for i in range(6)]
    BbTm = [[sb(f"BbTm{i}{j}", [128, 8, 128], BF16) for j in range(2)] for i in range(2)]
    CT = [sb(f"CT{i}", [128, 32, 64], BF16) for i in range(2)]
    aS = sb("aSt", [128, 96]); stmp = sb("stmp", [128, 12 * 32])
    bars = {k: sb(f"bar_{k}", [128, 4]) for k in ("dve", "act", "pool")}
    AW = 19456
    arena = es.enter_context(nc.sbuf_tensor("arena", [128, AW], F32))
    apos = [0]
    abufs = []

    def carve(shape, dt=F32):
        n = 1
        for d_ in shape[1:]:
            n *= d_
        words = n if dt == F32 else (n + 1) // 2
        o = apos[0]
        apos[0] += words
        assert apos[0] <= AW, ("arena overflow", apos[0])
        v = arena[:, o:o + words]
        if dt != F32:
            v = v.bitcast(dt)
        if len(shape) == 3:
            v = v.rearrange("p (a b) -> p a b", a=shape[1])
        elif len(shape) == 4:
            v = v.rearrange("p (a b c) -> p a b c", a=shape[1], b=shape[2])
        b = Buf(v)
        abufs.append(b)
        return b

    def barrier():
        W0 = list(abufs)
        P.op("dve", lambda e: e.memset(bars["dve"][:], 0.0), (), [bars["dve"]] + W0)
        P.op("pool", lambda e: e.memset(bars["pool"][:], 0.0), (), [bars["pool"]])
        P.op("act", lambda e: e.activation(out=bars["act"][:], in_=flag[:, 0:1].broadcast_to([128, 4]), func=AF.Identity), [flag], [bars["act"]])
        pb_ = nps()
        MM(pb_[0:1, 0:2], onesf[0:1, 0:1], onesf[0:1, 0:2], True, True, [onesf], [pb_])
        allb = [bars["dve"], bars["pool"], bars["act"], pb_]
        P.op("dve", lambda e: e.memset(small[:, 8:9], 0.0), allb, [small])
        P.op("pool", lambda e: e.memset(small[:, 9:10], 0.0), allb + [small], [small])
        P.op("act", lambda e: e.activation(out=small[:, 10:11], in_=flag[:, 0:1], func=AF.Identity), allb + [small], [small])
        pb2 = nps()
        MM(pb2[0:1, 0:2], onesf[0:1, 0:1], onesf[0:1, 0:2], True, True, [onesf, small] + allb, [pb2])
        DMA("sp", flag[:], flagd, R=[small, pb2], W=[flag])
        DMA("pool", ssq[:, 3:4], flagd, R=[small, pb2], W=[ssq])
        del abufs[:]
        apos[0] = 0

    rowb = carve([1 + 127, 512]); adabk = carve([128, 512])
    fT0 = carve([128, 8, 1024]); merged0 = carve([128, 3072]); ys0 = carve([128, 2048]); cv0 = carve([128, 4096])
    BbT = [carve([128, 8, 128], BF16) for i in range(2)]

    def V(off, n=1):
        return vec[:, off:off + n]

    def body():
        DMA("sp", identf[:], identd, W=[identf])
        DMA("sp", vec[:], vecd, W=[vec])
        DMA("sp", flag[:], flagd, W=[flag])
        DMA("sp", ccol[:], ccold, W=[ccol])
        DMA("sp", rwf[:].rearrange("p (k n) -> p k n", k=16), rwd.rearrange("(k p) n -> p k n", p=128), W=[rwf])
        DMA("sp", rbb[:], routerbd.broadcast_to([128, E]), W=[rbb])
        DMA("sp", pg1[:], postmixd.broadcast_to([128, D]), W=[pg1])
        DMA("sp", pg2[:], postffnd.broadcast_to([128, D]), W=[pg2])
        DMA("sp", aS[:], aSd, W=[aS])
        DMA("sp", tri[:], trid, W=[tri])
        DMA("sp", ecap[:], ecapd, W=[ecap])
        DMA("sp", b2s[:], b2d, W=[b2s])
        MEMSET("dve", basebc[:], 0.0, [basebc])
        CP("dve", identb[:], identf[:], [identf], [identb])
        MEMSET("dve", onesf[:], 1.0, [onesf])
        MEMSET("dve", onesm[:], 1.0 / 1024.0, [onesm])
        ACT(silub[:], ccol[:], AF.Silu, [ccol], [silub])

        for cb in range(24):
            wb = nwb()
            DMA("pool", wb[:], adaw[:, cb * 512:(cb + 1) * 512].rearrange("(k p) n -> p k n", p=128), W=[wb])
            DMA("sp", adabk[0:1, :], adab[0:1, cb * 512:(cb + 1) * 512], W=[adabk])
            p = nps()
            for k in range(16):
                MM(p[0:1, :], silub[:, k:k + 1], wb[:, k, :], k == 0, k == 15, [silub, wb], [p])
            TT("dve", rowb[0:1, :], p[0:1, :], adabk[0:1, :], ALU.add, [p, adabk], [rowb])
            p2 = nps()
            for j in range(4):
                MM(p2[:, j:j + 1], rowb[0:1, j * 128:(j + 1) * 128], onesf[0:1, 0:1], True, True, [rowb, onesf], [p2])
            CP("act", adacol[:, cb * 4:cb * 4 + 4], p2[:, 0:4], [p2], [adacol])
            if 8 <= cb < 12 or 20 <= cb < 24:
                pgx, c0 = (pg1, (cb - 8) * 512) if cb < 12 else (pg2, (cb - 20) * 512)
                p3 = nps()
                MM(p3[:, :], onesf[0:1, :], rowb[0:1, :], True, True, [rowb, onesf], [p3])
                TT("dve", pgx[:, c0:c0 + 512], pgx[:, c0:c0 + 512], p3[:, :], ALU.mult, [pgx, p3], [pgx])
        TS("dve", small[:, 0:16], adacol[:, 16:32], 1.0, None, ALU.add, None, [adacol], [small])
        TT("dve", scale1[:], small[:, 0:16], V(V_PREMIX, 16), ALU.mult, [small, vec], [scale1])
        TS("dve", small[:, 0:16], adacol[:, 64:80], 1.0, None, ALU.add, None, [adacol], [small])
        TT("dve", scale2[:], small[:, 0:16], V(V_PREFFN, 16), ALU.mult, [small, vec], [scale2])
        sh1 = lambda k: adacol[:, k:k + 1]
        sh2 = lambda k: adacol[:, 48 + k:49 + k]

        def abar(aR, aI, lD, t, Bs, B):
            dt_, x_, rho, th, c_, s_, t1, t2 = t
            ACT(dt_, lD, AF.Exp, Bs, [B])
            TT("dve", x_, aR, dt_, ALU.mult, Bs, [B])
            ACT(rho, x_, AF.Exp, [B], [B])
            TT("dve", th, aI, dt_, ALU.mult, Bs, [B])
            ACT(s_, th, AF.Sin, [B], [B], scale=1.0 / 32.0)
            TS("dve", x_, th, 1.0 / 32.0, math.pi / 2, ALU.mult, ALU.add, [B], [B])
            ACT(c_, x_, AF.Sin, [B], [B])
            for _ in range(5):
                TT("dve", t1, c_, c_, ALU.mult, [B], [B])
                TT("dve", t2, s_, s_, ALU.mult, [B], [B])
                TT("dve", x_, c_, s_, ALU.mult, [B], [B])
                TT("dve", c_, t1, t2, ALU.subtract, [B], [B])
                TS("dve", s_, x_, 2.0, None, ALU.mult, None, [B], [B])
            TT("dve", dt_, rho, c_, ALU.mult, [B], [B])
            TT("dve", x_, rho, s_, ALU.mult, [B], [B])
            return dt_, x_

        st = [stmp[:, i * 32:(i + 1) * 32] for i in range(8)]
        abr, abi = abar(aS[:, 0:32], aS[:, 32:64], aS[:, 64:96], st, [stmp, aS], stmp)
        CP("dve", AA[:, 0, 0, :], abr, [stmp], [AA])
        CP("dve", AA[:, 1, 1, :], abr, [stmp], [AA])
        CP("dve", AA[:, 1, 0, :], abi, [stmp], [AA])
        TS("dve", AA[:, 0, 1, :], abi, -1.0, None, ALU.mult, None, [stmp], [AA])
        pt = [fT0[:, i, :] for i in range(8)]
        DMA("sp", merged0[:], aPd, W=[merged0])
        DMA("sp", ys0[:], bexpd, W=[ys0])
        DMA("sp", cv0[:], cexpd, W=[cv0])
        aRp, aIp, lDp = merged0[:, 0:1024], merged0[:, 1024:2048], merged0[:, 2048:3072]
        abrP, abiP = abar(aRp, aIp, lDp, pt, [fT0, merged0], fT0)
        _, _, q2, q3, q4, q5, q6, q7 = pt
        Bs = [fT0, merged0]
        TT("dve", q2, aRp, aRp, ALU.mult, Bs, [fT0])
        TT("dve", q3, aIp, aIp, ALU.mult, Bs, [fT0])
        TT("dve", q2, q2, q3, ALU.add, [fT0], [fT0])
        RECIP(q2, q2, [fT0], [fT0])
        TS("dve", q3, abrP, -1.0, None, ALU.add, None, [fT0], [fT0])
        TT("dve", q4, q3, aRp, ALU.mult, Bs, [fT0])
        TT("dve", q5, abiP, aIp, ALU.mult, Bs, [fT0])
        TT("dve", q4, q4, q5, ALU.add, [fT0], [fT0])
        TT("dve", q4, q4, q2, ALU.mult, [fT0], [fT0])
        TT("dve", q5, abiP, aRp, ALU.mult, Bs, [fT0])
        TT("dve", q6, q3, aIp, ALU.mult, Bs, [fT0])
        TT("dve", q5, q5, q6, ALU.subtract, [fT0], [fT0])
        TT("dve", q5, q5, q2, ALU.mult, [fT0], [fT0])
        bre, bim = ys0[:, 0:1024], ys0[:, 1024:2048]
        Bs2 = [fT0, ys0]
        TT("dve", q6, q4, bre, ALU.mult, Bs2, [fT0])
        TT("dve", q7, q5, bim, ALU.mult, Bs2, [fT0])
        TT("dve", BbT[0][:].rearrange("p k n -> p (k n)"), q6, q7, ALU.subtract, [fT0], [BbT[0]])
        TT("dve", q6, q4, bim, ALU.mult, Bs2, [fT0])
        TT("dve", q7, q5, bre, ALU.mult, Bs2, [fT0])
        TT("dve", BbT[1][:].rearrange("p k n -> p (k n)"), q6, q7, ALU.add, [fT0], [BbT[1]])
        CP("dve", CT[0][:].rearrange("p k n -> p (k n)"), cv0[:, 0:2048], [cv0], [CT[0]])
        TS("dve", CT[1][:].rearrange("p k n -> p (k n)"), cv0[:, 2048:4096], -1.0, None, ALU.mult, None, [cv0], [CT[1]])
        for ri in range(2):
            for j in range(2):
                TS("dve", BbTm[ri][j][:], BbT[ri][:], V(V_M0 + j), None, ALU.mult, None, [BbT[ri], vec], [BbTm[ri][j]])

        dump("adacol", adacol, adacol[:], [128, 96]); dump("AA", AA, AA[:], [128, 2, 2, 32]); dump("pg1", pg1, pg1[:], [128, D])
        dump("BbT0", BbT[0], BbT[0][:], [128, 8, 128], BF16); dump("BbT1", BbT[1], BbT[1][:], [128, 8, 128], BF16)
        dump("CT0", CT[0], CT[0][:], [128, 32, 64], BF16); dump("scale1", scale1, scale1[:], [128, 16])
        stop_at("p0")
        barrier()
        xn = carve([128, D], BF16); hT = carve([128, 16, TB], BF16)
        vT = carve([128, 8, 30 + TB]); cv = carve([128, 8, TB])
        sqt = [carve([128, TB]) for i in range(2)]
        meansb = carve([128, TB]); rstdsb = carve([128, TB]); tmpa = [carve([128, TB]) for i in range(3)]
        vn = carve([128, 8, TB], BF16); uT = carve([128, 8, TB], BF16)
        ys = Alias(cv, cv.t); gy = carve([128, 8, TB], BF16)
        bu = carve([128, 2, 32, SBK]); sS = [carve([128, 2, 32, SBK]) for i in range(2)]
        sbf = carve([128, 2, 32, SBK], BF16)
        merged = carve([128, 16, TB], BF16)
        msb = Alias(cv, cv.t.rearrange("p a b -> p (a b)"))
        h2Tf = Alias(hT, hT.t.rearrange("p a b -> p (a b)").bitcast(F32).rearrange("p (a b) -> p a b", a=16))
        xt0 = xin[0]

        def rms_rstd(src_ap, srcB, col):
            ACT(xn[:], src_ap, AF.Square, [srcB], [xn, ssq], accum_out=ssq[:, col:col + 1])
            TS("dve", ssq[:, col:col + 1], ssq[:, col:col + 1], 1.0 / D, EPS, ALU.mult, ALU.add, [ssq], [ssq])
            ACT(ssq[:, col:col + 1], ssq[:, col:col + 1], AF.Sqrt, [ssq], [ssq])
            RECIP(ssq[:, col:col + 1], ssq[:, col:col + 1], [ssq], [ssq])

        def norm_T_bf(src_ap, srcB, scale_t, shf, dst, tt):
            rms_rstd(src_ap, srcB, 0)
            ACT(xn[:], src_ap, AF.Identity, [srcB, ssq], [xn], scale=ssq[:, 0:1])
            for k4 in range(4):
                p = nps()
                pb = p[:].bitcast(BF16)
                for kk in range(4):
                    k = k4 * 4 + kk
                    TR(pb[:, kk * 128:(kk + 1) * 128], xn[:, k * 128:(k + 1) * 128], identb[:], [xn, identb], [p])
                for kk in range(4):
                    k = k4 * 4 + kk
                    ACT(dst[:, k, tt * 128:(tt + 1) * 128], pb[:, kk * 128:(kk + 1) * 128], AF.Identity,
                        [p, scale_t, adacol], [dst], scale=scale_t[:, k:k + 1], bias=shf(k))

        def proj(pB_, pout, wb, c0, rhsB, rhs_fn, nk):
            for k in range(nk):
                MM(pout, wb[:, k, c0:c0 + 128], rhs_fn(k), k == 0, k == nk - 1, [wb, rhsB], [pB_])

        def wload(src_ap, nk, ncol=512):
            wb = nwb()
            DMA("pool", wb[:, 0:nk, 0:ncol], src_ap.rearrange("(k p) n -> p k n", p=128), W=[wb])
            return wb

        MEMSET("dve", sS[1][:], 0.0, [sS[1]])
        MEMSET("dve", vT[:], 0.0, [vT])
        sidx = [0]
        NS = TB // SBK

        def s5_scan_block(with_y):
            for sbk in range(NS):
                t0 = sbk * SBK
                for ri in range(2):
                    pp = [nps(), nps()]
                    pvs = [pp_[:, 0:16 * SBK].rearrange("p (q j n) -> p q j n", q=8, j=2) for pp_ in pp]
                    for kt in range(32):
                        q, ktl = kt // 4, kt % 4
                        h2_, j_ = ktl // 2, ktl % 2
                        MM(pvs[h2_][:, q, j_, :], BbTm[ri][j_][64 * h2_:64 * h2_ + 64, q, :], uT[64 * h2_:64 * h2_ + 64, q, t0:t0 + SBK],
                           True, True, [BbTm[ri][j_], uT], [pp[h2_]])
                    buv = bu[:, ri, :, :].rearrange("p (q k) n -> p q k n", k=4)
                    for h2_ in range(2):
                        CP("act", buv[:, :, 2 * h2_:2 * h2_ + 2, :], pvs[h2_], [pp[h2_]], [bu])
                cur = sS[sidx[0] % 2]
                prv = sS[(sidx[0] + 1) % 2]
                sidx[0] += 1
                for t in range(SBK):
                    if t == 0:
                        pa, pBuf = prv[:, :, :, SBK - 1], prv
                    else:
                        pa, pBuf = cur[:, :, :, t - 1], cur
                    TT("dve", Mt[:], AA[:], pa.unsqueeze(1).broadcast_to([128, 2, 2, 32]), ALU.mult, [AA, pBuf], [Mt])
                    TT("dve", Rt[:], Mt[:, :, 0, :], Mt[:, :, 1, :], ALU.add, [Mt], [Rt])
                    TT("dve", cur[:, :, :, t], Rt[:], bu[:, :, :, t], ALU.add, [Rt, bu], [cur])
                if with_y:
                    CP("act", sbf[:], cur[:], [cur], [sbf])
                    p = nps()
                    pv = p[:, 0:8 * SBK].rearrange("p (a n) -> p a n", a=8)
                    for kt in range(32):
                        q, ktl = kt // 4, kt % 4
                        h2_, j_ = ktl // 2, ktl % 2
                        MM(pv[64 * h2_:64 * h2_ + 64, q, :], CT[0][:, kt, :], sbf[:, 0, kt, :], j_ == 0, False, [CT[0], sbf], [p])
                        MM(pv[64 * h2_:64 * h2_ + 64, q, :], CT[1][:, kt, :], sbf[:, 1, kt, :], False, j_ == 1, [CT[1], sbf], [p])
                    for q in range(8):
                        STT(ys[:, q, t0:t0 + SBK], uT[:, q, t0:t0 + SBK], V(V_S5D + q), pv[:, q, :], ALU.mult, ALU.add,
                            [uT, vec, p], [ys])

        def u_proj():
            for half in range(2):
                wb = wload(win[:, 2048 + half * 512:2048 + (half + 1) * 512], 16)
                for jj in range(4):
                    p = nps()
                    proj(p, p[:, 0:TB], wb, jj * 128, hT, lambda k: hT[:, k, :], 16)
                    CP("act", uT[:, half * 4 + jj, :], p[:, 0:TB], [p], [uT])

        def conv_glu_proj():
            for half in range(2):
                wbv = wload(win[:, half * 512:(half + 1) * 512], 16)
                wbg = wload(win[:, 1024 + half * 512:1024 + (half + 1) * 512], 16)
                for jj in range(4):
                    j = half * 4 + jj
                    pv_ = nps()
                    proj(pv_, pv_[:, 0:TB], wbv, jj * 128, hT, lambda k: hT[:, k, :], 16)
                    pg_ = nps()
                    proj(pg_, pg_[:, 0:TB], wbg, jj * 128, hT, lambda k: hT[:, k, :], 16)
                    sg = sqt[j % 2]
                    ACT(sg[:], pg_[:, 0:TB], AF.Sigmoid, [pg_], [sg])
                    TT("dve", vT[:, j, 30:30 + TB], pv_[:, 0:TB], sg[:], ALU.mult, [pv_, sg], [vT])

        for bi in range(NBLK):
            for tt in range(TB // 128):
                r0 = bi * TB + tt * 128
                DMA("sp", xt0[:], xp[r0:r0 + 128, :], W=[xt0])
                norm_T_bf(xt0[:], xt0, scale1, sh1, hT, tt)
            if bi == 0:
                stop_at("n0")
            u_proj()
            if bi == 0:
                stop_at("u0")
            s5_scan_block(False)
            if bi == 0:
                stop_at("s0")
            if bi == NBLK - 1:
                conv_glu_proj()
        last = sS[(sidx[0] + 1) % 2]
        TS("dve", last[:, :, :, SBK - 1], last[:, :, :, SBK - 1], flag[:, 0:1], None, ALU.mult, None, [last, flag], [last])
        TS("dve", vT[:, :, 0:30], vT[:, :, TB:TB + 30], flag[:, 0:1], None, ALU.mult, None, [vT, flag], [vT])

        dump("last", last, last[:], [128, 2, 32, SBK]); dump("halo", vT, vT[:], [128, 8, 30 + TB])
        stop_at("p1a")
        for bi in range(NBLK):
            for tt in range(TB // 128):
                r0 = bi * TB + tt * 128
                DMA("sp", xt0[:], xo[r0:r0 + 128, :], W=[xt0])
                norm_T_bf(xt0[:], xt0, scale1, sh1, hT, tt)
            if bi == 0:
                dump("hT", hT, hT[:], [128, 16, TB], BF16)
            conv_glu_proj()
            pmean = nps(); pmsq = nps()
            for j in range(8):
                acc = cv[:, j, :]
                TS("dve", acc, vT[:, j, 0:TB], V(V_DWW + j * 31), V(V_DWB + j), ALU.mult, ALU.add, [vT, vec], [cv])
                for k in range(1, 31):
                    STT(acc, vT[:, j, k:k + TB], V(V_DWW + j * 31 + k), acc, ALU.mult, ALU.add, [vT, vec, cv], [cv])
                sq = sqt[j % 2]
                ACT(sq[:], acc, AF.Square, [cv], [sq])
                MM(pmean[:, 0:TB], onesm[:], acc, j == 0, j == 7, [onesm, cv], [pmean])
                MM(pmsq[:, 0:TB], onesm[:], sq[:], j == 0, j == 7, [onesm, sq], [pmsq])
            CP("act", meansb[:], pmean[:, 0:TB], [pmean], [meansb])
            TT("dve", rstdsb[:], meansb[:], meansb[:], ALU.mult, [meansb], [rstdsb])
            TT("dve", rstdsb[:], pmsq[:, 0:TB], rstdsb[:], ALU.subtract, [pmsq, rstdsb], [rstdsb])
            TS("dve", rstdsb[:], rstdsb[:], EPS, None, ALU.add, None, [rstdsb], [rstdsb])
            ACT(rstdsb[:], rstdsb[:], AF.Sqrt, [rstdsb], [rstdsb])
            RECIP(rstdsb[:], rstdsb[:], [rstdsb], [rstdsb])
            for j in range(8):
                t_ = tmpa[j % 2]
                TT("dve", t_[:], cv[:, j, :], meansb[:], ALU.subtract, [cv, meansb], [t_])
                TT("dve", t_[:], t_[:], rstdsb[:], ALU.mult, [t_, rstdsb], [t_])
                ACT(vn[:, j, :], t_[:], AF.Silu, [t_, vec], [vn], scale=V(V_CLNG + j), bias=V(V_CLNB + j))
            if bi == 0:
                dump("vT", vT, vT[:], [128, 8, 30 + TB]); dump("cv", cv, cv[:], [128, 8, TB]); dump("vn", vn, vn[:], [128, 8, TB], BF16)
            CP("dve", vT[:, :, 0:30], vT[:, :, TB:TB + 30], [vT], [vT])
            for db in range(4):
                wb = wload(cpw[:, db * 512:(db + 1) * 512], 8)
                wg = wload(win[:, 3072 + db * 512:3072 + (db + 1) * 512], 16)
                for dd in range(4):
                    d = db * 4 + dd
                    py = nps()
                    proj(py, py[:, 0:TB], wb, dd * 128, vn, lambda k: vn[:, k, :], 8)
                    pgt = nps()
                    proj(pgt, pgt[:, 0:TB], wg, dd * 128, hT, lambda k: hT[:, k, :], 16)
                    sg = sqt[d % 2]
                    ACT(sg[:], pgt[:, 0:TB], AF.Sigmoid, [pgt, vec], [sg], bias=V(V_GATEB + d))
                    STT(merged[:, d, :], py[:, 0:TB], V(V_CPB + d), sg[:], ALU.add, ALU.mult, [py, vec, sg], [merged])
            if bi == 0:
                dump("mergedc", merged, merged[:], [128, 16, TB], BF16)
            u_proj()
            s5_scan_block(True)
            for q in range(8):
                t_ = tmpa[q % 2]
                ACT(t_[:], ys[:, q, :], AF.Square, [ys], [t_])
                TS("dve", t_[:], t_[:], 0.044715, 1.0, ALU.mult, ALU.add, [t_], [t_])
                TT("dve", t_[:], t_[:], ys[:, q, :], ALU.mult, [t_, ys], [t_])
                ACT(t_[:], t_[:], AF.Sigmoid, [t_], [t_], scale=1.5957691216057308)
                TT("dve", gy[:, q, :], t_[:], ys[:, q, :], ALU.mult, [t_, ys], [gy])
            for db in range(4):
                wab = nwb()
                DMA("pool", wab[:, 0:8, :], gwa[:, db * 512:(db + 1) * 512].rearrange("(k p) n -> p k n", p=128), W=[wab])
                DMA("pool", wab[:, 8:16, :], gwb[:, db * 512:(db + 1) * 512].rearrange("(k p) n -> p k n", p=128), W=[wab])
                wg = wload(win[:, 5120 + db * 512:5120 + (db + 1) * 512], 16)
                for dd in range(4):
                    d = db * 4 + dd
                    pa_ = nps()
                    for k in range(8):
                        MM(pa_[:, 0:TB], wab[:, k, dd * 128:(dd + 1) * 128], gy[:, k, :], k == 0, k == 7, [wab, gy], [pa_])
                    pb_ = nps()
                    for k in range(8):
                        MM(pb_[:, 0:TB], wab[:, 8 + k, dd * 128:(dd + 1) * 128], gy[:, k, :], k == 0, k == 7, [wab, gy], [pb_])
                    pg2_ = nps()
                    proj(pg2_, pg2_[:, 0:TB], wg, dd * 128, hT, lambda k: hT[:, k, :], 16)
                    t1 = tmpa[0]; t2 = tmpa[1]; t3 = tmpa[2]
                    ACT(t1[:], pb_[:, 0:TB], AF.Sigmoid, [pb_], [t1])
                    TT("dve", t2[:], pa_[:, 0:TB], t1[:], ALU.mult, [pa_, t1], [t2])
                    ACT(t3[:], pg2_[:, 0:TB], AF.Sigmoid, [pg2_, vec], [t3], bias=V(V_GATEB + 16 + d))
                    TT("dve", t2[:], t2[:], t3[:], ALU.mult, [t2, t3], [t2])
                    TT("dve", merged[:, d, :], merged[:, d, :], t2[:], ALU.add, [merged, t2], [merged])
            if bi == 0:
                dump("uT", uT, uT[:], [128, 8, TB], BF16); dump("ys", ys, ys[:], [128, 8, TB]); dump("gy", gy, gy[:], [128, 8, TB], BF16)
                dump("merged", merged, merged[:], [128, 16, TB], BF16)
            for tt in range(TB // 128):
                ti = bi * (TB // 128) + tt
                r0 = ti * 128
                for nb in range(4):
                    wo = wload(wout[:, nb * 512:(nb + 1) * 512], 16)
                    pm = nps()
                    for k in range(16):
                        MM(pm[:, :], merged[:, k, tt * 128:(tt + 1) * 128], wo[:, k, :], k == 0, k == 15, [merged, wo], [pm])
                    CP("act", msb[:, nb * 512:(nb + 1) * 512], pm[:, :], [pm], [msb])
                rms_rstd(msb[:], msb, 1)
                STT(msb[:], msb[:], ssq[:, 1:2], pg1[:], ALU.mult, ALU.mult, [msb, ssq, pg1], [msb])
                DMA("sp", xt0[:], xo[r0:r0 + 128, :], W=[xt0])
                TT("dve", xt0[:], xt0[:], msb[:], ALU.add, [xt0, msb], [xt0])
                DMA("sp", outd[r0:r0 + 128, :], xt0[:], R=[xt0], W=[outdram])
                rms_rstd(xt0[:], xt0, 2)
                ACT(msb[:], xt0[:], AF.Identity, [xt0, ssq], [msb], scale=ssq[:, 2:3])
                for k4 in range(4):
                    p = nps()
                    for kk in range(4):
                        k = k4 * 4 + kk
                        TR(p[:, kk * 128:(kk + 1) * 128], msb[:, k * 128:(k + 1) * 128], identf[:], [msb, identf], [p])
                    for kk in range(4):
                        k = k4 * 4 + kk
                        ACT(h2Tf[:, k, :], p[:, kk * 128:(kk + 1) * 128], AF.Identity, [p, scale2, adacol], [h2Tf],
                            scale=scale2[:, k:k + 1], bias=sh2(k))
                pl = nps()
                for k in range(16):
                    MM(pl[:, 0:E], h2Tf[:, k, :], rwf[:, k * E:(k + 1) * E], k == 0, k == 15, [h2Tf, rwf], [pl])
                TT("dve", lg[:], pl[:, 0:E], rbb[:], ALU.add, [pl, rbb], [lg])
                P.op("dve", lambda e: e.max(out=mx8[:], in_=lg[:]), [lg], [mx8])
                TS("dve", gmask[:], lg[:], mx8[:, 3:4], None, ALU.is_ge, None, [lg, mx8], [gmask])
                TS("dve", small[:, 0:1], mx8[:, 0:1], -1.0, None, ALU.mult, None, [mx8], [small])
                ACT(gsm[:], lg[:], AF.Exp, [lg, small], [gsm], bias=small[:, 0:1])
                TT("dve", gsm[:], gsm[:], gmask[:], ALU.mult, [gsm, gmask], [gsm])
                P.op("dve", lambda e: e.tensor_reduce(out=small[:, 1:2], in_=gsm[:], axis=AX.X, op=ALU.add), [gsm], [small])
                RECIP(small[:, 1:2], small[:, 1:2], [small], [small])
                TS("dve", gsm[:], gsm[:], small[:, 1:2], None, ALU.mult, None, [gsm, small], [gsm])
                pt_ = nps()
                TR(pt_[0:E, 0:128], gsm[:], identf[:], [gsm, identf], [pt_])
                CP("act", GT[:, r0:r0 + 128], pt_[0:E, 0:128], [pt_], [GT])
                ACT(xn[:], xt0[:], AF.Identity, [xt0, ssq], [xn], scale=ssq[:, 2:3])
                pc = nps()
                MM(pc[:, 0:E], tri[:], gmask[:], True, True, [tri, gmask], [pc])
                MM(pc[:, E:2 * E], onesf[:], gmask[:], True, True, [onesf, gmask], [pc])
                slot, valid, idxm, rk, selk, tmpk = rt
                TT("dve", slot[:], pc[:, 0:E], basebc[:], ALU.add, [pc, basebc], [slot])
                TT("dve", basebc[:], basebc[:], pc[:, E:2 * E], ALU.add, [pc, basebc], [basebc])
                TS("dve", valid[:], slot[:], float(CAP), None, ALU.is_lt, None, [slot], [valid])
                TT("dve", valid[:], valid[:], gmask[:], ALU.mult, [valid, gmask], [valid])
                TT("dve", idxm[:], slot[:], ecap[:], ALU.add, [slot, ecap], [idxm])
                TS("dve", idxm[:], idxm[:], -BIGI, None, ALU.add, None, [idxm], [idxm])
                P.op("dve", lambda e: e.tensor_tensor_scan(out=rk[:], data0=onesf[:, 0:E], data1=gmask[:], initial=0.0,
                                                           op0=ALU.mult, op1=ALU.add), [onesf, gmask], [rk])
                TT("dve", rk[:], rk[:], gmask[:], ALU.subtract, [rk, gmask], [rk])
                for k in range(4):
                    TS("dve", selk[:], rk[:], float(k), None, ALU.is_equal, None, [rk], [selk])
                    TT("dve", selk[:], selk[:], valid[:], ALU.mult, [selk, valid], [selk])
                    TT("dve", tmpk[:], selk[:], idxm[:], ALU.mult, [selk, idxm], [tmpk])
                    P.op("dve", lambda e, k=k, ti=ti: e.tensor_reduce(out=IDXF[:, ti, k:k + 1], in_=tmpk[:], axis=AX.X, op=ALU.add), [tmpk], [IDXF])
                    TT("dve", tmpk[:], selk[:], gsm[:], ALU.mult, [selk, gsm], [tmpk])
                    P.op("dve", lambda e, k=k, ti=ti: e.tensor_reduce(out=GK[:, ti, k:k + 1], in_=tmpk[:], axis=AX.X, op=ALU.add), [tmpk], [GK])
                TS("dve", IDXF[:, ti, :], IDXF[:, ti, :], BIGI, None, ALU.add, None, [IDXF], [IDXF])
                CP("dve", IDXI[:, ti, :], IDXF[:, ti, :], [IDXF], [IDXI])
                for k in range(4):
                    P.dma("pool", lambda e, k=k, ti=ti: e.indirect_dma_start(
                        out=Xgd[:, :], out_offset=bass.IndirectOffsetOnAxis(ap=IDXI[:, ti, k:k + 1], axis=0),
                        in_=xn[:], in_offset=None, bounds_check=breg[0], oob_is_err=False), [xn, IDXI], [xgdram])
            if bi == 0:
                dump("lg", lg, lg[:], [128, E]); dump("gsm", gsm, gsm[:], [128, E])
                dump("idx", IDXI, IDXI[:, 1, :], [128, 4], mybir.dt.int32); dump("gk", GK, GK[:, 1, :], [128, 4])
                stop_at("b0")

        def actb_alias(v):
            b = Buf(v)
            abufs.append(b)
            return b

        stop_at("p1b")
        barrier()
        XgT = carve([128, 16, CAP], BF16); actb = carve([128, 16, CAP], BF16)
        xgs = [carve([128, D], BF16) for i in range(2)]
        ybuf = [carve([128, 512]) for i in range(4)]
        gtt = [carve([128, SBW]) for i in range(2)]; ltt = [carve([128, SBW])]; sgt = [carve([128, SBW])]
        NST = CAP // 128
        yi = [0]
        for e in range(E):
            for st_ in range(NST):
                xg = xgs[st_ % 2]
                DMA("sp", xg[:], Xgd[e * CAP + st_ * 128:e * CAP + (st_ + 1) * 128, :], R=[xgdram], W=[xg])
                for k4 in range(4):
                    p = nps()
                    pb = p[:].bitcast(BF16)
                    for kk in range(4):
                        k = k4 * 4 + kk
                        TR(pb[:, kk * 128:(kk + 1) * 128], xg[:, k * 128:(k + 1) * 128], identb[:], [xg, identb], [p])
                    for kk in range(4):
                        k = k4 * 4 + kk
                        ACT(XgT[:, k, st_ * 128:(st_ + 1) * 128], pb[:, kk * 128:(kk + 1) * 128], AF.Identity,
                            [p, scale2, adacol], [XgT], scale=scale2[:, k:k + 1], bias=sh2(k))
            for fb in range(8):
                wb = nwb()
                DMA("pool", wb[:, :, 0:256], w1g[e][:, fb * 256:(fb + 1) * 256].rearrange("(k p) n -> p k n", p=128), W=[wb])
                DMA("pool", wb[:, :, 256:512], w1l[e][:, fb * 256:(fb + 1) * 256].rearrange("(k p) n -> p k n", p=128), W=[wb])
                for fc in range(2):
                    f = fb * 2 + fc
                    for sb_ in range(CAP // SBW):
                        s0 = sb_ * SBW
                        pg_ = nps()
                        proj(pg_, pg_[:, 0:SBW], wb, fc * 128, XgT, lambda k: XgT[:, k, s0:s0 + SBW], 16)
                        pl_ = nps()
                        proj(pl_, pl_[:, 0:SBW], wb, 256 + fc * 128, XgT, lambda k: XgT[:, k, s0:s0 + SBW], 16)
                        g_ = gtt[(f + sb_) % 2]; l_ = ltt[0]; s_ = sgt[0]
                        TS("dve", g_[:], pg_[:, 0:SBW], V(V_B1G + e * 16 + f), 7.0, ALU.add, ALU.min, [pg_, vec], [g_])
                        ACT(s_[:], g_[:], AF.Sigmoid, [g_], [s_], scale=1.702)
                        TS("dve", l_[:], pl_[:, 0:SBW], V(V_B1L + e * 16 + f), 7.0, ALU.add, ALU.min, [pl_, vec], [l_])
                        TS("dve", l_[:], l_[:], -7.0, 1.0, ALU.max, ALU.add, [l_], [l_])
                        TT("dve", g_[:], g_[:], s_[:], ALU.mult, [g_, s_], [g_])
                        TT("dve", actb[:, f, s0:s0 + SBW], g_[:], l_[:], ALU.mult, [g_, l_], [actb])
            for db in range(4):
                wb = wload(w2d[e][:, db * 512:(db + 1) * 512], 16)
                for st_ in range(NST):
                    py = nps()
                    for k in range(16):
                        MM(py[:, :], actb[:, k, st_ * 128:(st_ + 1) * 128], wb[:, k, :], k == 0, k == 15, [actb, wb], [py])
                    yb = ybuf[yi[0] % 4]
                    yi[0] += 1
                    CP("act", yb[:], py[:, :], [py], [yb])
                    DMA("sp", Ygd[e * CAP + st_ * 128:e * CAP + (st_ + 1) * 128, db * 512:(db + 1) * 512], yb[:], R=[yb], W=[ygdram])
        barrier()
        facc = carve([128, D]); yk = [carve([128, D]) for i in range(2)]; junk = carve([128, D], BF16)
        gi = [0]
        for bi in range(NT):
            r0 = bi * 128
            for nb in range(4):
                pbias = nps()
                MM(pbias[:, :], GT[:, r0:r0 + 128], b2s[:, nb * 512:(nb + 1) * 512], True, True, [GT, b2s], [pbias])
                CP("act", facc[:, nb * 512:(nb + 1) * 512], pbias[:, :], [pbias], [facc])
            for k in range(4):
                y_ = yk[gi[0] % 2]
                gi[0] += 1
                MEMSET("pool", y_[:], 0.0, [y_])
                P.dma("pool", lambda e, k=k, bi=bi, y_=y_: e.indirect_dma_start(
                    out=y_[:], out_offset=None, in_=Ygd[:, :],
                    in_offset=bass.IndirectOffsetOnAxis(ap=IDXI[:, bi, k:k + 1], axis=0),
                    bounds_check=breg[0], oob_is_err=False), [IDXI, ygdram], [y_])
                STT(facc[:], y_[:], GK[:, bi, k:k + 1], facc[:], ALU.mult, ALU.add, [y_, GK, facc], [facc])
            ACT(junk[:], facc[:], AF.Square, [facc], [junk, ssq], accum_out=ssq[:, 3:4])
            TS("dve", ssq[:, 3:4], ssq[:, 3:4], 1.0 / D, EPS, ALU.mult, ALU.add, [ssq], [ssq])
            ACT(ssq[:, 3:4], ssq[:, 3:4], AF.Sqrt, [ssq], [ssq])
            RECIP(ssq[:, 3:4], ssq[:, 3:4], [ssq], [ssq])
            STT(facc[:], facc[:], ssq[:, 3:4], pg2[:], ALU.mult, ALU.mult, [facc, ssq, pg2], [facc])
            DMA("sp", xt0[:], outd[r0:r0 + 128, :], R=[outdram], W=[xt0])
            TT("dve", xt0[:], xt0[:], facc[:], ALU.add, [xt0, facc], [xt0])
            DMA("sp", outd[r0:r0 + 128, :], xt0[:], R=[xt0], W=[outdram])

    try:
        body()
    except _Stop:
        pass

    sems = {}
    for k in ("pe", "act", "dve", "pool", "sp"):
        sems[("c", k)] = es.enter_context(nc.semaphore(f"c_{k}"))
    for q in ("sp", "pool"):
        for i in range(Prog.R):
            sems[("d", q, i)] = es.enter_context(nc.semaphore(f"d_{q}{i}"))

    breg = [None]

    def run(name, e):
        if name == "pool":
            breg[0] = e.to_reg(E * CAP - 1)
        for waits, fn, key, amt in P.ops[name]:
            for wk, wv in waits:
                e.wait_ge(sems[wk], wv)
            fn(e).then_inc(sems[key], amt)
        if name == "sp":
            n = P.dq["sp"]
            for i in range(Prog.R):
                cnt = (n - i + Prog.R - 1) // Prog.R
                if cnt > 0:
                    e.wait_ge(sems[("d", "sp", i)], 16 * cnt)

    with es:
        with nc.Block() as block:
            @block.tensor
            def _(e):
                run("pe", e)

            @block.scalar
            def _(e):
                run("act", e)

            @block.vector
            def _(e):
                run("dve", e)

            @block.gpsimd
            def _(e):
                run("pool", e)

            @block.sync
            def _(e):
                run("sp", e)
    return nc


def _prep(inputs):
    f32 = np.float32
    g = {k: np.asarray(v) for k, v in inputs.items()}
    x = g["x"]; c = g["c"]
    sh = {}
    sh["ada_w"] = np.ascontiguousarray(g["ada_w"][0]); sh["ada_b"] = np.ascontiguousarray(g["ada_b"][0][None, :])
    def fm(v, n):
        return np.ascontiguousarray(v.reshape(n, 128).T)
    vec = np.zeros((128, NV), f32)
    vec[:, V_PREMIX:V_PREMIX + 16] = fm(g["pre_mix_g"][0], 16)
    vec[:, V_PREFFN:V_PREFFN + 16] = fm(g["pre_ffn_g"][0], 16)
    vec[:, V_GATEB:V_GATEB + 32] = fm(g["gate_b"][0], 32)
    vec[:, V_DWB:V_DWB + 8] = fm(g["dw_b"][0], 8)
    vec[:, V_CLNG:V_CLNG + 8] = fm(g["cln_g"][0], 8)
    vec[:, V_CLNB:V_CLNB + 8] = fm(g["cln_b"][0], 8)
    vec[:, V_CPB:V_CPB + 16] = fm(g["cp_b"][0], 16)
    vec[:, V_S5D:V_S5D + 8] = fm(g["s5_d"][0], 8)
    dww = g["dw_w"][0]
    vec[:, V_DWW:V_DWW + 248] = dww.T.reshape(8, 128, 31).transpose(1, 0, 2).reshape(128, 248)
    b1 = g["b1"][0]
    vec[:, V_B1G:V_B1G + 512] = b1[:, 0::2].reshape(32, 16, 128).transpose(2, 0, 1).reshape(128, 512)
    vec[:, V_B1L:V_B1L + 512] = b1[:, 1::2].reshape(32, 16, 128).transpose(2, 0, 1).reshape(128, 512)
    vec[:, V_B2:V_B2 + 512] = g["b2"][0].reshape(32, 16, 128).transpose(2, 0, 1).reshape(128, 512)
    pidx = (np.arange(128) // 32) % 2
    vec[:, V_M0] = (pidx == 0).astype(f32); vec[:, V_M0 + 1] = (pidx == 1).astype(f32)
    sh["vec"] = vec
    sh["routerb"] = np.ascontiguousarray(g["router_b"][0][None, :])
    sh["postmix"] = np.ascontiguousarray(g["post_mix_g"][0][None, :])
    sh["postffn"] = np.ascontiguousarray(g["post_ffn_g"][0][None, :])
    are, aim, ldt = g["s5_a_re"][0], g["s5_a_im"][0], g["s5_log_dt"][0]
    ldt2 = np.broadcast_to(ldt[:, None], (64, 64))
    def Sl(a):
        return a.reshape(32, 2, 64).transpose(1, 2, 0).reshape(128, 32)
    sh["aS"] = np.ascontiguousarray(np.concatenate([Sl(are), Sl(aim), Sl(ldt2)], axis=1).astype(f32))
    def Pl(a):
        t = a.reshape(8, 4, 2, 64)
        t = t.transpose(1, 0, 2, 3)
        t = np.broadcast_to(t[:, None, None], (4, 2, 16, 8, 2, 64))
        return t.reshape(128, 1024)
    sh["aP"] = np.ascontiguousarray(np.concatenate([Pl(are), Pl(aim), Pl(ldt2)], axis=1).astype(f32))
    def Bexp(b):
        t = b.reshape(8, 4, 2, 64, 16)
        o = np.zeros((4, 2, 16, 8, 2, 64), f32)
        for gl in range(2):
            o[:, gl, :, :, gl, :] = t[:, :, gl].transpose(1, 3, 0, 2)
        return o.reshape(128, 1024)
    sh["bexp"] = np.concatenate([Bexp(g["s5_b_re"][0]), Bexp(g["s5_b_im"][0])], axis=1)
    def Cexp(cc):
        t = cc.reshape(32, 2, 16, 64)
        o = np.zeros((2, 64, 32, 2, 2, 16), f32)
        for kt in range(32):
            for gl in range(2):
                o[gl, :, kt, kt % 2, gl, :] = t[kt, gl].T
        return o.reshape(128, 2048)
    sh["cexp"] = np.concatenate([Cexp(g["s5_c_re"][0]), Cexp(g["s5_c_im"][0])], axis=1)
    sh["w_in"] = np.ascontiguousarray(g["w_in"][0]); sh["cp_w"] = np.ascontiguousarray(g["cp_w"][0])
    sh["glu_wa"] = np.ascontiguousarray(g["glu_wa"][0]); sh["glu_wb"] = np.ascontiguousarray(g["glu_wb"][0])
    sh["w_out"] = np.ascontiguousarray(g["w_out"][0]); sh["router_w"] = np.ascontiguousarray(g["router_w"][0])
    w1 = g["w1"][0]
    sh["w1g"] = np.ascontiguousarray(w1[:, :, 0::2]); sh["w1l"] = np.ascontiguousarray(w1[:, :, 1::2])
    sh["w2"] = np.ascontiguousarray(g["w2"][0])
    sh["ident"] = np.eye(128, dtype=f32)
    sh["tri"] = np.triu(np.ones((128, 128), f32), 1)
    sh["ecap"] = np.broadcast_to((np.arange(E, dtype=f32) * CAP)[None, :], (128, E)).copy()
    sh["b2s"] = np.ascontiguousarray(g["b2"][0])
    maps = []
    for core in range(8):
        b, half = core // 2, core % 2
        m = dict(sh)
        m["xo"] = np.ascontiguousarray(x[b, half * NTOK:(half + 1) * NTOK])
        m["xp"] = np.ascontiguousarray(x[b, 0:NTOK]) if half == 1 else np.zeros((NTOK, D), f32)
        m["flag"] = np.full((128, 1), float(half), f32)
        m["ccol"] = fm(c[b], 16)
        maps.append(m)
    return maps


def kernel(**inputs):
    maps = _prep(inputs)
    nc = build_nc()
    res = run_bass_kernel_spmd(nc, maps, core_ids=list(range(8)))
    out = np.zeros((4, 4096, D), np.float32)
    for core in range(8):
        b, half = core // 2, core % 2
        out[b, half * NTOK:(half + 1) * NTOK] = res.results[core]["out"]
    return out
```

```python
import math
from contextlib import ExitStack
import numpy as np
import concourse.bass as bass
import concourse.mybir as mybir
from concourse.bass_utils import run_bass_kernel_spmd

F32 = mybir.dt.float32
BF16 = mybir.dt.bfloat16
AF = mybir.ActivationFunctionType
ALU = mybir.AluOpType
AX = mybir.AxisListType

D = 2048
NTOK = 2048
TB = 256
NT = NTOK // 128
SBK = 16
NBLK = NTOK // TB
E = 32
MTB = 512
CAP = 768
SBW = 384
BIGI = 4000000.0
EPS = 1e-6

V_PREMIX, V_PREFFN, V_GATEB, V_DWB, V_CLNG, V_CLNB, V_CPB, V_S5D = 0, 16, 32, 64, 72, 80, 88, 104
V_DWW = 112
V_B1G = V_DWW + 8 * 31
V_B1L = V_B1G + 512
V_B2 = V_B1L + 512
V_M0 = V_B2 + 512
NV = V_M0 + 2


class Buf:

    def __init__(self, t):
        self.t = t
        self.w = None
        self.r = {}

    def __getitem__(self, idx):
        return self.t[idx]


class Alias(Buf):
    def __init__(self, parent, t):
        self.p = parent
        self.t = t

    @property
    def w(self):
        return self.p.w

    @w.setter
    def w(self, v):
        self.p.w = v

    @property
    def r(self):
        return self.p.r

    @r.setter
    def r(self, v):
        self.p.r = v


class Prog:
    R = 8

    def __init__(self):
        self.ops = {k: [] for k in ("pe", "act", "dve", "pool", "sp")}
        self.cnt = {k: 0 for k in self.ops}
        self.known = {k: {} for k in self.ops}
        self.dq = {"sp": 0, "pool": 0}

    def _emit(self, eng, fn, R, W, key, amt, val, extra=()):
        need = {}

        def add(tok):
            if tok is None:
                return
            k, v = tok
            if need.get(k, 0) < v:
                need[k] = v

        for b in R:
            add(b.w)
        for b in W:
            add(b.w)
            for k, v in b.r.items():
                add((k, v))
        for t in extra:
            add(t)
        waits = []
        for k, v in need.items():
            if k == ("c", "pe") and eng == "pe":
                continue
            if self.known[eng].get(k, 0) >= v:
                continue
            self.known[eng][k] = v
            waits.append((k, v))
        self.ops[eng].append((waits, fn, key, amt))
        tok = (key, val)
        for b in W:
            b.w = tok
            b.r = {}
        for b in R:
            if b not in W and b.r.get(key, 0) < val:
                b.r[key] = val
        return tok

    def op(self, eng, fn, R=(), W=()):
        self.cnt[eng] += 1
        return self._emit(eng, fn, R, W, ("c", eng), 1, self.cnt[eng])

    def dma(self, q, fn, R=(), W=()):
        j = self.dq[q]
        self.dq[q] += 1
        key = ("d", q, j % self.R)
        extra = []
        if j >= self.R:
            extra.append((key, 16 * (j // self.R)))
        return self._emit(q, fn, R, W, key, 16, 16 * (j // self.R + 1), extra)


class _Stop(Exception):
    pass


def build_nc(stop=None, dumps=()):
    nc = bass.Bass("TRN2", target_bir_lowering=False)
    P = Prog()
    dbg = {}

    def dump(name, B, ap, shape, dt=F32):
        if name not in dumps:
            return
        t = nc.dram_tensor("dbg_" + name, list(shape), dt, kind="ExternalOutput").ap()
        P.dma("sp", lambda e: e.dma_start(out=t, in_=ap), [B], ())

    def stop_at(tag):
        if stop == tag:
            raise _Stop()

    def din(name, shape, dt=F32):
        return nc.dram_tensor(name, list(shape), dt, kind="ExternalInput").ap()

    xo = din("xo", [NTOK, D]); xp = din("xp", [NTOK, D]); flagd = din("flag", [128, 1])
    ccold = din("ccol", [128, 16]); adaw = din("ada_w", [D, 6 * D]); adab = din("ada_b", [1, 6 * D])
    vecd = din("vec", [128, NV]); routerbd = din("routerb", [1, E])
    postmixd = din("postmix", [1, D]); postffnd = din("postffn", [1, D])
    aSd = din("aS", [128, 96]); aPd = din("aP", [128, 3072])
    bexpd = din("bexp", [128, 2048]); cexpd = din("cexp", [128, 4096])
    win = din("w_in", [D, 7168]); cpw = din("cp_w", [1024, D]); gwa = din("glu_wa", [1024, D])
    gwb = din("glu_wb", [1024, D]); wout = din("w_out", [D, D]); rwd = din("router_w", [D, E])
    w1g = din("w1g", [E, D, 2048]); w1l = din("w1l", [E, D, 2048]); w2d = din("w2", [E, 2048, D])
    identd = din("ident", [128, 128]); trid = din("tri", [128, 128]); ecapd = din("ecap", [128, E]); b2d = din("b2s", [E, D])
    outd = nc.dram_tensor("out", [NTOK, D], F32, kind="ExternalOutput").ap()
    Xgd = nc.dram_tensor("xg_scratch", [E * CAP, D], BF16, kind="Internal").ap()
    Ygd = nc.dram_tensor("yg_scratch", [E * CAP, D], F32, kind="Internal").ap()

    es = ExitStack()
    h2dram = Buf(None); outdram = Buf(None); xgdram = Buf(None); ygdram = Buf(None)

    def sb(name, shape, dt=F32):
        return Buf(es.enter_context(nc.sbuf_tensor(name, list(shape), dt)))

    ps = [Buf(es.enter_context(nc.psum_tensor(f"ps{i}", [128, 512], F32))) for i in range(8)]
    psi = [0]

    def nps():
        p = ps[psi[0] % 6]
        psi[0] += 1
        return p

    def DMA(q, out, in_, R=(), W=()):
        P.dma(q, lambda e: e.dma_start(out=out, in_=in_), R, W)

    def ACT(out, in_, func, R, W, **kw):
        P.op("act", lambda e: e.activation(out=out, in_=in_, func=func, **kw), R, W)

    def TT(eng, out, a, b, op, R, W):
        P.op(eng, lambda e: e.tensor_tensor(out=out, in0=a, in1=b, op=op), R, W)

    def TS(eng, out, a, s1, s2, op0, op1, R, W):
        if op1 is None:
            P.op(eng, lambda e: e.tensor_scalar(out=out, in0=a, scalar1=s1, scalar2=None, op0=op0), R, W)
        else:
            P.op(eng, lambda e: e.tensor_scalar(out=out, in0=a, scalar1=s1, scalar2=s2, op0=op0, op1=op1), R, W)

    def STT(out, a, s, b, op0, op1, R, W):
        P.op("dve", lambda e: e.scalar_tensor_tensor(out=out, in0=a, scalar=s, in1=b, op0=op0, op1=op1), R, W)

    def MM(out, l, r, st, sp_, R, W):
        P.op("pe", lambda e: e.matmul(out, l, r, start=st, stop=sp_), R, W)

    def TR(out, in_, ident, R, W):
        P.op("pe", lambda e: e.transpose(out, in_, ident), R, W)

    def CP(eng, out, in_, R, W):
        if eng == "act":
            P.op(eng, lambda e: e.activation(out=out, in_=in_, func=AF.Identity), R, W)
        else:
            P.op(eng, lambda e: e.tensor_copy(out=out, in_=in_), R, W)

    def RECIP(out, in_, R, W):
        P.op("dve", lambda e: e.reciprocal(out=out, in_=in_), R, W)

    def MEMSET(eng, ap, c, W):
        P.op(eng, lambda e: e.memset(ap, c), (), W)

    identf = sb("identf", [128, 128]); identb = sb("identb", [128, 128], BF16)
    onesf = sb("onesf", [128, 128]); onesm = sb("onesm", [128, 128])
    vec = sb("vecs", [128, NV]); flag = sb("flagt", [128, 1])
    ccol = sb("ccolt", [128, 16]); silub = sb("silub", [128, 16], BF16)
    adacol = sb("adacol", [128, 96]); scale1 = sb("scale1", [128, 16]); scale2 = sb("scale2", [128, 16])
    pg1 = sb("pg1", [128, D]); pg2 = sb("pg2", [128, D])
    rbb = sb("rbb", [128, E]); rwf = sb("rwf", [128, 16 * E])
    wblk = [sb(f"wblk{i}", [128, 16, 512], BF16) for i in range(2)]
    wbi = [0]

    def nwb():
        w = wblk[wbi[0] % 2]
        wbi[0] += 1
        return w

    xin = [sb("xin0", [128, D])]
    ssq = sb("ssq", [128, 4]); small = sb("small", [128, 16])
    Mt = sb("Mt", [128, 2, 2, 32]); Rt = sb("Rt", [128, 2, 32]); AA = sb("AA", [128, 2, 2, 32])
    lg = sb("lg", [128, E]); mx8 = sb("mx8", [128, 8]); gsm = sb("gsm", [128, E]); gmask = sb("gmask", [128, E])
    GT = sb("GT", [32, NTOK])
    tri = sb("tri_t", [128, 128]); ecap = sb("ecap_t", [128, E]); basebc = sb("basebc", [128, E])
    IDXF = sb("IDXF", [128, NT, 4]); GK = sb("GK", [128, NT, 4]); IDXI = sb("IDXI", [128, NT, 4], mybir.dt.int32)
    rt = [sb(f"rt{i}", [128, E]) for i in range(6)]
    BbTm = [[sb(f"BbTm{i}{j}", [128, 8, 128], BF16) for j in range(2)] for i in range(2)]
    CT = [sb(f"CT{i}", [128, 32, 64], BF16) for i in range(2)]
    aS = sb("aSt", [128, 96]); stmp = sb("stmp", [128, 12 * 32])
    bars = {k: sb(f"bar_{k}", [128, 4]) for k in ("dve", "act", "pool")}
    AW = 22016
    arena = es.enter_context(nc.sbuf_tensor("arena", [128, AW], F32))
    apos = [0]
    abufs = []

    def carve(shape, dt=F32):
        n = 1
        for d_ in shape[1:]:
            n *= d_
        words = n if dt == F32 else (n + 1) // 2
        o = apos[0]
        apos[0] += words
        assert apos[0] <= AW, ("arena overflow", apos[0])
        v = arena[:, o:o + words]
        if dt != F32:
            v = v.bitcast(dt)
        if len(shape) == 3:
            v = v.rearrange("p (a b) -> p a b", a=shape[1])
        elif len(shape) == 4:
            v = v.rearrange("p (a b c) -> p a b c", a=shape[1], b=shape[2])
        b = Buf(v)
        abufs.append(b)
        return b

    def barrier():
        W0 = list(abufs)
        P.op("dve", lambda e: e.memset(bars["dve"][:], 0.0), (), [bars["dve"]] + W0)
        P.op("pool", lambda e: e.memset(bars["pool"][:], 0.0), (), [bars["pool"]])
        P.op("act", lambda e: e.activation(out=bars["act"][:], in_=flag[:, 0:1].broadcast_to([128, 4]), func=AF.Identity), [flag], [bars["act"]])
        pb_ = nps()
        MM(pb_[0:1, 0:2], onesf[0:1, 0:1], onesf[0:1, 0:2], True, True, [onesf], [pb_])
        allb = [bars["dve"], bars["pool"], bars["act"], pb_]
        P.op("dve", lambda e: e.memset(small[:, 8:9], 0.0), allb, [small])
        P.op("pool", lambda e: e.memset(small[:, 9:10], 0.0), allb + [small], [small])
        P.op("act", lambda e: e.activation(out=small[:, 10:11], in_=flag[:, 0:1], func=AF.Identity), allb + [small], [small])
        pb2 = nps()
        MM(pb2[0:1, 0:2], onesf[0:1, 0:1], onesf[0:1, 0:2], True, True, [onesf, small] + allb, [pb2])
        DMA("sp", flag[:], flagd, R=[small, pb2], W=[flag])
        DMA("pool", ssq[:, 3:4], flagd, R=[small, pb2], W=[ssq])
        del abufs[:]
        apos[0] = 0

    rowb = carve([1 + 127, 512]); adabk = carve([128, 512])
    fT0 = carve([128, 8, 1024]); merged0 = carve([128, 3072]); ys0 = carve([128, 2048]); cv0 = carve([128, 4096])
    BbT = [carve([128, 8, 128], BF16) for i in range(2)]

    def V(off, n=1):
        return vec[:, off:off + n]

    def body():
        DMA("sp", identf[:], identd, W=[identf])
        DMA("sp", vec[:], vecd, W=[vec])
        DMA("sp", flag[:], flagd, W=[flag])
        DMA("sp", ccol[:], ccold, W=[ccol])
        DMA("sp", rwf[:].rearrange("p (k n) -> p k n", k=16), rwd.rearrange("(k p) n -> p k n", p=128), W=[rwf])
        DMA("sp", rbb[:], routerbd.broadcast_to([128, E]), W=[rbb])
        DMA("sp", pg1[:], postmixd.broadcast_to([128, D]), W=[pg1])
        DMA("sp", pg2[:], postffnd.broadcast_to([128, D]), W=[pg2])
        DMA("sp", aS[:], aSd, W=[aS])
        DMA("sp", tri[:], trid, W=[tri])
        DMA("sp", ecap[:], ecapd, W=[ecap])
        MEMSET("dve", basebc[:], 0.0, [basebc])
        CP("dve", identb[:], identf[:], [identf], [identb])
        MEMSET("dve", onesf[:], 1.0, [onesf])
        MEMSET("dve", onesm[:], 1.0 / 1024.0, [onesm])
        ACT(silub[:], ccol[:], AF.Silu, [ccol], [silub])

        for cb in range(24):
            wb = nwb()
            DMA("pool", wb[:], adaw[:, cb * 512:(cb + 1) * 512].rearrange("(k p) n -> p k n", p=128), W=[wb])
            DMA("sp", adabk[0:1, :], adab[0:1, cb * 512:(cb + 1) * 512], W=[adabk])
            p = nps()
            for k in range(16):
                MM(p[0:1, :], silub[:, k:k + 1], wb[:, k, :], k == 0, k == 15, [silub, wb], [p])
            TT("dve", rowb[0:1, :], p[0:1, :], adabk[0:1, :], ALU.add, [p, adabk], [rowb])
            p2 = nps()
            for j in range(4):
                MM(p2[:, j:j + 1], rowb[0:1, j * 128:(j + 1) * 128], onesf[0:1, 0:1], True, True, [rowb, onesf], [p2])
            CP("act", adacol[:, cb * 4:cb * 4 + 4], p2[:, 0:4], [p2], [adacol])
            if 8 <= cb < 12 or 20 <= cb < 24:
                pgx, c0 = (pg1, (cb - 8) * 512) if cb < 12 else (pg2, (cb - 20) * 512)
                p3 = nps()
                MM(p3[:, :], onesf[0:1, :], rowb[0:1, :], True, True, [rowb, onesf], [p3])
                TT("dve", pgx[:, c0:c0 + 512], pgx[:, c0:c0 + 512], p3[:, :], ALU.mult, [pgx, p3], [pgx])
        TS("dve", small[:, 0:16], adacol[:, 16:32], 1.0, None, ALU.add, None, [adacol], [small])
        TT("dve", scale1[:], small[:, 0:16], V(V_PREMIX, 16), ALU.mult, [small, vec], [scale1])
        TS("dve", small[:, 0:16], adacol[:, 64:80], 1.0, None, ALU.add, None, [adacol], [small])
        TT("dve", scale2[:], small[:, 0:16], V(V_PREFFN, 16), ALU.mult, [small, vec], [scale2])
        sh1 = lambda k: adacol[:, k:k + 1]
        sh2 = lambda k: adacol[:, 48 + k:49 + k]

        def abar(aR, aI, lD, t, Bs, B):
            dt_, x_, rho, th, c_, s_, t1, t2 = t
            ACT(dt_, lD, AF.Exp, Bs, [B])
            TT("dve", x_, aR, dt_, ALU.mult, Bs, [B])
            ACT(rho, x_, AF.Exp, [B], [B])
            TT("dve", th, aI, dt_, ALU.mult, Bs, [B])
            ACT(s_, th, AF.Sin, [B], [B], scale=1.0 / 32.0)
            TS("dve", x_, th, 1.0 / 32.0, math.pi / 2, ALU.mult, ALU.add, [B], [B])
            ACT(c_, x_, AF.Sin, [B], [B])
            for _ in range(5):
                TT("dve", t1, c_, c_, ALU.mult, [B], [B])
                TT("dve", t2, s_, s_, ALU.mult, [B], [B])
                TT("dve", x_, c_, s_, ALU.mult, [B], [B])
                TT("dve", c_, t1, t2, ALU.subtract, [B], [B])
                TS("dve", s_, x_, 2.0, None, ALU.mult, None, [B], [B])
            TT("dve", dt_, rho, c_, ALU.mult, [B], [B])
            TT("dve", x_, rho, s_, ALU.mult, [B], [B])
            return dt_, x_

        st = [stmp[:, i * 32:(i + 1) * 32] for i in range(8)]
        abr, abi = abar(aS[:, 0:32], aS[:, 32:64], aS[:, 64:96], st, [stmp, aS], stmp)
        CP("dve", AA[:, 0, 0, :], abr, [stmp], [AA])
        CP("dve", AA[:, 1, 1, :], abr, [stmp], [AA])
        CP("dve", AA[:, 1, 0, :], abi, [stmp], [AA])
        TS("dve", AA[:, 0, 1, :], abi, -1.0, None, ALU.mult, None, [stmp], [AA])
        pt = [fT0[:, i, :] for i in range(8)]
        DMA("sp", merged0[:], aPd, W=[merged0])
        DMA("sp", ys0[:], bexpd, W=[ys0])
        DMA("sp", cv0[:], cexpd, W=[cv0])
        aRp, aIp, lDp = merged0[:, 0:1024], merged0[:, 1024:2048], merged0[:, 2048:3072]
        abrP, abiP = abar(aRp, aIp, lDp, pt, [fT0, merged0], fT0)
        _, _, q2, q3, q4, q5, q6, q7 = pt
        Bs = [fT0, merged0]
        TT("dve", q2, aRp, aRp, ALU.mult, Bs, [fT0])
        TT("dve", q3, aIp, aIp, ALU.mult, Bs, [fT0])
        TT("dve", q2, q2, q3, ALU.add, [fT0], [fT0])
        RECIP(q2, q2, [fT0], [fT0])
        TS("dve", q3, abrP, -1.0, None, ALU.add, None, [fT0], [fT0])
        TT("dve", q4, q3, aRp, ALU.mult, Bs, [fT0])
        TT("dve", q5, abiP, aIp, ALU.mult, Bs, [fT0])
        TT("dve", q4, q4, q5, ALU.add, [fT0], [fT0])
        TT("dve", q4, q4, q2, ALU.mult, [fT0], [fT0])
        TT("dve", q5, abiP, aRp, ALU.mult, Bs, [fT0])
        TT("dve", q6, q3, aIp, ALU.mult, Bs, [fT0])
        TT("dve", q5, q5, q6, ALU.subtract, [fT0], [fT0])
        TT("dve", q5, q5, q2, ALU.mult, [fT0], [fT0])
        bre, bim = ys0[:, 0:1024], ys0[:, 1024:2048]
        Bs2 = [fT0, ys0]
        TT("dve", q6, q4, bre, ALU.mult, Bs2, [fT0])
        TT("dve", q7, q5, bim, ALU.mult, Bs2, [fT0])
        TT("dve", BbT[0][:].rearrange("p k n -> p (k n)"), q6, q7, ALU.subtract, [fT0], [BbT[0]])
        TT("dve", q6, q4, bim, ALU.mult, Bs2, [fT0])
        TT("dve", q7, q5, bre, ALU.mult, Bs2, [fT0])
        TT("dve", BbT[1][:].rearrange("p k n -> p (k n)"), q6, q7, ALU.add, [fT0], [BbT[1]])
        CP("dve", CT[0][:].rearrange("p k n -> p (k n)"), cv0[:, 0:2048], [cv0], [CT[0]])
        TS("dve", CT[1][:].rearrange("p k n -> p (k n)"), cv0[:, 2048:4096], -1.0, None, ALU.mult, None, [cv0], [CT[1]])
        for ri in range(2):
            for j in range(2):
                TS("dve", BbTm[ri][j][:], BbT[ri][:], V(V_M0 + j), None, ALU.mult, None, [BbT[ri], vec], [BbTm[ri][j]])

        dump("adacol", adacol, adacol[:], [128, 96]); dump("AA", AA, AA[:], [128, 2, 2, 32]); dump("pg1", pg1, pg1[:], [128, D])
        dump("BbT0", BbT[0], BbT[0][:], [128, 8, 128], BF16); dump("BbT1", BbT[1], BbT[1][:], [128, 8, 128], BF16)
        dump("CT0", CT[0], CT[0][:], [128, 32, 64], BF16); dump("scale1", scale1, scale1[:], [128, 16])
        stop_at("p0")
        barrier()
        xn = carve([128, D], BF16); hT = carve([128, 16, TB], BF16)
        vT = carve([128, 8, 30 + TB]); cv = carve([128, 8, TB])
        sqt = [carve([128, TB]) for i in range(2)]
        meansb = carve([128, TB]); rstdsb = carve([128, TB]); tmpa = [carve([128, TB]) for i in range(3)]
        vn = carve([128, 8, TB], BF16); uT = carve([128, 8, TB], BF16)
        ys = carve([128, 8, TB]); gy = carve([128, 8, TB], BF16)
        Mt3 = carve([128, SBK * 2 * 32 * 3]); sS = [carve([128, 2, 32, SBK]) for i in range(2)]
        M5 = Mt3[:].rearrange("p (t o k i) -> p t o k i", t=SBK, o=2, k=32)
        AAp = AA[:].rearrange("p o i k -> p o k i")
        print("phase1 arena words", apos[0])
        sbf = carve([128, 2, 32, SBK], BF16)
        merged = carve([128, 16, TB], BF16)
        msb = Alias(cv, cv.t.rearrange("p a b -> p (a b)"))
        h2Tf = Alias(hT, hT.t.rearrange("p a b -> p (a b)").bitcast(F32).rearrange("p (a b) -> p a b", a=16))
        xt0 = xin[0]

        def rms_rstd(src_ap, srcB, col):
            ACT(xn[:], src_ap, AF.Square, [srcB], [xn, ssq], accum_out=ssq[:, col:col + 1])
            TS("dve", ssq[:, col:col + 1], ssq[:, col:col + 1], 1.0 / D, EPS, ALU.mult, ALU.add, [ssq], [ssq])
            ACT(ssq[:, col:col + 1], ssq[:, col:col + 1], AF.Sqrt, [ssq], [ssq])
            RECIP(ssq[:, col:col + 1], ssq[:, col:col + 1], [ssq], [ssq])

        def norm_T_bf(src_ap, srcB, scale_t, shf, dst, tt):
            rms_rstd(src_ap, srcB, 0)
            ACT(xn[:], src_ap, AF.Identity, [srcB, ssq], [xn], scale=ssq[:, 0:1])
            for k4 in range(4):
                p = nps()
                pb = p[:].bitcast(BF16)
                for kk in range(4):
                    k = k4 * 4 + kk
                    TR(pb[:, kk * 128:(kk + 1) * 128], xn[:, k * 128:(k + 1) * 128], identb[:], [xn, identb], [p])
                for kk in range(4):
                    k = k4 * 4 + kk
                    ACT(dst[:, k, tt * 128:(tt + 1) * 128], pb[:, kk * 128:(kk + 1) * 128], AF.Identity,
                        [p, scale_t, adacol], [dst], scale=scale_t[:, k:k + 1], bias=shf(k))

        def proj(pB_, pout, wb, c0, rhsB, rhs_fn, nk):
            for k in range(nk):
                MM(pout, wb[:, k, c0:c0 + 128], rhs_fn(k), k == 0, k == nk - 1, [wb, rhsB], [pB_])

        def wload(src_ap, nk, ncol=512):
            wb = nwb()
            DMA("pool", wb[:, 0:nk, 0:ncol], src_ap.rearrange("(k p) n -> p k n", p=128), W=[wb])
            return wb

        MEMSET("dve", sS[1][:], 0.0, [sS[1]])
        MEMSET("dve", vT[:], 0.0, [vT])
        sidx = [0]
        NS = TB // SBK

        def s5_scan_gen(with_y):
            for sbk in range(NS):
                t0 = sbk * SBK
                for ri in range(2):
                    pp = [nps(), nps()]
                    pvs = [pp_[:, 0:16 * SBK].rearrange("p (q j n) -> p q j n", q=8, j=2) for pp_ in pp]
                    for kt in range(32):
                        q, ktl = kt // 4, kt % 4
                        h2_, j_ = ktl // 2, ktl % 2
                        MM(pvs[h2_][:, q, j_, :], BbTm[ri][j_][64 * h2_:64 * h2_ + 64, q, :], uT[64 * h2_:64 * h2_ + 64, q, t0:t0 + SBK],
                           True, True, [BbTm[ri][j_], uT], [pp[h2_]])
                    buv = M5[:, :, ri, :, 2].rearrange("p t (q k) -> p q k t", k=4)
                    for h2_ in range(2):
                        CP("act", buv[:, :, 2 * h2_:2 * h2_ + 2, :], pvs[h2_], [pp[h2_]], [Mt3])
                cur = sS[sidx[0] % 2]
                prv = sS[(sidx[0] + 1) % 2]
                sidx[0] += 1
                for t in range(SBK):
                    if t == 0:
                        pa, pBuf = prv[:, :, :, SBK - 1], prv
                    else:
                        pa, pBuf = cur[:, :, :, t - 1], cur
                    pin = pa.rearrange("p i k -> p k i").unsqueeze(1).broadcast_to([128, 2, 32, 2])
                    TT("dve", M5[:, t, :, :, 0:2], AAp, pin, ALU.mult, [AA, pBuf], [Mt3])
                    P.op("dve", lambda e, t=t, cur=cur: e.tensor_reduce(out=cur[:, :, :, t], in_=M5[:, t], axis=AX.X, op=ALU.add), [Mt3], [cur])
                if with_y:
                    CP("act", sbf[:], cur[:], [cur], [sbf])
                    p = nps()
                    pv = p[:, 0:8 * SBK].rearrange("p (a n) -> p a n", a=8)
                    for kt in range(32):
                        q, ktl = kt // 4, kt % 4
                        h2_, j_ = ktl // 2, ktl % 2
                        MM(pv[64 * h2_:64 * h2_ + 64, q, :], CT[0][:, kt, :], sbf[:, 0, kt, :], j_ == 0, False, [CT[0], sbf], [p])
                        MM(pv[64 * h2_:64 * h2_ + 64, q, :], CT[1][:, kt, :], sbf[:, 1, kt, :], False, j_ == 1, [CT[1], sbf], [p])
                    for q in range(8):
                        STT(ys[:, q, t0:t0 + SBK], uT[:, q, t0:t0 + SBK], V(V_S5D + q), pv[:, q, :], ALU.mult, ALU.add,
                            [uT, vec, p], [ys])
                yield

        def s5_scan_block(with_y):
            for _ in s5_scan_gen(with_y):
                pass

        def u_proj():
            for half in range(2):
                wb = wload(win[:, 2048 + half * 512:2048 + (half + 1) * 512], 16)
                for jj in range(4):
                    p = nps()
                    proj(p, p[:, 0:TB], wb, jj * 128, hT, lambda k: hT[:, k, :], 16)
                    CP("act", uT[:, half * 4 + jj, :], p[:, 0:TB], [p], [uT])

        def conv_glu_proj():
            for half in range(2):
                wbv = wload(win[:, half * 512:(half + 1) * 512], 16)
                wbg = wload(win[:, 1024 + half * 512:1024 + (half + 1) * 512], 16)
                for jj in range(4):
                    j = half * 4 + jj
                    pv_ = nps()
                    proj(pv_, pv_[:, 0:TB], wbv, jj * 128, hT, lambda k: hT[:, k, :], 16)
                    pg_ = nps()
                    proj(pg_, pg_[:, 0:TB], wbg, jj * 128, hT, lambda k: hT[:, k, :], 16)
                    sg = sqt[j % 2]
                    ACT(sg[:], pg_[:, 0:TB], AF.Sigmoid, [pg_], [sg])
                    TT("dve", vT[:, j, 30:30 + TB], pv_[:, 0:TB], sg[:], ALU.mult, [pv_, sg], [vT])
                    yield

        for bi in range(NBLK):
            for tt in range(TB // 128):
                r0 = bi * TB + tt * 128
                DMA("sp", xt0[:], xp[r0:r0 + 128, :], W=[xt0])
                norm_T_bf(xt0[:], xt0, scale1, sh1, hT, tt)
            if bi == 0:
                stop_at("n0")
            u_proj()
            if bi == 0:
                stop_at("u0")
            s5_scan_block(False)
            if bi == 0:
                stop_at("s0")
            if bi == NBLK - 1:
                for _ in conv_glu_proj():
                    pass
        last = sS[(sidx[0] + 1) % 2]
        TS("dve", last[:, :, :, SBK - 1], last[:, :, :, SBK - 1], flag[:, 0:1], None, ALU.mult, None, [last, flag], [last])
        TS("dve", vT[:, :, 0:30], vT[:, :, TB:TB + 30], flag[:, 0:1], None, ALU.mult, None, [vT, flag], [vT])

        dump("last", last, last[:], [128, 2, 32, SBK]); dump("halo", vT, vT[:], [128, 8, 30 + TB])
        stop_at("p1a")
        for bi in range(NBLK):
            for tt in range(TB // 128):
                r0 = bi * TB + tt * 128
                DMA("sp", xt0[:], xo[r0:r0 + 128, :], W=[xt0])
                norm_T_bf(xt0[:], xt0, scale1, sh1, hT, tt)
            if bi == 0:
                dump("hT", hT, hT[:], [128, 16, TB], BF16)
            def gen_def(bi=bi):
                yield from conv_glu_proj()
                pmean = ps[6]; pmsq = ps[7]
                for j in range(8):
                    acc = cv[:, j, :]
                    TS("dve", acc, vT[:, j, 0:TB], V(V_DWW + j * 31), V(V_DWB + j), ALU.mult, ALU.add, [vT, vec], [cv])
                    for k in range(1, 31):
                        STT(acc, vT[:, j, k:k + TB], V(V_DWW + j * 31 + k), acc, ALU.mult, ALU.add, [vT, vec, cv], [cv])
                    sq = sqt[j % 2]
                    ACT(sq[:], acc, AF.Square, [cv], [sq])
                    MM(pmean[:, 0:TB], onesm[:], acc, j == 0, j == 7, [onesm, cv], [pmean])
                    MM(pmsq[:, 0:TB], onesm[:], sq[:], j == 0, j == 7, [onesm, sq], [pmsq])
                    yield
                CP("act", meansb[:], pmean[:, 0:TB], [pmean], [meansb])
                TT("dve", rstdsb[:], meansb[:], meansb[:], ALU.mult, [meansb], [rstdsb])
                TT("dve", rstdsb[:], pmsq[:, 0:TB], rstdsb[:], ALU.subtract, [pmsq, rstdsb], [rstdsb])
                TS("dve", rstdsb[:], rstdsb[:], EPS, None, ALU.add, None, [rstdsb], [rstdsb])
                ACT(rstdsb[:], rstdsb[:], AF.Sqrt, [rstdsb], [rstdsb])
                RECIP(rstdsb[:], rstdsb[:], [rstdsb], [rstdsb])
                for j in range(8):
                    t_ = tmpa[j % 2]
                    TT("dve", t_[:], cv[:, j, :], meansb[:], ALU.subtract, [cv, meansb], [t_])
                    TT("dve", t_[:], t_[:], rstdsb[:], ALU.mult, [t_, rstdsb], [t_])
                    ACT(vn[:, j, :], t_[:], AF.Silu, [t_, vec], [vn], scale=V(V_CLNG + j), bias=V(V_CLNB + j))
                yield
                if bi == 0:
                    dump("vT", vT, vT[:], [128, 8, 30 + TB]); dump("cv", cv, cv[:], [128, 8, TB]); dump("vn", vn, vn[:], [128, 8, TB], BF16)
                CP("dve", vT[:, :, 0:30], vT[:, :, TB:TB + 30], [vT], [vT])
                for db in range(4):
                    wb = wload(cpw[:, db * 512:(db + 1) * 512], 8)
                    wg = wload(win[:, 3072 + db * 512:3072 + (db + 1) * 512], 16)
                    for dd in range(4):
                        d = db * 4 + dd
                        py = nps()
                        proj(py, py[:, 0:TB], wb, dd * 128, vn, lambda k: vn[:, k, :], 8)
                        pgt = nps()
                        proj(pgt, pgt[:, 0:TB], wg, dd * 128, hT, lambda k: hT[:, k, :], 16)
                        sg = sqt[d % 2]
                        ACT(sg[:], pgt[:, 0:TB], AF.Sigmoid, [pgt, vec], [sg], bias=V(V_GATEB + d))
                        STT(merged[:, d, :], py[:, 0:TB], V(V_CPB + d), sg[:], ALU.add, ALU.mult, [py, vec, sg], [merged])
                        yield
                if bi == 0:
                    dump("mergedc", merged, merged[:], [128, 16, TB], BF16)

            u_proj()
            gc = s5_scan_gen(True); gd = gen_def()
            alive_c = alive_d = True
            while alive_c or alive_d:
                if alive_c:
                    try:
                        next(gc)
                    except StopIteration:
                        alive_c = False
                for _ in range(3):
                    if alive_d:
                        try:
                            next(gd)
                        except StopIteration:
                            alive_d = False
            for q in range(8):
                t_ = tmpa[q % 2]
                ACT(t_[:], ys[:, q, :], AF.Square, [ys], [t_])
                TS("dve", t_[:], t_[:], 0.044715, 1.0, ALU.mult, ALU.add, [t_], [t_])
                TT("dve", t_[:], t_[:], ys[:, q, :], ALU.mult, [t_, ys], [t_])
                ACT(t_[:], t_[:], AF.Sigmoid, [t_], [t_], scale=1.5957691216057308)
                TT("dve", gy[:, q, :], t_[:], ys[:, q, :], ALU.mult, [t_, ys], [gy])
            for db in range(4):
                wab = nwb()
                DMA("pool", wab[:, 0:8, :], gwa[:, db * 512:(db + 1) * 512].rearrange("(k p) n -> p k n", p=128), W=[wab])
                DMA("pool", wab[:, 8:16, :], gwb[:, db * 512:(db + 1) * 512].rearrange("(k p) n -> p k n", p=128), W=[wab])
                wg = wload(win[:, 5120 + db * 512:5120 + (db + 1) * 512], 16)
                for dd in range(4):
                    d = db * 4 + dd
                    pa_ = nps()
                    for k in range(8):
                        MM(pa_[:, 0:TB], wab[:, k, dd * 128:(dd + 1) * 128], gy[:, k, :], k == 0, k == 7, [wab, gy], [pa_])
                    pb_ = nps()
                    for k in range(8):
                        MM(pb_[:, 0:TB], wab[:, 8 + k, dd * 128:(dd + 1) * 128], gy[:, k, :], k == 0, k == 7, [wab, gy], [pb_])
                    pg2_ = nps()
                    proj(pg2_, pg2_[:, 0:TB], wg, dd * 128, hT, lambda k: hT[:, k, :], 16)
                    t1 = tmpa[0]; t2 = tmpa[1]; t3 = tmpa[2]
                    ACT(t1[:], pb_[:, 0:TB], AF.Sigmoid, [pb_], [t1])
                    TT("dve", t2[:], pa_[:, 0:TB], t1[:], ALU.mult, [pa_, t1], [t2])
                    ACT(t3[:], pg2_[:, 0:TB], AF.Sigmoid, [pg2_, vec], [t3], bias=V(V_GATEB + 16 + d))
                    TT("dve", t2[:], t2[:], t3[:], ALU.mult, [t2, t3], [t2])
                    TT("dve", merged[:, d, :], merged[:, d, :], t2[:], ALU.add, [merged, t2], [merged])
            if bi == 0:
                dump("uT", uT, uT[:], [128, 8, TB], BF16); dump("ys", ys, ys[:], [128, 8, TB]); dump("gy", gy, gy[:], [128, 8, TB], BF16)
                dump("merged", merged, merged[:], [128, 16, TB], BF16)
            for tt in range(TB // 128):
                ti = bi * (TB // 128) + tt
                r0 = ti * 128
                for nb in range(4):
                    wo = wload(wout[:, nb * 512:(nb + 1) * 512], 16)
                    pm = nps()
                    for k in range(16):
                        MM(pm[:, :], merged[:, k, tt * 128:(tt + 1) * 128], wo[:, k, :], k == 0, k == 15, [merged, wo], [pm])
                    CP("act", msb[:, nb * 512:(nb + 1) * 512], pm[:, :], [pm], [msb])
                rms_rstd(msb[:], msb, 1)
                STT(msb[:], msb[:], ssq[:, 1:2], pg1[:], ALU.mult, ALU.mult, [msb, ssq, pg1], [msb])
                DMA("sp", xt0[:], xo[r0:r0 + 128, :], W=[xt0])
                TT("dve", xt0[:], xt0[:], msb[:], ALU.add, [xt0, msb], [xt0])
                DMA("sp", outd[r0:r0 + 128, :], xt0[:], R=[xt0], W=[outdram])
                rms_rstd(xt0[:], xt0, 2)
                ACT(msb[:], xt0[:], AF.Identity, [xt0, ssq], [msb], scale=ssq[:, 2:3])
                for k4 in range(4):
                    p = nps()
                    for kk in range(4):
                        k = k4 * 4 + kk
                        TR(p[:, kk * 128:(kk + 1) * 128], msb[:, k * 128:(k + 1) * 128], identf[:], [msb, identf], [p])
                    for kk in range(4):
                        k = k4 * 4 + kk
                        ACT(h2Tf[:, k, :], p[:, kk * 128:(kk + 1) * 128], AF.Identity, [p, scale2, adacol], [h2Tf],
                            scale=scale2[:, k:k + 1], bias=sh2(k))
                pl = nps()
                for k in range(16):
                    MM(pl[:, 0:E], h2Tf[:, k, :], rwf[:, k * E:(k + 1) * E], k == 0, k == 15, [h2Tf, rwf], [pl])
                TT("dve", lg[:], pl[:, 0:E], rbb[:], ALU.add, [pl, rbb], [lg])
                P.op("dve", lambda e: e.max(out=mx8[:], in_=lg[:]), [lg], [mx8])
                TS("dve", gmask[:], lg[:], mx8[:, 3:4], None, ALU.is_ge, None, [lg, mx8], [gmask])
                TS("dve", small[:, 0:1], mx8[:, 0:1], -1.0, None, ALU.mult, None, [mx8], [small])
                ACT(gsm[:], lg[:], AF.Exp, [lg, small], [gsm], bias=small[:, 0:1])
                TT("dve", gsm[:], gsm[:], gmask[:], ALU.mult, [gsm, gmask], [gsm])
                P.op("dve", lambda e: e.tensor_reduce(out=small[:, 1:2], in_=gsm[:], axis=AX.X, op=ALU.add), [gsm], [small])
                RECIP(small[:, 1:2], small[:, 1:2], [small], [small])
                TS("dve", gsm[:], gsm[:], small[:, 1:2], None, ALU.mult, None, [gsm, small], [gsm])
                pt_ = nps()
                TR(pt_[0:E, 0:128], gsm[:], identf[:], [gsm, identf], [pt_])
                CP("act", GT[:, r0:r0 + 128], pt_[0:E, 0:128], [pt_], [GT])
                ACT(xn[:], xt0[:], AF.Identity, [xt0, ssq], [xn], scale=ssq[:, 2:3])
                pc = nps()
                MM(pc[:, 0:E], tri[:], gmask[:], True, True, [tri, gmask], [pc])
                MM(pc[:, E:2 * E], onesf[:], gmask[:], True, True, [onesf, gmask], [pc])
                slot, valid, idxm, rk, selk, tmpk = rt
                TT("dve", slot[:], pc[:, 0:E], basebc[:], ALU.add, [pc, basebc], [slot])
                TT("dve", basebc[:], basebc[:], pc[:, E:2 * E], ALU.add, [pc, basebc], [basebc])
                TS("dve", valid[:], slot[:], float(CAP), None, ALU.is_lt, None, [slot], [valid])
                TT("dve", valid[:], valid[:], gmask[:], ALU.mult, [valid, gmask], [valid])
                TT("dve", idxm[:], slot[:], ecap[:], ALU.add, [slot, ecap], [idxm])
                TS("dve", idxm[:], idxm[:], -BIGI, None, ALU.add, None, [idxm], [idxm])
                P.op("dve", lambda e: e.tensor_tensor_scan(out=rk[:], data0=onesf[:, 0:E], data1=gmask[:], initial=0.0,
                                                           op0=ALU.mult, op1=ALU.add), [onesf, gmask], [rk])
                TT("dve", rk[:], rk[:], gmask[:], ALU.subtract, [rk, gmask], [rk])
                for k in range(4):
                    TS("dve", selk[:], rk[:], float(k), None, ALU.is_equal, None, [rk], [selk])
                    TT("dve", selk[:], selk[:], valid[:], ALU.mult, [selk, valid], [selk])
                    TT("dve", tmpk[:], selk[:], idxm[:], ALU.mult, [selk, idxm], [tmpk])
                    P.op("dve", lambda e, k=k, ti=ti: e.tensor_reduce(out=IDXF[:, ti, k:k + 1], in_=tmpk[:], axis=AX.X, op=ALU.add), [tmpk], [IDXF])
                    TT("dve", tmpk[:], selk[:], gsm[:], ALU.mult, [selk, gsm], [tmpk])
                    P.op("dve", lambda e, k=k, ti=ti: e.tensor_reduce(out=GK[:, ti, k:k + 1], in_=tmpk[:], axis=AX.X, op=ALU.add), [tmpk], [GK])
                TS("dve", IDXF[:, ti, :], IDXF[:, ti, :], BIGI, None, ALU.add, None, [IDXF], [IDXF])
                CP("dve", IDXI[:, ti, :], IDXF[:, ti, :], [IDXF], [IDXI])
                for k in range(4):
                    P.dma("pool", lambda e, k=k, ti=ti: e.indirect_dma_start(
                        out=Xgd[:, :], out_offset=bass.IndirectOffsetOnAxis(ap=IDXI[:, ti, k:k + 1], axis=0),
                        in_=xn[:], in_offset=None, bounds_check=breg[0], oob_is_err=False), [xn, IDXI], [xgdram])
            if bi == 0:
                dump("lg", lg, lg[:], [128, E]); dump("gsm", gsm, gsm[:], [128, E])
                dump("idx", IDXI, IDXI[:, 1, :], [128, 4], mybir.dt.int32); dump("gk", GK, GK[:, 1, :], [128, 4])
                stop_at("b0")

        def actb_alias(v):
            b = Buf(v)
            abufs.append(b)
            return b

        stop_at("p1b")
        barrier()
        XgT = carve([128, 16, CAP], BF16); actb = carve([128, 16, CAP], BF16)
        xgs = [carve([128, D], BF16) for i in range(2)]
        ybuf = [carve([128, 512]) for i in range(4)]
        gtt = [carve([128, SBW]) for i in range(2)]; ltt = [carve([128, SBW])]; sgt = [carve([128, SBW])]
        NST = CAP // 128
        yi = [0]
        for e in range(E):
            for st_ in range(NST):
                xg = xgs[st_ % 2]
                DMA("sp", xg[:], Xgd[e * CAP + st_ * 128:e * CAP + (st_ + 1) * 128, :], R=[xgdram], W=[xg])
                for k4 in range(4):
                    p = nps()
                    pb = p[:].bitcast(BF16)
                    for kk in range(4):
                        k = k4 * 4 + kk
                        TR(pb[:, kk * 128:(kk + 1) * 128], xg[:, k * 128:(k + 1) * 128], identb[:], [xg, identb], [p])
                    for kk in range(4):
                        k = k4 * 4 + kk
                        ACT(XgT[:, k, st_ * 128:(st_ + 1) * 128], pb[:, kk * 128:(kk + 1) * 128], AF.Identity,
                            [p, scale2, adacol], [XgT], scale=scale2[:, k:k + 1], bias=sh2(k))
            for fb in range(8):
                wb = nwb()
                DMA("pool", wb[:, :, 0:256], w1g[e][:, fb * 256:(fb + 1) * 256].rearrange("(k p) n -> p k n", p=128), W=[wb])
                DMA("pool", wb[:, :, 256:512], w1l[e][:, fb * 256:(fb + 1) * 256].rearrange("(k p) n -> p k n", p=128), W=[wb])
                for fc in range(2):
                    f = fb * 2 + fc
                    for sb_ in range(CAP // SBW):
                        s0 = sb_ * SBW
                        pg_ = nps()
                        proj(pg_, pg_[:, 0:SBW], wb, fc * 128, XgT, lambda k: XgT[:, k, s0:s0 + SBW], 16)
                        pl_ = nps()
                        proj(pl_, pl_[:, 0:SBW], wb, 256 + fc * 128, XgT, lambda k: XgT[:, k, s0:s0 + SBW], 16)
                        g_ = gtt[(f + sb_) % 2]; l_ = ltt[0]; s_ = sgt[0]
                        TS("dve", g_[:], pg_[:, 0:SBW], V(V_B1G + e * 16 + f), 7.0, ALU.add, ALU.min, [pg_, vec], [g_])
                        ACT(s_[:], g_[:], AF.Sigmoid, [g_], [s_], scale=1.702)
                        TS("dve", l_[:], pl_[:, 0:SBW], V(V_B1L + e * 16 + f), 7.0, ALU.add, ALU.min, [pl_, vec], [l_])
                        TS("dve", l_[:], l_[:], -7.0, 1.0, ALU.max, ALU.add, [l_], [l_])
                        TT("dve", g_[:], g_[:], s_[:], ALU.mult, [g_, s_], [g_])
                        TT("dve", actb[:, f, s0:s0 + SBW], g_[:], l_[:], ALU.mult, [g_, l_], [actb])
            for db in range(4):
                wb = wload(w2d[e][:, db * 512:(db + 1) * 512], 16)
                for st_ in range(NST):
                    py = nps()
                    for k in range(16):
                        MM(py[:, :], actb[:, k, st_ * 128:(st_ + 1) * 128], wb[:, k, :], k == 0, k == 15, [actb, wb], [py])
                    yb = ybuf[yi[0] % 4]
                    yi[0] += 1
                    CP("act", yb[:], py[:, :], [py], [yb])
                    DMA("sp", Ygd[e * CAP + st_ * 128:e * CAP + (st_ + 1) * 128, db * 512:(db + 1) * 512], yb[:], R=[yb], W=[ygdram])
        barrier()
        facc = carve([128, D]); yk = [carve([128, D]) for i in range(2)]; junk = carve([128, D], BF16)
        b2s = carve([128, D])
        DMA("sp", b2s[0:E, :], b2d, W=[b2s])
        gi = [0]
        for bi in range(NT):
            r0 = bi * 128
            for nb in range(4):
                pbias = nps()
                MM(pbias[:, :], GT[:, r0:r0 + 128], b2s[0:E, nb * 512:(nb + 1) * 512], True, True, [GT, b2s], [pbias])
                CP("act", facc[:, nb * 512:(nb + 1) * 512], pbias[:, :], [pbias], [facc])
            for k in range(4):
                y_ = yk[gi[0] % 2]
                gi[0] += 1
                MEMSET("pool", y_[:], 0.0, [y_])
                P.dma("pool", lambda e, k=k, bi=bi, y_=y_: e.indirect_dma_start(
                    out=y_[:], out_offset=None, in_=Ygd[:, :],
                    in_offset=bass.IndirectOffsetOnAxis(ap=IDXI[:, bi, k:k + 1], axis=0),
                    bounds_check=breg[0], oob_is_err=False), [IDXI, ygdram], [y_])
                STT(facc[:], y_[:], GK[:, bi, k:k + 1], facc[:], ALU.mult, ALU.add, [y_, GK, facc], [facc])
            ACT(junk[:], facc[:], AF.Square, [facc], [junk, ssq], accum_out=ssq[:, 3:4])
            TS("dve", ssq[:, 3:4], ssq[:, 3:4], 1.0 / D, EPS, ALU.mult, ALU.add, [ssq], [ssq])
            ACT(ssq[:, 3:4], ssq[:, 3:4], AF.Sqrt, [ssq], [ssq])
            RECIP(ssq[:, 3:4], ssq[:, 3:4], [ssq], [ssq])
            STT(facc[:], facc[:], ssq[:, 3:4], pg2[:], ALU.mult, ALU.mult, [facc, ssq, pg2], [facc])
            DMA("sp", xt0[:], outd[r0:r0 + 128, :], R=[outdram], W=[xt0])
            TT("dve", xt0[:], xt0[:], facc[:], ALU.add, [xt0, facc], [xt0])
            DMA("sp", outd[r0:r0 + 128, :], xt0[:], R=[xt0], W=[outdram])

    try:
        body()
    except _Stop:
        pass

    sems = {}
    for k in ("pe", "act", "dve", "pool", "sp"):
        sems[("c", k)] = es.enter_context(nc.semaphore(f"c_{k}"))
    for q in ("sp", "pool"):
        for i in range(Prog.R):
            sems[("d", q, i)] = es.enter_context(nc.semaphore(f"d_{q}{i}"))

    breg = [None]

    def run(name, e):
        if name == "pool":
            breg[0] = e.to_reg(E * CAP - 1)
        for waits, fn, key, amt in P.ops[name]:
            for wk, wv in waits:
                e.wait_ge(sems[wk], wv)
            fn(e).then_inc(sems[key], amt)
        if name == "sp":
            n = P.dq["sp"]
            for i in range(Prog.R):
                cnt = (n - i + Prog.R - 1) // Prog.R
                if cnt > 0:
                    e.wait_ge(sems[("d", "sp", i)], 16 * cnt)

    with es:
        with nc.Block() as block:
            @block.tensor
            def _(e):
                run("pe", e)

            @block.scalar
            def _(e):
                run("act", e)

            @block.vector
            def _(e):
                run("dve", e)

            @block.gpsimd
            def _(e):
                run("pool", e)

            @block.sync
            def _(e):
                run("sp", e)
    return nc


def _prep(inputs):
    f32 = np.float32
    g = {k: np.asarray(v) for k, v in inputs.items()}
    x = g["x"]; c = g["c"]
    sh = {}
    sh["ada_w"] = np.ascontiguousarray(g["ada_w"][0]); sh["ada_b"] = np.ascontiguousarray(g["ada_b"][0][None, :])
    def fm(v, n):
        return np.ascontiguousarray(v.reshape(n, 128).T)
    vec = np.zeros((128, NV), f32)
    vec[:, V_PREMIX:V_PREMIX + 16] = fm(g["pre_mix_g"][0], 16)
    vec[:, V_PREFFN:V_PREFFN + 16] = fm(g["pre_ffn_g"][0], 16)
    vec[:, V_GATEB:V_GATEB + 32] = fm(g["gate_b"][0], 32)
    vec[:, V_DWB:V_DWB + 8] = fm(g["dw_b"][0], 8)
    vec[:, V_CLNG:V_CLNG + 8] = fm(g["cln_g"][0], 8)
    vec[:, V_CLNB:V_CLNB + 8] = fm(g["cln_b"][0], 8)
    vec[:, V_CPB:V_CPB + 16] = fm(g["cp_b"][0], 16)
    vec[:, V_S5D:V_S5D + 8] = fm(g["s5_d"][0], 8)
    dww = g["dw_w"][0]
    vec[:, V_DWW:V_DWW + 248] = dww.T.reshape(8, 128, 31).transpose(1, 0, 2).reshape(128, 248)
    b1 = g["b1"][0]
    vec[:, V_B1G:V_B1G + 512] = b1[:, 0::2].reshape(32, 16, 128).transpose(2, 0, 1).reshape(128, 512)
    vec[:, V_B1L:V_B1L + 512] = b1[:, 1::2].reshape(32, 16, 128).transpose(2, 0, 1).reshape(128, 512)
    vec[:, V_B2:V_B2 + 512] = g["b2"][0].reshape(32, 16, 128).transpose(2, 0, 1).reshape(128, 512)
    pidx = (np.arange(128) // 32) % 2
    vec[:, V_M0] = (pidx == 0).astype(f32); vec[:, V_M0 + 1] = (pidx == 1).astype(f32)
    sh["vec"] = vec
    sh["routerb"] = np.ascontiguousarray(g["router_b"][0][None, :])
    sh["postmix"] = np.ascontiguousarray(g["post_mix_g"][0][None, :])
    sh["postffn"] = np.ascontiguousarray(g["post_ffn_g"][0][None, :])
    are, aim, ldt = g["s5_a_re"][0], g["s5_a_im"][0], g["s5_log_dt"][0]
    ldt2 = np.broadcast_to(ldt[:, None], (64, 64))
    def Sl(a):
        return a.reshape(32, 2, 64).transpose(1, 2, 0).reshape(128, 32)
    sh["aS"] = np.ascontiguousarray(np.concatenate([Sl(are), Sl(aim), Sl(ldt2)], axis=1).astype(f32))
    def Pl(a):
        t = a.reshape(8, 4, 2, 64)
        t = t.transpose(1, 0, 2, 3)
        t = np.broadcast_to(t[:, None, None], (4, 2, 16, 8, 2, 64))
        return t.reshape(128, 1024)
    sh["aP"] = np.ascontiguousarray(np.concatenate([Pl(are), Pl(aim), Pl(ldt2)], axis=1).astype(f32))
    def Bexp(b):
        t = b.reshape(8, 4, 2, 64, 16)
        o = np.zeros((4, 2, 16, 8, 2, 64), f32)
        for gl in range(2):
            o[:, gl, :, :, gl, :] = t[:, :, gl].transpose(1, 3, 0, 2)
        return o.reshape(128, 1024)
    sh["bexp"] = np.concatenate([Bexp(g["s5_b_re"][0]), Bexp(g["s5_b_im"][0])], axis=1)
    def Cexp(cc):
        t = cc.reshape(32, 2, 16, 64)
        o = np.zeros((2, 64, 32, 2, 2, 16), f32)
        for kt in range(32):
            for gl in range(2):
                o[gl, :, kt, kt % 2, gl, :] = t[kt, gl].T
        return o.reshape(128, 2048)
    sh["cexp"] = np.concatenate([Cexp(g["s5_c_re"][0]), Cexp(g["s5_c_im"][0])], axis=1)
    sh["w_in"] = np.ascontiguousarray(g["w_in"][0]); sh["cp_w"] = np.ascontiguousarray(g["cp_w"][0])
    sh["glu_wa"] = np.ascontiguousarray(g["glu_wa"][0]); sh["glu_wb"] = np.ascontiguousarray(g["glu_wb"][0])
    sh["w_out"] = np.ascontiguousarray(g["w_out"][0]); sh["router_w"] = np.ascontiguousarray(g["router_w"][0])
    w1 = g["w1"][0]
    sh["w1g"] = np.ascontiguousarray(w1[:, :, 0::2]); sh["w1l"] = np.ascontiguousarray(w1[:, :, 1::2])
    sh["w2"] = np.ascontiguousarray(g["w2"][0])
    sh["ident"] = np.eye(128, dtype=f32)
    sh["tri"] = np.triu(np.ones((128, 128), f32), 1)
    sh["ecap"] = np.broadcast_to((np.arange(E, dtype=f32) * CAP)[None, :], (128, E)).copy()
    sh["b2s"] = np.ascontiguousarray(g["b2"][0])
    maps = []
    for core in range(8):
        b, half = core // 2, core % 2
        m = dict(sh)
        m["xo"] = np.ascontiguousarray(x[b, half * NTOK:(half + 1) * NTOK])
        m["xp"] = np.ascontiguousarray(x[b, 0:NTOK]) if half == 1 else np.zeros((NTOK, D), f32)
        m["flag"] = np.full((128, 1), float(half), f32)
        m["ccol"] = fm(c[b], 16)
        maps.append(m)
    return maps


def kernel(**inputs):
    maps = _prep(inputs)
    nc = build_nc()
    res = run_bass_kernel_spmd(nc, maps, core_ids=list(range(8)))
    out = np.zeros((4, 4096, D), np.float32)
    for core in range(8):
        b, half = core // 2, core % 2
        out[b, half * NTOK:(half + 1) * NTOK] = res.results[core]["out"]
    return out
```

```python
import math
from contextlib import ExitStack
import numpy as np
import concourse.bass as bass
import concourse.mybir as mybir
from concourse.bass_utils import run_bass_kernel_spmd

F32 = mybir.dt.float32
BF16 = mybir.dt.bfloat16
AF = mybir.ActivationFunctionType
ALU = mybir.AluOpType
AX = mybir.AxisListType

D = 2048
NTOK = 2048
TB = 256
NT = NTOK // 128
SBK = 16
NBLK = NTOK // TB
E = 32
MTB = 512
CAP = 768
SBW = 384
BIGI = 4000000.0
EPS = 1e-6

V_PREMIX, V_PREFFN, V_GATEB, V_DWB, V_CLNG, V_CLNB, V_CPB, V_S5D = 0, 16, 32, 64, 72, 80, 88, 104
V_DWW = 112
V_B1G = V_DWW + 8 * 31
V_B1L = V_B1G + 512
V_B2 = V_B1L + 512
V_M0 = V_B2 + 512
NV = V_M0 + 2


class Buf:

    def __init__(self, t):
        self.t = t
        self.w = None
        self.r = {}

    def __getitem__(self, idx):
        return self.t[idx]


class Alias(Buf):
    def __init__(self, parent, t):
        self.p = parent
        self.t = t

    @property
    def w(self):
        return self.p.w

    @w.setter
    def w(self, v):
        self.p.w = v

    @property
    def r(self):
        return self.p.r

    @r.setter
    def r(self, v):
        self.p.r = v


class Prog:
    R = 8

    def __init__(self):
        self.ops = {k: [] for k in ("pe", "act", "dve", "pool", "sp")}
        self.cnt = {k: 0 for k in self.ops}
        self.known = {k: {} for k in self.ops}
        self.dq = {"sp": 0, "pool": 0}

    def _emit(self, eng, fn, R, W, key, amt, val, extra=()):
        need = {}

        def add(tok):
            if tok is None:
                return
            k, v = tok
            if need.get(k, 0) < v:
                need[k] = v

        for b in R:
            add(b.w)
        for b in W:
            add(b.w)
            for k, v in b.r.items():
                add((k, v))
        for t in extra:
            add(t)
        waits = []
        for k, v in need.items():
            if k == ("c", "pe") and eng == "pe":
                continue
            if self.known[eng].get(k, 0) >= v:
                continue
            self.known[eng][k] = v
            waits.append((k, v))
        self.ops[eng].append((waits, fn, key, amt))
        tok = (key, val)
        for b in W:
            b.w = tok
            b.r = {}
        for b in R:
            if b not in W and b.r.get(key, 0) < val:
                b.r[key] = val
        return tok

    def op(self, eng, fn, R=(), W=()):
        self.cnt[eng] += 1
        return self._emit(eng, fn, R, W, ("c", eng), 1, self.cnt[eng])

    def dma(self, q, fn, R=(), W=()):
        j = self.dq[q]
        self.dq[q] += 1
        key = ("d", q, j % self.R)
        extra = []
        if j >= self.R:
            extra.append((key, 16 * (j // self.R)))
        return self._emit(q, fn, R, W, key, 16, 16 * (j // self.R + 1), extra)


class _Stop(Exception):
    pass


def build_nc(stop=None, dumps=()):
    nc = bass.Bass("TRN2", target_bir_lowering=False)
    P = Prog()
    dbg = {}

    def dump(name, B, ap, shape, dt=F32):
        if name not in dumps:
            return
        t = nc.dram_tensor("dbg_" + name, list(shape), dt, kind="ExternalOutput").ap()
        P.dma("sp", lambda e: e.dma_start(out=t, in_=ap), [B], ())

    def stop_at(tag):
        if stop == tag:
            raise _Stop()

    def din(name, shape, dt=F32):
        return nc.dram_tensor(name, list(shape), dt, kind="ExternalInput").ap()

    xo = din("xo", [NTOK, D]); xp = din("xp", [NTOK, D]); flagd = din("flag", [128, 1])
    ccold = din("ccol", [128, 16]); adaw = din("ada_w", [D, 6 * D]); adab = din("ada_b", [1, 6 * D])
    vecd = din("vec", [128, NV]); routerbd = din("routerb", [1, E])
    postmixd = din("postmix", [1, D]); postffnd = din("postffn", [1, D])
    aSd = din("aS", [128, 96]); aPd = din("aP", [128, 3072])
    bexpd = din("bexp", [128, 2048]); cexpd = din("cexp", [128, 4096])
    win = din("w_in", [D, 7168]); cpw = din("cp_w", [1024, D]); gwa = din("glu_wa", [1024, D])
    gwb = din("glu_wb", [1024, D]); wout = din("w_out", [D, D]); rwd = din("router_w", [D, E])
    w1g = din("w1g", [E, D, 2048]); w1l = din("w1l", [E, D, 2048]); w2d = din("w2", [E, 2048, D])
    identd = din("ident", [128, 128]); trid = din("tri", [128, 128]); ecapd = din("ecap", [128, E]); b2d = din("b2s", [E, D])
    outd = nc.dram_tensor("out", [NTOK, D], F32, kind="ExternalOutput").ap()
    Xgd = nc.dram_tensor("xg_scratch", [E * CAP, D], BF16, kind="Internal").ap()
    Ygd = nc.dram_tensor("yg_scratch", [E * CAP, D], F32, kind="Internal").ap()

    es = ExitStack()
    h2dram = Buf(None); outdram = Buf(None); xgdram = Buf(None); ygdram = Buf(None)

    def sb(name, shape, dt=F32):
        return Buf(es.enter_context(nc.sbuf_tensor(name, list(shape), dt)))

    ps = [Buf(es.enter_context(nc.psum_tensor(f"ps{i}", [128, 512], F32))) for i in range(8)]
    psi = [0]

    def nps():
        p = ps[psi[0] % 6]
        psi[0] += 1
        return p

    def DMA(q, out, in_, R=(), W=()):
        P.dma(q, lambda e: e.dma_start(out=out, in_=in_), R, W)

    def ACT(out, in_, func, R, W, **kw):
        P.op("act", lambda e: e.activation(out=out, in_=in_, func=func, **kw), R, W)

    def TT(eng, out, a, b, op, R, W):
        P.op(eng, lambda e: e.tensor_tensor(out=out, in0=a, in1=b, op=op), R, W)

    def TS(eng, out, a, s1, s2, op0, op1, R, W):
        if op1 is None:
            P.op(eng, lambda e: e.tensor_scalar(out=out, in0=a, scalar1=s1, scalar2=None, op0=op0), R, W)
        else:
            P.op(eng, lambda e: e.tensor_scalar(out=out, in0=a, scalar1=s1, scalar2=s2, op0=op0, op1=op1), R, W)

    def STT(out, a, s, b, op0, op1, R, W):
        P.op("dve", lambda e: e.scalar_tensor_tensor(out=out, in0=a, scalar=s, in1=b, op0=op0, op1=op1), R, W)

    def MM(out, l, r, st, sp_, R, W):
        P.op("pe", lambda e: e.matmul(out, l, r, start=st, stop=sp_), R, W)

    def TR(out, in_, ident, R, W):
        P.op("pe", lambda e: e.transpose(out, in_, ident), R, W)

    def CP(eng, out, in_, R, W):
        if eng == "act":
            P.op(eng, lambda e: e.activation(out=out, in_=in_, func=AF.Identity), R, W)
        else:
            P.op(eng, lambda e: e.tensor_copy(out=out, in_=in_), R, W)

    def RECIP(out, in_, R, W):
        P.op("dve", lambda e: e.reciprocal(out=out, in_=in_), R, W)

    def MEMSET(eng, ap, c, W):
        P.op(eng, lambda e: e.memset(ap, c), (), W)

    identf = sb("identf", [128, 128]); identb = sb("identb", [128, 128], BF16)
    onesf = sb("onesf", [128, 128]); onesm = sb("onesm", [128, 128])
    vec = sb("vecs", [128, NV]); flag = sb("flagt", [128, 1])
    ccol = sb("ccolt", [128, 16]); silub = sb("silub", [128, 16], BF16)
    adacol = sb("adacol", [128, 96]); scale1 = sb("scale1", [128, 16]); scale2 = sb("scale2", [128, 16])
    pg1 = sb("pg1", [128, D]); pg2 = sb("pg2", [128, D])
    rbb = sb("rbb", [128, E]); rwf = sb("rwf", [128, 16 * E])
    wblk = [sb(f"wblk{i}", [128, 16, 512], BF16) for i in range(2)]
    wbi = [0]

    def nwb():
        w = wblk[wbi[0] % 2]
        wbi[0] += 1
        return w

    xin = [sb("xin0", [128, D])]
    ssq = sb("ssq", [128, 4]); small = sb("small", [128, 16])
    Mt = sb("Mt", [128, 2, 2, 32]); Rt = sb("Rt", [128, 2, 32]); AA = sb("AA", [128, 2, 2, 32])
    lg = sb("lg", [128, E]); mx8 = sb("mx8", [128, 8]); gsm = sb("gsm", [128, E]); gmask = sb("gmask", [128, E])
    GT = sb("GT", [32, NTOK])
    tri = sb("tri_t", [128, 128]); ecap = sb("ecap_t", [128, E]); basebc = sb("basebc", [128, E])
    IDXF = sb("IDXF", [128, NT, 4]); GK = sb("GK", [128, NT, 4]); IDXI = sb("IDXI", [128, NT, 4], mybir.dt.int32)
    rt = [sb(f"rt{i}", [128, E]) for i in range(6)]
    BbTm = [[sb(f"BbTm{i}{j}", [128, 8, 128], BF16) for j in range(2)] for i in range(2)]
    CT = [sb(f"CT{i}", [128, 32, 64], BF16) for i in range(2)]
    aS = sb("aSt", [128, 96]); stmp = sb("stmp", [128, 12 * 32])
    bars = {k: sb(f"bar_{k}", [128, 4]) for k in ("dve", "act", "pool")}
    AW = 22016
    arena = es.enter_context(nc.sbuf_tensor("arena", [128, AW], F32))
    apos = [0]
    abufs = []

    def carve(shape, dt=F32):
        n = 1
        for d_ in shape[1:]:
            n *= d_
        words = n if dt == F32 else (n + 1) // 2
        o = apos[0]
        apos[0] += words
        assert apos[0] <= AW, ("arena overflow", apos[0])
        v = arena[:, o:o + words]
        if dt != F32:
            v = v.bitcast(dt)
        if len(shape) == 3:
            v = v.rearrange("p (a b) -> p a b", a=shape[1])
        elif len(shape) == 4:
            v = v.rearrange("p (a b c) -> p a b c", a=shape[1], b=shape[2])
        b = Buf(v)
        abufs.append(b)
        return b

    def barrier():
        W0 = list(abufs)
        P.op("dve", lambda e: e.memset(bars["dve"][:], 0.0), (), [bars["dve"]] + W0)
        P.op("pool", lambda e: e.memset(bars["pool"][:], 0.0), (), [bars["pool"]])
        P.op("act", lambda e: e.activation(out=bars["act"][:], in_=flag[:, 0:1].broadcast_to([128, 4]), func=AF.Identity), [flag], [bars["act"]])
        pb_ = nps()
        MM(pb_[0:1, 0:2], onesf[0:1, 0:1], onesf[0:1, 0:2], True, True, [onesf], [pb_])
        allb = [bars["dve"], bars["pool"], bars["act"], pb_]
        P.op("dve", lambda e: e.memset(small[:, 8:9], 0.0), allb, [small])
        P.op("pool", lambda e: e.memset(small[:, 9:10], 0.0), allb + [small], [small])
        P.op("act", lambda e: e.activation(out=small[:, 10:11], in_=flag[:, 0:1], func=AF.Identity), allb + [small], [small])
        pb2 = nps()
        MM(pb2[0:1, 0:2], onesf[0:1, 0:1], onesf[0:1, 0:2], True, True, [onesf, small] + allb, [pb2])
        DMA("sp", flag[:], flagd, R=[small, pb2], W=[flag])
        DMA("pool", ssq[:, 3:4], flagd, R=[small, pb2], W=[ssq])
        del abufs[:]
        apos[0] = 0

    rowb = carve([1 + 127, 512]); adabk = carve([128, 512])
    fT0 = carve([128, 8, 1024]); merged0 = carve([128, 3072]); ys0 = carve([128, 2048]); cv0 = carve([128, 4096])
    BbT = [carve([128, 8, 128], BF16) for i in range(2)]

    def V(off, n=1):
        return vec[:, off:off + n]

    def body():
        DMA("sp", identf[:], identd, W=[identf])
        DMA("sp", vec[:], vecd, W=[vec])
        DMA("sp", flag[:], flagd, W=[flag])
        DMA("sp", ccol[:], ccold, W=[ccol])
        DMA("sp", rwf[:].rearrange("p (k n) -> p k n", k=16), rwd.rearrange("(k p) n -> p k n", p=128), W=[rwf])
        DMA("sp", rbb[:], routerbd.broadcast_to([128, E]), W=[rbb])
        DMA("sp", pg1[:], postmixd.broadcast_to([128, D]), W=[pg1])
        DMA("sp", pg2[:], postffnd.broadcast_to([128, D]), W=[pg2])
        DMA("sp", aS[:], aSd, W=[aS])
        DMA("sp", tri[:], trid, W=[tri])
        DMA("sp", ecap[:], ecapd, W=[ecap])
        MEMSET("dve", basebc[:], 0.0, [basebc])
        CP("dve", identb[:], identf[:], [identf], [identb])
        MEMSET("dve", onesf[:], 1.0, [onesf])
        MEMSET("dve", onesm[:], 1.0 / 1024.0, [onesm])
        ACT(silub[:], ccol[:], AF.Silu, [ccol], [silub])

        for cb in range(24):
            wb = nwb()
            DMA("pool", wb[:], adaw[:, cb * 512:(cb + 1) * 512].rearrange("(k p) n -> p k n", p=128), W=[wb])
            DMA("sp", adabk[0:1, :], adab[0:1, cb * 512:(cb + 1) * 512], W=[adabk])
            p = nps()
            for k in range(16):
                MM(p[0:1, :], silub[:, k:k + 1], wb[:, k, :], k == 0, k == 15, [silub, wb], [p])
            TT("dve", rowb[0:1, :], p[0:1, :], adabk[0:1, :], ALU.add, [p, adabk], [rowb])
            p2 = nps()
            for j in range(4):
                MM(p2[:, j:j + 1], rowb[0:1, j * 128:(j + 1) * 128], onesf[0:1, 0:1], True, True, [rowb, onesf], [p2])
            CP("act", adacol[:, cb * 4:cb * 4 + 4], p2[:, 0:4], [p2], [adacol])
            if 8 <= cb < 12 or 20 <= cb < 24:
                pgx, c0 = (pg1, (cb - 8) * 512) if cb < 12 else (pg2, (cb - 20) * 512)
                p3 = nps()
                MM(p3[:, :], onesf[0:1, :], rowb[0:1, :], True, True, [rowb, onesf], [p3])
                TT("dve", pgx[:, c0:c0 + 512], pgx[:, c0:c0 + 512], p3[:, :], ALU.mult, [pgx, p3], [pgx])
        TS("dve", small[:, 0:16], adacol[:, 16:32], 1.0, None, ALU.add, None, [adacol], [small])
        TT("dve", scale1[:], small[:, 0:16], V(V_PREMIX, 16), ALU.mult, [small, vec], [scale1])
        TS("dve", small[:, 0:16], adacol[:, 64:80], 1.0, None, ALU.add, None, [adacol], [small])
        TT("dve", scale2[:], small[:, 0:16], V(V_PREFFN, 16), ALU.mult, [small, vec], [scale2])
        sh1 = lambda k: adacol[:, k:k + 1]
        sh2 = lambda k: adacol[:, 48 + k:49 + k]

        def abar(aR, aI, lD, t, Bs, B):
            dt_, x_, rho, th, c_, s_, t1, t2 = t
            ACT(dt_, lD, AF.Exp, Bs, [B])
            TT("dve", x_, aR, dt_, ALU.mult, Bs, [B])
            ACT(rho, x_, AF.Exp, [B], [B])
            TT("dve", th, aI, dt_, ALU.mult, Bs, [B])
            ACT(s_, th, AF.Sin, [B], [B], scale=1.0 / 32.0)
            TS("dve", x_, th, 1.0 / 32.0, math.pi / 2, ALU.mult, ALU.add, [B], [B])
            ACT(c_, x_, AF.Sin, [B], [B])
            for _ in range(5):
                TT("dve", t1, c_, c_, ALU.mult, [B], [B])
                TT("dve", t2, s_, s_, ALU.mult, [B], [B])
                TT("dve", x_, c_, s_, ALU.mult, [B], [B])
                TT("dve", c_, t1, t2, ALU.subtract, [B], [B])
                TS("dve", s_, x_, 2.0, None, ALU.mult, None, [B], [B])
            TT("dve", dt_, rho, c_, ALU.mult, [B], [B])
            TT("dve", x_, rho, s_, ALU.mult, [B], [B])
            return dt_, x_

        st = [stmp[:, i * 32:(i + 1) * 32] for i in range(8)]
        abr, abi = abar(aS[:, 0:32], aS[:, 32:64], aS[:, 64:96], st, [stmp, aS], stmp)
        CP("dve", AA[:, 0, 0, :], abr, [stmp], [AA])
        CP("dve", AA[:, 1, 1, :], abr, [stmp], [AA])
        CP("dve", AA[:, 1, 0, :], abi, [stmp], [AA])
        TS("dve", AA[:, 0, 1, :], abi, -1.0, None, ALU.mult, None, [stmp], [AA])
        pt = [fT0[:, i, :] for i in range(8)]
        DMA("sp", merged0[:], aPd, W=[merged0])
        DMA("sp", ys0[:], bexpd, W=[ys0])
        DMA("sp", cv0[:], cexpd, W=[cv0])
        aRp, aIp, lDp = merged0[:, 0:1024], merged0[:, 1024:2048], merged0[:, 2048:3072]
        abrP, abiP = abar(aRp, aIp, lDp, pt, [fT0, merged0], fT0)
        _, _, q2, q3, q4, q5, q6, q7 = pt
        Bs = [fT0, merged0]
        TT("dve", q2, aRp, aRp, ALU.mult, Bs, [fT0])
        TT("dve", q3, aIp, aIp, ALU.mult, Bs, [fT0])
        TT("dve", q2, q2, q3, ALU.add, [fT0], [fT0])
        RECIP(q2, q2, [fT0], [fT0])
        TS("dve", q3, abrP, -1.0, None, ALU.add, None, [fT0], [fT0])
        TT("dve", q4, q3, aRp, ALU.mult, Bs, [fT0])
        TT("dve", q5, abiP, aIp, ALU.mult, Bs, [fT0])
        TT("dve", q4, q4, q5, ALU.add, [fT0], [fT0])
        TT("dve", q4, q4, q2, ALU.mult, [fT0], [fT0])
        TT("dve", q5, abiP, aRp, ALU.mult, Bs, [fT0])
        TT("dve", q6, q3, aIp, ALU.mult, Bs, [fT0])
        TT("dve", q5, q5, q6, ALU.subtract, [fT0], [fT0])
        TT("dve", q5, q5, q2, ALU.mult, [fT0], [fT0])
        bre, bim = ys0[:, 0:1024], ys0[:, 1024:2048]
        Bs2 = [fT0, ys0]
        TT("dve", q6, q4, bre, ALU.mult, Bs2, [fT0])
        TT("dve", q7, q5, bim, ALU.mult, Bs2, [fT0])
        TT("dve", BbT[0][:].rearrange("p k n -> p (k n)"), q6, q7, ALU.subtract, [fT0], [BbT[0]])
        TT("dve", q6, q4, bim, ALU.mult, Bs2, [fT0])
        TT("dve", q7, q5, bre, ALU.mult, Bs2, [fT0])
        TT("dve", BbT[1][:].rearrange("p k n -> p (k n)"), q6, q7, ALU.add, [fT0], [BbT[1]])
        CP("dve", CT[0][:].rearrange("p k n -> p (k n)"), cv0[:, 0:2048], [cv0], [CT[0]])
        TS("dve", CT[1][:].rearrange("p k n -> p (k n)"), cv0[:, 2048:4096], -1.0, None, ALU.mult, None, [cv0], [CT[1]])
        for ri in range(2):
            for j in range(2):
                TS("dve", BbTm[ri][j][:], BbT[ri][:], V(V_M0 + j), None, ALU.mult, None, [BbT[ri], vec], [BbTm[ri][j]])

        dump("adacol", adacol, adacol[:], [128, 96]); dump("AA", AA, AA[:], [128, 2, 2, 32]); dump("pg1", pg1, pg1[:], [128, D])
        dump("BbT0", BbT[0], BbT[0][:], [128, 8, 128], BF16); dump("BbT1", BbT[1], BbT[1][:], [128, 8, 128], BF16)
        dump("CT0", CT[0], CT[0][:], [128, 32, 64], BF16); dump("scale1", scale1, scale1[:], [128, 16])
        stop_at("p0")
        barrier()
        xn = carve([128, D], BF16); hT = carve([128, 16, TB], BF16)
        vT = carve([128, 8, 30 + TB]); cv = carve([128, 8, TB])
        sqt = [carve([128, TB]) for i in range(2)]
        meansb = carve([128, TB]); rstdsb = carve([128, TB]); tmpa = [carve([128, TB]) for i in range(3)]
        vn = carve([128, 8, TB], BF16); uT = carve([128, 8, TB], BF16)
        ys = carve([128, 8, TB]); gy = carve([128, 8, TB], BF16)
        Mt3 = carve([128, SBK * 2 * 32 * 3]); sS = [carve([128, 2, 32, SBK]) for i in range(2)]
        M5 = Mt3[:].rearrange("p (t o k i) -> p t o k i", t=SBK, o=2, k=32)
        AAp = AA[:].rearrange("p o i k -> p o k i")
        sbf = carve([128, 2, 32, SBK], BF16)
        merged = carve([128, 16, TB], BF16)
        msb = Alias(cv, cv.t.rearrange("p a b -> p (a b)"))
        h2Tf = Alias(hT, hT.t.rearrange("p a b -> p (a b)").bitcast(F32).rearrange("p (a b) -> p a b", a=16))
        xt0 = xin[0]

        def rms_rstd(src_ap, srcB, col):
            ACT(xn[:], src_ap, AF.Square, [srcB], [xn, ssq], accum_out=ssq[:, col:col + 1])
            TS("dve", ssq[:, col:col + 1], ssq[:, col:col + 1], 1.0 / D, EPS, ALU.mult, ALU.add, [ssq], [ssq])
            ACT(ssq[:, col:col + 1], ssq[:, col:col + 1], AF.Sqrt, [ssq], [ssq])
            RECIP(ssq[:, col:col + 1], ssq[:, col:col + 1], [ssq], [ssq])

        def norm_T_bf(src_ap, srcB, scale_t, shf, dst, tt):
            rms_rstd(src_ap, srcB, 0)
            ACT(xn[:], src_ap, AF.Identity, [srcB, ssq], [xn], scale=ssq[:, 0:1])
            for k4 in range(4):
                p = nps()
                pb = p[:].bitcast(BF16)
                for kk in range(4):
                    k = k4 * 4 + kk
                    TR(pb[:, kk * 128:(kk + 1) * 128], xn[:, k * 128:(k + 1) * 128], identb[:], [xn, identb], [p])
                for kk in range(4):
                    k = k4 * 4 + kk
                    ACT(dst[:, k, tt * 128:(tt + 1) * 128], pb[:, kk * 128:(kk + 1) * 128], AF.Identity,
                        [p, scale_t, adacol], [dst], scale=scale_t[:, k:k + 1], bias=shf(k))

        def proj(pB_, pout, wb, c0, rhsB, rhs_fn, nk):
            for k in range(nk):
                MM(pout, wb[:, k, c0:c0 + 128], rhs_fn(k), k == 0, k == nk - 1, [wb, rhsB], [pB_])

        def wload(src_ap, nk, ncol=512):
            wb = nwb()
            DMA("pool", wb[:, 0:nk, 0:ncol], src_ap.rearrange("(k p) n -> p k n", p=128), W=[wb])
            return wb

        MEMSET("dve", sS[1][:], 0.0, [sS[1]])
        MEMSET("dve", vT[:], 0.0, [vT])
        sidx = [0]
        NS = TB // SBK

        def s5_scan_gen(with_y):
            for sbk in range(NS):
                t0 = sbk * SBK
                for ri in range(2):
                    pp = [nps(), nps()]
                    pvs = [pp_[:, 0:16 * SBK].rearrange("p (q j n) -> p q j n", q=8, j=2) for pp_ in pp]
                    for kt in range(32):
                        q, ktl = kt // 4, kt % 4
                        h2_, j_ = ktl // 2, ktl % 2
                        MM(pvs[h2_][:, q, j_, :], BbTm[ri][j_][64 * h2_:64 * h2_ + 64, q, :], uT[64 * h2_:64 * h2_ + 64, q, t0:t0 + SBK],
                           True, True, [BbTm[ri][j_], uT], [pp[h2_]])
                    buv = M5[:, :, ri, :, 2].rearrange("p t (q k) -> p q k t", k=4)
                    for h2_ in range(2):
                        CP("act", buv[:, :, 2 * h2_:2 * h2_ + 2, :], pvs[h2_], [pp[h2_]], [Mt3])
                cur = sS[sidx[0] % 2]
                prv = sS[(sidx[0] + 1) % 2]
                sidx[0] += 1
                for t in range(SBK):
                    if t == 0:
                        pa, pBuf = prv[:, :, :, SBK - 1], prv
                    else:
                        pa, pBuf = cur[:, :, :, t - 1], cur
                    pin = pa.rearrange("p i k -> p k i").unsqueeze(1).broadcast_to([128, 2, 32, 2])
                    TT("dve", M5[:, t, :, :, 0:2], AAp, pin, ALU.mult, [AA, pBuf], [Mt3])
                    P.op("dve", lambda e, t=t, cur=cur: e.tensor_reduce(out=cur[:, :, :, t], in_=M5[:, t], axis=AX.X, op=ALU.add), [Mt3], [cur])
                if with_y:
                    CP("act", sbf[:], cur[:], [cur], [sbf])
                    p = nps()
                    pv = p[:, 0:8 * SBK].rearrange("p (a n) -> p a n", a=8)
                    for kt in range(32):
                        q, ktl = kt // 4, kt % 4
                        h2_, j_ = ktl // 2, ktl % 2
                        MM(pv[64 * h2_:64 * h2_ + 64, q, :], CT[0][:, kt, :], sbf[:, 0, kt, :], j_ == 0, False, [CT[0], sbf], [p])
                        MM(pv[64 * h2_:64 * h2_ + 64, q, :], CT[1][:, kt, :], sbf[:, 1, kt, :], False, j_ == 1, [CT[1], sbf], [p])
                    for q in range(8):
                        STT(ys[:, q, t0:t0 + SBK], uT[:, q, t0:t0 + SBK], V(V_S5D + q), pv[:, q, :], ALU.mult, ALU.add,
                            [uT, vec, p], [ys])
                yield

        def s5_scan_block(with_y):
            for _ in s5_scan_gen(with_y):
                pass

        def u_proj():
            for half in range(2):
                wb = wload(win[:, 2048 + half * 512:2048 + (half + 1) * 512], 16)
                for jj in range(4):
                    p = nps()
                    proj(p, p[:, 0:TB], wb, jj * 128, hT, lambda k: hT[:, k, :], 16)
                    CP("act", uT[:, half * 4 + jj, :], p[:, 0:TB], [p], [uT])

        def conv_glu_proj():
            for half in range(2):
                wbv = wload(win[:, half * 512:(half + 1) * 512], 16)
                wbg = wload(win[:, 1024 + half * 512:1024 + (half + 1) * 512], 16)
                for jj in range(4):
                    j = half * 4 + jj
                    pv_ = nps()
                    proj(pv_, pv_[:, 0:TB], wbv, jj * 128, hT, lambda k: hT[:, k, :], 16)
                    pg_ = nps()
                    proj(pg_, pg_[:, 0:TB], wbg, jj * 128, hT, lambda k: hT[:, k, :], 16)
                    sg = sqt[j % 2]
                    ACT(sg[:], pg_[:, 0:TB], AF.Sigmoid, [pg_], [sg])
                    TT("dve", vT[:, j, 30:30 + TB], pv_[:, 0:TB], sg[:], ALU.mult, [pv_, sg], [vT])
                    yield

        for bi in range(NBLK):
            for tt in range(TB // 128):
                r0 = bi * TB + tt * 128
                DMA("sp", xt0[:], xp[r0:r0 + 128, :], W=[xt0])
                norm_T_bf(xt0[:], xt0, scale1, sh1, hT, tt)
            if bi == 0:
                stop_at("n0")
            u_proj()
            if bi == 0:
                stop_at("u0")
            s5_scan_block(False)
            if bi == 0:
                stop_at("s0")
            if bi == NBLK - 1:
                for _ in conv_glu_proj():
                    pass
        last = sS[(sidx[0] + 1) % 2]
        TS("dve", last[:, :, :, SBK - 1], last[:, :, :, SBK - 1], flag[:, 0:1], None, ALU.mult, None, [last, flag], [last])
        TS("dve", vT[:, :, 0:30], vT[:, :, TB:TB + 30], flag[:, 0:1], None, ALU.mult, None, [vT, flag], [vT])

        dump("last", last, last[:], [128, 2, 32, SBK]); dump("halo", vT, vT[:], [128, 8, 30 + TB])
        stop_at("p1a")
        for bi in range(NBLK):
            for tt in range(TB // 128):
                r0 = bi * TB + tt * 128
                DMA("sp", xt0[:], xo[r0:r0 + 128, :], W=[xt0])
                norm_T_bf(xt0[:], xt0, scale1, sh1, hT, tt)
            if bi == 0:
                dump("hT", hT, hT[:], [128, 16, TB], BF16)
            def gen_def(bi=bi):
                yield from conv_glu_proj()
                pmean = ps[6]; pmsq = ps[7]
                for j in range(8):
                    acc = cv[:, j, :]
                    TS("dve", acc, vT[:, j, 0:TB], V(V_DWW + j * 31), V(V_DWB + j), ALU.mult, ALU.add, [vT, vec], [cv])
                    for k in range(1, 31):
                        STT(acc, vT[:, j, k:k + TB], V(V_DWW + j * 31 + k), acc, ALU.mult, ALU.add, [vT, vec, cv], [cv])
                    sq = sqt[j % 2]
                    ACT(sq[:], acc, AF.Square, [cv], [sq])
                    MM(pmean[:, 0:TB], onesm[:], acc, j == 0, j == 7, [onesm, cv], [pmean])
                    MM(pmsq[:, 0:TB], onesm[:], sq[:], j == 0, j == 7, [onesm, sq], [pmsq])
                    yield
                CP("act", meansb[:], pmean[:, 0:TB], [pmean], [meansb])
                TT("dve", rstdsb[:], meansb[:], meansb[:], ALU.mult, [meansb], [rstdsb])
                TT("dve", rstdsb[:], pmsq[:, 0:TB], rstdsb[:], ALU.subtract, [pmsq, rstdsb], [rstdsb])
                TS("dve", rstdsb[:], rstdsb[:], EPS, None, ALU.add, None, [rstdsb], [rstdsb])
                ACT(rstdsb[:], rstdsb[:], AF.Sqrt, [rstdsb], [rstdsb])
                RECIP(rstdsb[:], rstdsb[:], [rstdsb], [rstdsb])
                for j in range(8):
                    t_ = tmpa[j % 2]
                    TT("dve", t_[:], cv[:, j, :], meansb[:], ALU.subtract, [cv, meansb], [t_])
                    TT("dve", t_[:], t_[:], rstdsb[:], ALU.mult, [t_, rstdsb], [t_])
                    ACT(vn[:, j, :], t_[:], AF.Silu, [t_, vec], [vn], scale=V(V_CLNG + j), bias=V(V_CLNB + j))
                yield
                if bi == 0:
                    dump("vT", vT, vT[:], [128, 8, 30 + TB]); dump("cv", cv, cv[:], [128, 8, TB]); dump("vn", vn, vn[:], [128, 8, TB], BF16)
                CP("dve", vT[:, :, 0:30], vT[:, :, TB:TB + 30], [vT], [vT])
                for db in range(4):
                    wb = wload(cpw[:, db * 512:(db + 1) * 512], 8)
                    wg = wload(win[:, 3072 + db * 512:3072 + (db + 1) * 512], 16)
                    for dd in range(4):
                        d = db * 4 + dd
                        py = nps()
                        proj(py, py[:, 0:TB], wb, dd * 128, vn, lambda k: vn[:, k, :], 8)
                        pgt = nps()
                        proj(pgt, pgt[:, 0:TB], wg, dd * 128, hT, lambda k: hT[:, k, :], 16)
                        sg = sqt[d % 2]
                        ACT(sg[:], pgt[:, 0:TB], AF.Sigmoid, [pgt, vec], [sg], bias=V(V_GATEB + d))
                        STT(merged[:, d, :], py[:, 0:TB], V(V_CPB + d), sg[:], ALU.add, ALU.mult, [py, vec, sg], [merged])
                        yield
                if bi == 0:
                    dump("mergedc", merged, merged[:], [128, 16, TB], BF16)

            u_proj()
            gc = s5_scan_gen(True); gd = gen_def()
            alive_c = alive_d = True
            while alive_c or alive_d:
                if alive_c:
                    try:
                        next(gc)
                    except StopIteration:
                        alive_c = False
                for _ in range(3):
                    if alive_d:
                        try:
                            next(gd)
                        except StopIteration:
                            alive_d = False
            for q in range(8):
                t_ = tmpa[q % 2]
                ACT(t_[:], ys[:, q, :], AF.Square, [ys], [t_])
                TS("dve", t_[:], t_[:], 0.044715, 1.0, ALU.mult, ALU.add, [t_], [t_])
                TT("dve", t_[:], t_[:], ys[:, q, :], ALU.mult, [t_, ys], [t_])
                ACT(t_[:], t_[:], AF.Sigmoid, [t_], [t_], scale=1.5957691216057308)
                TT("dve", gy[:, q, :], t_[:], ys[:, q, :], ALU.mult, [t_, ys], [gy])
            for db in range(4):
                wab = nwb()
                DMA("pool", wab[:, 0:8, :], gwa[:, db * 512:(db + 1) * 512].rearrange("(k p) n -> p k n", p=128), W=[wab])
                DMA("pool", wab[:, 8:16, :], gwb[:, db * 512:(db + 1) * 512].rearrange("(k p) n -> p k n", p=128), W=[wab])
                wg = wload(win[:, 5120 + db * 512:5120 + (db + 1) * 512], 16)
                for dd in range(4):
                    d = db * 4 + dd
                    pa_ = nps()
                    for k in range(8):
                        MM(pa_[:, 0:TB], wab[:, k, dd * 128:(dd + 1) * 128], gy[:, k, :], k == 0, k == 7, [wab, gy], [pa_])
                    pb_ = nps()
                    for k in range(8):
                        MM(pb_[:, 0:TB], wab[:, 8 + k, dd * 128:(dd + 1) * 128], gy[:, k, :], k == 0, k == 7, [wab, gy], [pb_])
                    pg2_ = nps()
                    proj(pg2_, pg2_[:, 0:TB], wg, dd * 128, hT, lambda k: hT[:, k, :], 16)
                    t1 = tmpa[0]; t2 = tmpa[1]; t3 = tmpa[2]
                    ACT(t1[:], pb_[:, 0:TB], AF.Sigmoid, [pb_], [t1])
                    TT("dve", t2[:], pa_[:, 0:TB], t1[:], ALU.mult, [pa_, t1], [t2])
                    ACT(t3[:], pg2_[:, 0:TB], AF.Sigmoid, [pg2_, vec], [t3], bias=V(V_GATEB + 16 + d))
                    TT("dve", t2[:], t2[:], t3[:], ALU.mult, [t2, t3], [t2])
                    TT("dve", merged[:, d, :], merged[:, d, :], t2[:], ALU.add, [merged, t2], [merged])
            if bi == 0:
                dump("uT", uT, uT[:], [128, 8, TB], BF16); dump("ys", ys, ys[:], [128, 8, TB]); dump("gy", gy, gy[:], [128, 8, TB], BF16)
                dump("merged", merged, merged[:], [128, 16, TB], BF16)
            for tt in range(TB // 128):
                ti = bi * (TB // 128) + tt
                r0 = ti * 128
                for nb in range(4):
                    wo = wload(wout[:, nb * 512:(nb + 1) * 512], 16)
                    pm = nps()
                    for k in range(16):
                        MM(pm[:, :], merged[:, k, tt * 128:(tt + 1) * 128], wo[:, k, :], k == 0, k == 15, [merged, wo], [pm])
                    CP("act", msb[:, nb * 512:(nb + 1) * 512], pm[:, :], [pm], [msb])
                rms_rstd(msb[:], msb, 1)
                STT(msb[:], msb[:], ssq[:, 1:2], pg1[:], ALU.mult, ALU.mult, [msb, ssq, pg1], [msb])
                DMA("sp", xt0[:], xo[r0:r0 + 128, :], W=[xt0])
                TT("dve", xt0[:], xt0[:], msb[:], ALU.add, [xt0, msb], [xt0])
                DMA("sp", outd[r0:r0 + 128, :], xt0[:], R=[xt0], W=[outdram])
                rms_rstd(xt0[:], xt0, 2)
                ACT(msb[:], xt0[:], AF.Identity, [xt0, ssq], [msb], scale=ssq[:, 2:3])
                for k4 in range(4):
                    p = nps()
                    for kk in range(4):
                        k = k4 * 4 + kk
                        TR(p[:, kk * 128:(kk + 1) * 128], msb[:, k * 128:(k + 1) * 128], identf[:], [msb, identf], [p])
                    for kk in range(4):
                        k = k4 * 4 + kk
                        ACT(h2Tf[:, k, :], p[:, kk * 128:(kk + 1) * 128], AF.Identity, [p, scale2, adacol], [h2Tf],
                            scale=scale2[:, k:k + 1], bias=sh2(k))
                pl = nps()
                for k in range(16):
                    MM(pl[:, 0:E], h2Tf[:, k, :], rwf[:, k * E:(k + 1) * E], k == 0, k == 15, [h2Tf, rwf], [pl])
                TT("dve", lg[:], pl[:, 0:E], rbb[:], ALU.add, [pl, rbb], [lg])
                P.op("dve", lambda e: e.max(out=mx8[:], in_=lg[:]), [lg], [mx8])
                TS("dve", gmask[:], lg[:], mx8[:, 3:4], None, ALU.is_ge, None, [lg, mx8], [gmask])
                TS("dve", small[:, 0:1], mx8[:, 0:1], -1.0, None, ALU.mult, None, [mx8], [small])
                ACT(gsm[:], lg[:], AF.Exp, [lg, small], [gsm], bias=small[:, 0:1])
                TT("dve", gsm[:], gsm[:], gmask[:], ALU.mult, [gsm, gmask], [gsm])
                P.op("dve", lambda e: e.tensor_reduce(out=small[:, 1:2], in_=gsm[:], axis=AX.X, op=ALU.add), [gsm], [small])
                RECIP(small[:, 1:2], small[:, 1:2], [small], [small])
                TS("dve", gsm[:], gsm[:], small[:, 1:2], None, ALU.mult, None, [gsm, small], [gsm])
                pt_ = nps()
                TR(pt_[0:E, 0:128], gsm[:], identf[:], [gsm, identf], [pt_])
                CP("act", GT[:, r0:r0 + 128], pt_[0:E, 0:128], [pt_], [GT])
                ACT(xn[:], xt0[:], AF.Identity, [xt0, ssq], [xn], scale=ssq[:, 2:3])
                pc = nps()
                MM(pc[:, 0:E], tri[:], gmask[:], True, True, [tri, gmask], [pc])
                MM(pc[:, E:2 * E], onesf[:], gmask[:], True, True, [onesf, gmask], [pc])
                slot, valid, idxm, rk, selk, tmpk = rt
                TT("dve", slot[:], pc[:, 0:E], basebc[:], ALU.add, [pc, basebc], [slot])
                TT("dve", basebc[:], basebc[:], pc[:, E:2 * E], ALU.add, [pc, basebc], [basebc])
                TS("dve", valid[:], slot[:], float(CAP), None, ALU.is_lt, None, [slot], [valid])
                TT("dve", valid[:], valid[:], gmask[:], ALU.mult, [valid, gmask], [valid])
                TT("dve", idxm[:], slot[:], ecap[:], ALU.add, [slot, ecap], [idxm])
                TS("dve", idxm[:], idxm[:], -BIGI, None, ALU.add, None, [idxm], [idxm])
                P.op("dve", lambda e: e.tensor_tensor_scan(out=rk[:], data0=onesf[:, 0:E], data1=gmask[:], initial=0.0,
                                                           op0=ALU.mult, op1=ALU.add), [onesf, gmask], [rk])
                TT("dve", rk[:], rk[:], gmask[:], ALU.subtract, [rk, gmask], [rk])
                for k in range(4):
                    TS("dve", selk[:], rk[:], float(k), None, ALU.is_equal, None, [rk], [selk])
                    TT("dve", selk[:], selk[:], valid[:], ALU.mult, [selk, valid], [selk])
                    TT("dve", tmpk[:], selk[:], idxm[:], ALU.mult, [selk, idxm], [tmpk])
                    P.op("dve", lambda e, k=k, ti=ti: e.tensor_reduce(out=IDXF[:, ti, k:k + 1], in_=tmpk[:], axis=AX.X, op=ALU.add), [tmpk], [IDXF])
                    TT("dve", tmpk[:], selk[:], gsm[:], ALU.mult, [selk, gsm], [tmpk])
                    P.op("dve", lambda e, k=k, ti=ti: e.tensor_reduce(out=GK[:, ti, k:k + 1], in_=tmpk[:], axis=AX.X, op=ALU.add), [tmpk], [GK])
                TS("dve", IDXF[:, ti, :], IDXF[:, ti, :], BIGI, None, ALU.add, None, [IDXF], [IDXF])
                CP("dve", IDXI[:, ti, :], IDXF[:, ti, :], [IDXF], [IDXI])
                for k in range(4):
                    P.dma("pool", lambda e, k=k, ti=ti: e.indirect_dma_start(
                        out=Xgd[:, :], out_offset=bass.IndirectOffsetOnAxis(ap=IDXI[:, ti, k:k + 1], axis=0),
                        in_=xn[:], in_offset=None, bounds_check=breg[0], oob_is_err=False), [xn, IDXI], [xgdram])
            if bi == 0:
                dump("lg", lg, lg[:], [128, E]); dump("gsm", gsm, gsm[:], [128, E])
                dump("idx", IDXI, IDXI[:, 1, :], [128, 4], mybir.dt.int32); dump("gk", GK, GK[:, 1, :], [128, 4])
                stop_at("b0")

        def actb_alias(v):
            b = Buf(v)
            abufs.append(b)
            return b

        stop_at("p1b")
        barrier()
        XgT = carve([128, 16, CAP], BF16); actb = carve([128, 16, CAP], BF16)
        xgs = [carve([128, D], BF16) for i in range(2)]
        ybuf = [carve([128, 512]) for i in range(4)]
        gtt = [carve([128, SBW]) for i in range(2)]; ltt = [carve([128, SBW])]; sgt = [carve([128, SBW])]
        NST = CAP // 128
        yi = [0]
        for e in range(E):
            for st_ in range(NST):
                xg = xgs[st_ % 2]
                DMA("sp", xg[:], Xgd[e * CAP + st_ * 128:e * CAP + (st_ + 1) * 128, :], R=[xgdram], W=[xg])
                for k4 in range(4):
                    p = nps()
                    pb = p[:].bitcast(BF16)
                    for kk in range(4):
                        k = k4 * 4 + kk
                        TR(pb[:, kk * 128:(kk + 1) * 128], xg[:, k * 128:(k + 1) * 128], identb[:], [xg, identb], [p])
                    for kk in range(4):
                        k = k4 * 4 + kk
                        ACT(XgT[:, k, st_ * 128:(st_ + 1) * 128], pb[:, kk * 128:(kk + 1) * 128], AF.Identity,
                            [p, scale2, adacol], [XgT], scale=scale2[:, k:k + 1], bias=sh2(k))
            for fb in range(8):
                wb = nwb()
                DMA("pool", wb[:, :, 0:256], w1g[e][:, fb * 256:(fb + 1) * 256].rearrange("(k p) n -> p k n", p=128), W=[wb])
                DMA("pool", wb[:, :, 256:512], w1l[e][:, fb * 256:(fb + 1) * 256].rearrange("(k p) n -> p k n", p=128), W=[wb])
                for fc in range(2):
                    f = fb * 2 + fc
                    for sb_ in range(CAP // SBW):
                        s0 = sb_ * SBW
                        pg_ = nps()
                        proj(pg_, pg_[:, 0:SBW], wb, fc * 128, XgT, lambda k: XgT[:, k, s0:s0 + SBW], 16)
                        pl_ = nps()
                        proj(pl_, pl_[:, 0:SBW], wb, 256 + fc * 128, XgT, lambda k: XgT[:, k, s0:s0 + SBW], 16)
                        g_ = gtt[(f + sb_) % 2]; l_ = ltt[0]; s_ = sgt[0]
                        TS("dve", g_[:], pg_[:, 0:SBW], V(V_B1G + e * 16 + f), 7.0, ALU.add, ALU.min, [pg_, vec], [g_])
                        ACT(s_[:], g_[:], AF.Sigmoid, [g_], [s_], scale=1.702)
                        TS("dve", l_[:], pl_[:, 0:SBW], V(V_B1L + e * 16 + f), 7.0, ALU.add, ALU.min, [pl_, vec], [l_])
                        TS("dve", l_[:], l_[:], -7.0, 1.0, ALU.max, ALU.add, [l_], [l_])
                        TT("dve", g_[:], g_[:], s_[:], ALU.mult, [g_, s_], [g_])
                        TT("dve", actb[:, f, s0:s0 + SBW], g_[:], l_[:], ALU.mult, [g_, l_], [actb])
            for db in range(4):
                wb = wload(w2d[e][:, db * 512:(db + 1) * 512], 16)
                for st_ in range(NST):
                    py = nps()
                    for k in range(16):
                        MM(py[:, :], actb[:, k, st_ * 128:(st_ + 1) * 128], wb[:, k, :], k == 0, k == 15, [actb, wb], [py])
                    yb = ybuf[yi[0] % 4]
                    yi[0] += 1
                    CP("act", yb[:], py[:, :], [py], [yb])
                    DMA("sp", Ygd[e * CAP + st_ * 128:e * CAP + (st_ + 1) * 128, db * 512:(db + 1) * 512], yb[:], R=[yb], W=[ygdram])
        barrier()
        facc = carve([128, D]); yk = [carve([128, D]) for i in range(2)]; junk = carve([128, D], BF16)
        b2s = carve([128, D])
        DMA("sp", b2s[0:E, :], b2d, W=[b2s])
        gi = [0]
        for bi in range(NT):
            r0 = bi * 128
            for nb in range(4):
                pbias = nps()
                MM(pbias[:, :], GT[:, r0:r0 + 128], b2s[0:E, nb * 512:(nb + 1) * 512], True, True, [GT, b2s], [pbias])
                CP("act", facc[:, nb * 512:(nb + 1) * 512], pbias[:, :], [pbias], [facc])
            for k in range(4):
                y_ = yk[gi[0] % 2]
                gi[0] += 1
                MEMSET("pool", y_[:], 0.0, [y_])
                P.dma("pool", lambda e, k=k, bi=bi, y_=y_: e.indirect_dma_start(
                    out=y_[:], out_offset=None, in_=Ygd[:, :],
                    in_offset=bass.IndirectOffsetOnAxis(ap=IDXI[:, bi, k:k + 1], axis=0),
                    bounds_check=breg[0], oob_is_err=False), [IDXI, ygdram], [y_])
                STT(facc[:], y_[:], GK[:, bi, k:k + 1], facc[:], ALU.mult, ALU.add, [y_, GK, facc], [facc])
            ACT(junk[:], facc[:], AF.Square, [facc], [junk, ssq], accum_out=ssq[:, 3:4])
            TS("dve", ssq[:, 3:4], ssq[:, 3:4], 1.0 / D, EPS, ALU.mult, ALU.add, [ssq], [ssq])
            ACT(ssq[:, 3:4], ssq[:, 3:4], AF.Sqrt, [ssq], [ssq])
            RECIP(ssq[:, 3:4], ssq[:, 3:4], [ssq], [ssq])
            STT(facc[:], facc[:], ssq[:, 3:4], pg2[:], ALU.mult, ALU.mult, [facc, ssq, pg2], [facc])
            DMA("sp", xt0[:], outd[r0:r0 + 128, :], R=[outdram], W=[xt0])
            TT("dve", xt0[:], xt0[:], facc[:], ALU.add, [xt0, facc], [xt0])
            DMA("sp", outd[r0:r0 + 128, :], xt0[:], R=[xt0], W=[outdram])

    try:
        body()
    except _Stop:
        pass

    sems = {}
    for k in ("pe", "act", "dve", "pool", "sp"):
        sems[("c", k)] = es.enter_context(nc.semaphore(f"c_{k}"))
    for q in ("sp", "pool"):
        for i in range(Prog.R):
            sems[("d", q, i)] = es.enter_context(nc.semaphore(f"d_{q}{i}"))

    breg = [None]

    def run(name, e):
        if name == "pool":
            breg[0] = e.to_reg(E * CAP - 1)
        for waits, fn, key, amt in P.ops[name]:
            for wk, wv in waits:
                e.wait_ge(sems[wk], wv)
            fn(e).then_inc(sems[key], amt)
        if name == "sp":
            n = P.dq["sp"]
            for i in range(Prog.R):
                cnt = (n - i + Prog.R - 1) // Prog.R
                if cnt > 0:
                    e.wait_ge(sems[("d", "sp", i)], 16 * cnt)

    with es:
        with nc.Block() as block:
            @block.tensor
            def _(e):
                run("pe", e)

            @block.scalar
            def _(e):
                run("act", e)

            @block.vector
            def _(e):
                run("dve", e)

            @block.gpsimd
            def _(e):
                run("pool", e)

            @block.sync
            def _(e):
                run("sp", e)
    return nc


def _prep(inputs):
    f32 = np.float32
    g = {k: np.asarray(v) for k, v in inputs.items()}
    x = g["x"]; c = g["c"]
    sh = {}
    sh["ada_w"] = np.ascontiguousarray(g["ada_w"][0]); sh["ada_b"] = np.ascontiguousarray(g["ada_b"][0][None, :])
    def fm(v, n):
        return np.ascontiguousarray(v.reshape(n, 128).T)
    vec = np.zeros((128, NV), f32)
    vec[:, V_PREMIX:V_PREMIX + 16] = fm(g["pre_mix_g"][0], 16)
    vec[:, V_PREFFN:V_PREFFN + 16] = fm(g["pre_ffn_g"][0], 16)
    vec[:, V_GATEB:V_GATEB + 32] = fm(g["gate_b"][0], 32)
    vec[:, V_DWB:V_DWB + 8] = fm(g["dw_b"][0], 8)
    vec[:, V_CLNG:V_CLNG + 8] = fm(g["cln_g"][0], 8)
    vec[:, V_CLNB:V_CLNB + 8] = fm(g["cln_b"][0], 8)
    vec[:, V_CPB:V_CPB + 16] = fm(g["cp_b"][0], 16)
    vec[:, V_S5D:V_S5D + 8] = fm(g["s5_d"][0], 8)
    dww = g["dw_w"][0]
    vec[:, V_DWW:V_DWW + 248] = dww.T.reshape(8, 128, 31).transpose(1, 0, 2).reshape(128, 248)
    b1 = g["b1"][0]
    vec[:, V_B1G:V_B1G + 512] = b1[:, 0::2].reshape(32, 16, 128).transpose(2, 0, 1).reshape(128, 512)
    vec[:, V_B1L:V_B1L + 512] = b1[:, 1::2].reshape(32, 16, 128).transpose(2, 0, 1).reshape(128, 512)
    vec[:, V_B2:V_B2 + 512] = g["b2"][0].reshape(32, 16, 128).transpose(2, 0, 1).reshape(128, 512)
    pidx = (np.arange(128) // 32) % 2
    vec[:, V_M0] = (pidx == 0).astype(f32); vec[:, V_M0 + 1] = (pidx == 1).astype(f32)
    sh["vec"] = vec
    sh["routerb"] = np.ascontiguousarray(g["router_b"][0][None, :])
    sh["postmix"] = np.ascontiguousarray(g["post_mix_g"][0][None, :])
    sh["postffn"] = np.ascontiguousarray(g["post_ffn_g"][0][None, :])
    are, aim, ldt = g["s5_a_re"][0], g["s5_a_im"][0], g["s5_log_dt"][0]
    ldt2 = np.broadcast_to(ldt[:, None], (64, 64))
    def Sl(a):
        return a.reshape(32, 2, 64).transpose(1, 2, 0).reshape(128, 32)
    sh["aS"] = np.ascontiguousarray(np.concatenate([Sl(are), Sl(aim), Sl(ldt2)], axis=1).astype(f32))
    def Pl(a):
        t = a.reshape(8, 4, 2, 64)
        t = t.transpose(1, 0, 2, 3)
        t = np.broadcast_to(t[:, None, None], (4, 2, 16, 8, 2, 64))
        return t.reshape(128, 1024)
    sh["aP"] = np.ascontiguousarray(np.concatenate([Pl(are), Pl(aim), Pl(ldt2)], axis=1).astype(f32))
    def Bexp(b):
        t = b.reshape(8, 4, 2, 64, 16)
        o = np.zeros((4, 2, 16, 8, 2, 64), f32)
        for gl in range(2):
            o[:, gl, :, :, gl, :] = t[:, :, gl].transpose(1, 3, 0, 2)
        return o.reshape(128, 1024)
    sh["bexp"] = np.concatenate([Bexp(g["s5_b_re"][0]), Bexp(g["s5_b_im"][0])], axis=1)
    def Cexp(cc):
        t = cc.reshape(32, 2, 16, 64)
        o = np.zeros((2, 64, 32, 2, 2, 16), f32)
        for kt in range(32):
            for gl in range(2):
                o[gl, :, kt, kt % 2, gl, :] = t[kt, gl].T
        return o.reshape(128, 2048)
    sh["cexp"] = np.concatenate([Cexp(g["s5_c_re"][0]), Cexp(g["s5_c_im"][0])], axis=1)
    sh["w_in"] = np.ascontiguousarray(g["w_in"][0]); sh["cp_w"] = np.ascontiguousarray(g["cp_w"][0])
    sh["glu_wa"] = np.ascontiguousarray(g["glu_wa"][0]); sh["glu_wb"] = np.ascontiguousarray(g["glu_wb"][0])
    sh["w_out"] = np.ascontiguousarray(g["w_out"][0]); sh["router_w"] = np.ascontiguousarray(g["router_w"][0])
    w1 = g["w1"][0]
    sh["w1g"] = np.ascontiguousarray(w1[:, :, 0::2]); sh["w1l"] = np.ascontiguousarray(w1[:, :, 1::2])
    sh["w2"] = np.ascontiguousarray(g["w2"][0])
    sh["ident"] = np.eye(128, dtype=f32)
    sh["tri"] = np.triu(np.ones((128, 128), f32), 1)
    sh["ecap"] = np.broadcast_to((np.arange(E, dtype=f32) * CAP)[None, :], (128, E)).copy()
    sh["b2s"] = np.ascontiguousarray(g["b2"][0])
    maps = []
    for core in range(8):
        b, half = core // 2, core % 2
        m = dict(sh)
        m["xo"] = np.ascontiguousarray(x[b, half * NTOK:(half + 1) * NTOK])
        m["xp"] = np.ascontiguousarray(x[b, 0:NTOK]) if half == 1 else np.zeros((NTOK, D), f32)
        m["flag"] = np.full((128, 1), float(half), f32)
        m["ccol"] = fm(c[b], 16)
        maps.append(m)
    return maps


def kernel(**inputs):
    maps = _prep(inputs)
    nc = build_nc()
    res = run_bass_kernel_spmd(nc, maps, core_ids=list(range(8)))
    out = np.zeros((4, 4096, D), np.float32)
    for core in range(8):
        b, half = core // 2, core % 2
        out[b, half * NTOK:(half + 1) * NTOK] = res.results[core]["out"]
    return out
```
